# Optimizing a Trainium2 kernel written in Bass

```python
import math
import jax
import jax.numpy as jnp
from jax import lax
import numpy as np


D_MODEL = 1024
BATCH = 16
SEQ = 2048
DEPTH = 4

A_HEADS = 8
A_KV_HEADS = 2
A_HEAD_DIM = 64
A_WIDTH = A_HEADS * A_HEAD_DIM
IDX_HEADS = 8
IDX_DIM = 32
TOPK_MAX = 256
Q_BLOCK = 128
ROPE_THETA = 500000.0
ROPE_FRACTION = 4
SC_WIDTH = 512
SC_CONV = 3
RW_HEADS = 8
RW_HEAD_DIM = 64
RW_WIDTH = RW_HEADS * RW_HEAD_DIM
RW_W_LORA = 64
RW_A_LORA = 64
RW_G_LORA = 128
RW_IN = 3 * RW_WIDTH + RW_W_LORA + RW_A_LORA + RW_G_LORA
RW_GN_EPS = 64e-5
N_BRANCH = 3
BRANCH_WIDTH = 512
D_FF = 2816
FFN_CONV = 3
RMS_EPS = 1e-6
POS_OFFSET_MAX = 4096

IN_SIZES = (A_WIDTH, A_KV_HEADS * A_HEAD_DIM, A_KV_HEADS * A_HEAD_DIM,
            IDX_HEADS * IDX_DIM, IDX_DIM, IDX_HEADS,
            SC_WIDTH, SC_WIDTH, SC_WIDTH,
            RW_IN,
            N_BRANCH * D_MODEL)
N_IN = sum(IN_SIZES)

kernel_name = 'hybrid_dsa_shortconv_rwkv7_block'


def rms_norm(x, g):
    xf = x.astype(jnp.float32)
    y = xf * lax.rsqrt(jnp.mean(xf * xf, axis=-1, keepdims=True) + RMS_EPS)
    return (y * g.astype(jnp.float32)).astype(x.dtype)


def partial_rope(x, pos):
    dh = x.shape[-1]
    rot = dh // ROPE_FRACTION
    half = rot // 2
    inv = ROPE_THETA ** (-jnp.arange(half, dtype=jnp.float32) * 2.0 / rot)
    ang = pos.astype(jnp.float32)[:, :, None] * inv
    cos = jnp.cos(ang)[:, :, None, :]
    sin = jnp.sin(ang)[:, :, None, :]
    xf = x.astype(jnp.float32)
    x1 = xf[..., :half]
    x2 = xf[..., half:rot]
    out = jnp.concatenate([x1 * cos - x2 * sin, x2 * cos + x1 * sin, xf[..., rot:]], axis=-1)
    return out.astype(x.dtype)


def causal_dwconv(u, w):
    width = w.shape[0]
    seq = u.shape[1]
    up = jnp.pad(u, ((0, 0), (width - 1, 0), (0, 0)))
    out = up[:, 0:seq] * w[0]
    for j in range(1, width):
        out = out + up[:, j:j + seq] * w[j]
    return out


def dsa_attention(q, k, v, qi, ki, wi):
    bsz, seq = q.shape[0], q.shape[1]
    k_sel = min(TOPK_MAX, seq // 4)
    n_blk = seq // Q_BLOCK
    group = A_HEADS // A_KV_HEADS
    scale = A_HEAD_DIM ** -0.5
    idx_scale = (IDX_HEADS ** -0.5) * (IDX_DIM ** -0.5)
    b_idx = jnp.arange(bsz)[:, None, None]
    key_pos = jnp.arange(seq)

    def block(i):
        t0 = i * Q_BLOCK
        q_b = lax.dynamic_slice_in_dim(q, t0, Q_BLOCK, axis=1)
        qi_b = lax.dynamic_slice_in_dim(qi, t0, Q_BLOCK, axis=1)
        wi_b = lax.dynamic_slice_in_dim(wi, t0, Q_BLOCK, axis=1)
        t_pos = t0 + jnp.arange(Q_BLOCK)
        causal = key_pos[None, :] <= t_pos[:, None]
        rel = jax.nn.relu(jnp.einsum('bthd,bsd->bths', qi_b, ki).astype(jnp.float32))
        isc = jnp.einsum('bths,bth->bts', rel, wi_b.astype(jnp.float32) * idx_scale)
        isc = jnp.where(causal[None], isc, -jnp.inf)
        _, sel = lax.top_k(isc, k_sel)
        valid = sel <= t_pos[None, :, None]
        k_g = k[b_idx, sel]
        v_g = v[b_idx, sel]
        q_g = q_b.reshape(bsz, Q_BLOCK, A_KV_HEADS, group, A_HEAD_DIM)
        s = jnp.einsum('btngd,btknd->btngk', q_g, k_g).astype(jnp.float32) * scale
        s = jnp.where(valid[:, :, None, None, :], s, -jnp.inf)
        p = jax.nn.softmax(s, axis=-1).astype(v.dtype)
        o = jnp.einsum('btngk,btknd->btngd', p, v_g)
        return o.reshape(bsz, Q_BLOCK, A_WIDTH)

    out = lax.map(block, jnp.arange(n_blk))
    return out.transpose(1, 0, 2, 3).reshape(bsz, seq, A_WIDTH)


def short_conv_mixer(u, sc_b, sc_c, w_conv):
    return sc_b * causal_dwconv(sc_c * u, w_conv)


def rwkv7_time_mix(z, mu, w0, w_up, a0, a_up, g_up, k_k, k_a, r_k, ln_w, ln_b):
    bsz, seq = z.shape[0], z.shape[1]
    f32 = jnp.float32
    z_prev = jnp.pad(z, ((0, 0), (1, 0), (0, 0)))[:, :seq]
    z = z + (z_prev - z) * mu
    r, k, v, lw, la, lg = jnp.split(
        z, [RW_WIDTH, 2 * RW_WIDTH, 3 * RW_WIDTH, 3 * RW_WIDTH + RW_W_LORA,
            3 * RW_WIDTH + RW_W_LORA + RW_A_LORA], axis=-1)
    zw = (w0 + jnp.tanh(lw) @ w_up).astype(f32)
    decay = jnp.exp(-math.exp(-0.5) * jax.nn.sigmoid(zw))
    a = jax.nn.sigmoid((a0 + la @ a_up).astype(f32))
    g = (jax.nn.sigmoid(lg) @ g_up).astype(f32)
    hs = (bsz, seq, RW_HEADS, RW_HEAD_DIM)
    r = r.astype(f32).reshape(hs)
    v = v.astype(f32).reshape(hs)
    kf = k.astype(f32)
    decay = decay.reshape(hs)
    a = a.reshape(hs)
    kk = (kf * k_k.astype(f32)).reshape(hs)
    kk = kk * lax.rsqrt(jnp.maximum(jnp.sum(kk * kk, axis=-1, keepdims=True), 1e-24))
    kf = (kf * (1.0 + (a.reshape(bsz, seq, RW_WIDTH) - 1.0) * k_a.astype(f32))).reshape(hs)

    def step(state, inp):
        r_t, w_t, k_t, v_t, ka_t, kb_t = inp
        sa = jnp.einsum('bhvk,bhk->bhv', state, ka_t)
        state = (state * w_t[:, :, None, :] + sa[..., None] * kb_t[:, :, None, :]
                 + v_t[..., None] * k_t[:, :, None, :])
        y_t = jnp.einsum('bhvk,bhk->bhv', state, r_t)
        return state, y_t

    tm = lambda t: jnp.swapaxes(t, 0, 1)
    xs = (tm(r), tm(decay), tm(kf), tm(v), tm(-kk), tm(kk * a))
    s0 = jnp.zeros((bsz, RW_HEADS, RW_HEAD_DIM, RW_HEAD_DIM), f32)
    _, y = lax.scan(step, s0, xs)
    y = jnp.swapaxes(y, 0, 1)
    mean = jnp.mean(y, axis=-1, keepdims=True)
    var = jnp.mean(jnp.square(y - mean), axis=-1, keepdims=True)
    yn = ((y - mean) * lax.rsqrt(var + RW_GN_EPS) * ln_w.astype(f32).reshape(RW_HEADS, RW_HEAD_DIM)
          + ln_b.astype(f32).reshape(RW_HEADS, RW_HEAD_DIM))
    bonus = jnp.sum(r * kf * r_k.astype(f32), axis=-1, keepdims=True) * v
    out = (yn + bonus) * g.reshape(hs)
    return out.reshape(bsz, seq, RW_WIDTH).astype(z.dtype)


def conv_glu_ffn(x, w_up, w_conv, w_down):
    u = causal_dwconv(x @ w_up, w_conv)
    gate, up = jnp.split(u, 2, axis=-1)
    return (jax.nn.silu(gate) * up) @ w_down


def setup_inputs(seed: int = 0) -> dict:
    key = jax.random.key(seed)
    ks = jax.random.split(key, 26)
    f32 = jnp.float32

    def nrm(k, shape, scale):
        return jax.random.normal(k, shape, f32) * scale

    L = DEPTH
    x = nrm(ks[0], (BATCH, SEQ, D_MODEL), 1.0)
    positions = (jax.random.randint(ks[1], (BATCH, 1), 0, POS_OFFSET_MAX, dtype=jnp.int32)
                 + jnp.arange(SEQ, dtype=jnp.int32)[None, :])
    return {
        'x': x,
        'positions': positions,
        'norm_mix': 1.0 + nrm(ks[2], (L, D_MODEL), 0.02),
        'w_in': nrm(ks[3], (L, D_MODEL, N_IN), D_MODEL ** -0.5),
        'b_gate': nrm(ks[4], (L, N_BRANCH * D_MODEL), 0.02),
        'sc_conv': nrm(ks[5], (L, SC_CONV, SC_WIDTH), SC_CONV ** -0.5),
        'rw_mu': jax.random.uniform(ks[6], (L, RW_IN), f32),
        'rw_w0': -1.0 + nrm(ks[7], (L, RW_WIDTH), 0.5),
        'rw_w_up': nrm(ks[8], (L, RW_W_LORA, RW_WIDTH), 0.5 * RW_W_LORA ** -0.5),
        'rw_a0': nrm(ks[9], (L, RW_WIDTH), 0.1),
        'rw_a_up': nrm(ks[10], (L, RW_A_LORA, RW_WIDTH), 0.5 * RW_A_LORA ** -0.5),
        'rw_g_up': nrm(ks[11], (L, RW_G_LORA, RW_WIDTH), RW_G_LORA ** -0.5),
        'rw_k_k': 0.85 + nrm(ks[12], (L, RW_WIDTH), 0.05),
        'rw_k_a': 1.0 + nrm(ks[13], (L, RW_WIDTH), 0.05),
        'rw_r_k': nrm(ks[14], (L, RW_HEADS, RW_HEAD_DIM), 0.1),
        'rw_ln_w': 1.0 + nrm(ks[15], (L, RW_WIDTH), 0.02),
        'rw_ln_b': nrm(ks[16], (L, RW_WIDTH), 0.02),
        'w_branch': nrm(ks[17], (L, N_BRANCH, BRANCH_WIDTH, D_MODEL), BRANCH_WIDTH ** -0.5),
        'w_out': nrm(ks[18], (L, D_MODEL, D_MODEL), D_MODEL ** -0.5),
        'norm_ffn': 1.0 + nrm(ks[19], (L, D_MODEL), 0.02),
        'ffn_up': nrm(ks[20], (L, D_MODEL, 2 * D_FF), D_MODEL ** -0.5),
        'ffn_conv': nrm(ks[21], (L, FFN_CONV, 2 * D_FF), FFN_CONV ** -0.5),
        'ffn_down': nrm(ks[22], (L, D_FF, D_MODEL), D_FF ** -0.5),
        'norm_final': 1.0 + nrm(ks[23], (D_MODEL,), 0.02),
    }


def reference(x, positions, norm_mix, w_in, b_gate, sc_conv, rw_mu, rw_w0, rw_w_up, rw_a0,
              rw_a_up, rw_g_up, rw_k_k, rw_k_a, rw_r_k, rw_ln_w, rw_ln_b, w_branch, w_out,
              norm_ffn, ffn_up, ffn_conv, ffn_down, norm_final):
    bsz, seq = x.shape[0], x.shape[1]
    split_points = [int(p) for p in np.cumsum(IN_SIZES)[:-1]]
    h = x
    for l in range(DEPTH):
        xn = rms_norm(h, norm_mix[l])
        proj = xn @ w_in[l]
        (q, k, v, qi, ki, wi, sc_u, sc_b, sc_c, rw_z, gate_pre) = jnp.split(proj, split_points, axis=-1)
        q = partial_rope(q.reshape(bsz, seq, A_HEADS, A_HEAD_DIM), positions)
        k = partial_rope(k.reshape(bsz, seq, A_KV_HEADS, A_HEAD_DIM), positions)
        v = v.reshape(bsz, seq, A_KV_HEADS, A_HEAD_DIM)
        qi = partial_rope(qi.reshape(bsz, seq, IDX_HEADS, IDX_DIM), positions)
        ki = partial_rope(ki.reshape(bsz, seq, 1, IDX_DIM), positions)[:, :, 0]
        y_a = dsa_attention(q, k, v, qi, ki, wi)
        y_b = short_conv_mixer(sc_u, sc_b, sc_c, sc_conv[l])
        y_c = rwkv7_time_mix(rw_z, rw_mu[l], rw_w0[l], rw_w_up[l], rw_a0[l], rw_a_up[l],
                             rw_g_up[l], rw_k_k[l], rw_k_a[l], rw_r_k[l], rw_ln_w[l], rw_ln_b[l])
        branches = jnp.stack([y_a, y_b, y_c], axis=2)
        up = jnp.einsum('bsgc,gcd->bsgd', branches, w_branch[l])
        gates = jax.nn.sigmoid(gate_pre + b_gate[l]).reshape(bsz, seq, N_BRANCH, D_MODEL)
        mixed = jnp.sum(gates * up, axis=2)
        h = h + mixed @ w_out[l]
        h = h + conv_glu_ffn(rms_norm(h, norm_ffn[l]), ffn_up[l], ffn_conv[l], ffn_down[l])
    return rms_norm(h, norm_final)
```

```python
import math
import numpy as np
import concourse.bass as bass
import concourse.mybir as mybir
from concourse.bass_utils import run_bass_kernel_spmd

F32 = mybir.dt.float32
BF16 = mybir.dt.bfloat16
I32 = mybir.dt.int32
AF = mybir.ActivationFunctionType
ALU = mybir.AluOpType
AX = mybir.AxisListType

D = 1024
KC = 8
NIN = 7464
DFF = 2816
NFF = 22
OFF_Q, OFF_K, OFF_V, OFF_QI, OFF_KI, OFF_WI = 0, 512, 640, 768, 1024, 1056
OFF_SC, OFF_RW, OFF_GATE = 1064, 2600, 4392
ATT_MAIN = 960
ATT_COLS = 2 * ATT_MAIN + 136
RMS_EPS = 1e-6
C0 = math.exp(-0.5)
NEG = -1.0e30
BIS_ITERS = 14
STRICT = False


class Sched:
    def __init__(self, nc, n_dma_sems=32):
        if STRICT:
            n_dma_sems = 8
        self.nc = nc
        self.nc = nc
        self.E = {'pe': nc.tensor, 'act': nc.scalar, 'dve': nc.vector,
                  'pool': nc.gpsimd, 'sp': nc.sync}
        self.sem = {k: nc.alloc_semaphore('sem_' + k) for k in ('pe', 'act', 'dve', 'pool')}
        self.cnt = {k: 0 for k in self.sem}
        self.dsem = [nc.alloc_semaphore('dsem%d' % i) for i in range(n_dma_sems)]
        self.dval = [0] * n_dma_sems
        self.nrot = n_dma_sems
        self.drr = 0
        self.known = {k: {} for k in self.E}
        self.lastw = {}
        self.readers = {}
        self.nins = 0
        self.strict = STRICT
        self.strict_dma = STRICT

    def _semh(self, sk):
        return self.sem[sk] if isinstance(sk, str) else self.dsem[sk]

    def _wait(self, e, sk, val):
        if self.known[e].get(sk, 0) >= val:
            return
        self.E[e].wait_ge(self._semh(sk), val)
        self.known[e][sk] = val
        self.nins += 1

    def _deps(self, e, r, w):
        need = {}

        def add(sk, v):
            if need.get(sk, 0) < v:
                need[sk] = v
        for k in r:
            if k in self.lastw:
                add(*self.lastw[k])
        for k in w:
            if k in self.lastw:
                sk, v = self.lastw[k]
                if sk != e or self.strict:
                    add(sk, v)
            for sk, v in self.readers.get(k, {}).items():
                if sk != e or self.strict:
                    add(sk, v)
        for sk, v in need.items():
            self._wait(e, sk, v)

    def _commit(self, ev, r, w):
        for k in w:
            self.lastw[k] = ev
            self.readers[k] = {}
        for k in r:
            d = self.readers.setdefault(k, {})
            if d.get(ev[0], 0) < ev[1]:
                d[ev[0]] = ev[1]

    def op(self, e, fn, r=(), w=()):
        self._deps(e, r, w)
        ins = fn(self.E[e])
        self.cnt[e] += 1
        ins.then_inc(self.sem[e], 1)
        self.nins += 1
        self._commit((e, self.cnt[e]), r, w)

    def dma(self, q, out, in_, r=(), w=()):
        self._deps(q, r, w)
        if self.strict_dma and q == 'pool':
            self.dsem.append(self.nc.alloc_semaphore('dsx%d' % len(self.dsem)))
            self.dval.append(0)
            i = len(self.dsem) - 1
            ins = self.E[q].dma_start(out=out, in_=in_)
            self.dval[i] += 16
            ins.then_inc(self.dsem[i], 16)
            self._commit((i, self.dval[i]), r, w)
            return
        i = self.drr
        self.drr = (self.drr + 1) % self.nrot
        if self.dval[i] > 0:
            self._wait(q, i, self.dval[i])
        ins = self.E[q].dma_start(out=out, in_=in_)
        self.dval[i] += 16
        ins.then_inc(self.dsem[i], 16)
        self.nins += 1
        self._commit((i, self.dval[i]), r, w)

    def barrier(self):
        for e in ('pe', 'act', 'dve', 'pool', 'sp'):
            for f in ('pe', 'act', 'dve', 'pool'):
                if f != e and self.cnt[f] > 0:
                    self._wait(e, f, self.cnt[f])

    def finish(self, keys):
        for k in keys:
            if k in self.lastw:
                sk, v = self.lastw[k]
                self._wait('sp', sk, v)
        for i in range(len(self.dsem)):
            if self.dval[i] > 0:
                self._wait('sp', i, self.dval[i])


def build_program(S, L, NSEQ, flags):
    do_attn = flags.get('attn', True)
    do_rwkv = flags.get('rwkv', True)
    NT = S // 128
    NB = S // 512
    KSEL = min(256, S // 4)
    nc = bass.Bass("TRN2", target_bir_lowering=False)
    dt = nc.dram_tensor

    def din(name, shape, dtype=F32):
        return dt(name, list(shape), dtype, kind="ExternalInput").ap()
    x_d = din("x", [NSEQ, S, D])
    pos_d = din("positions", [NSEQ, S], I32)
    watt_d = din("w_att", [L, D, ATT_COLS])
    win_d = din("w_in", [L, D, NIN])
    wbr_d = din("w_branch", [L, 3, 512, D])
    wout_d = din("w_out", [L, D, D])
    fup_d = din("ffn_up", [L, D, 2 * DFF])
    fdn_d = din("ffn_down", [L, DFF, D])
    norms_d = din("norms", [2 * L + 1, D])
    ppl_d = din("pp_layer", [L, 128, 256])
    consts_d = din("consts", [128, 1024])
    rww_d = din("rw_small", [L, 128, 1536])
    y_d = dt("y", [NSEQ, S, D], F32, kind="ExternalOutput").ap()

    sc = Sched(nc)
    if flags.get('strict_deps', False):
        sc.strict = True
    al = nc.alloc_sbuf_tensor

    h = al("h_sb", [128, NT, D], F32)
    xnT = al("xnT", [128, KC, 512], BF16)
    NSLAB = 2
    slabs = [al("slab%d" % i, [128, 4096], BF16) for i in range(NSLAB)]
    slab_rr = [0]
    gmix = al("gmix", [128, D], BF16)
    gffn = al("gffn", [128, D], BF16)
    ppl = al("ppl", [128, 256], F32)
    consts = al("consts_sb", [128, 1024], F32)
    ident = al("ident", [128, 128], BF16)
    onesb = al("onesb", [128, 128], BF16)
    bdb = al("bdb", [128, 128], BF16)
    ss = al("ss", [128, 2 * NT], F32)
    rstd = al("rstd", [128, 2 * NT], F32)
    junk = al("junk", [128, D], BF16)
    xnb = [al("xnb0", [128, D], BF16)] * 2
    yaT = al("yaT", [64, 8, 512], BF16)
    ycT = al("ycT", [128, 4, 512], BF16)
    MSL8 = al("MSL8", [64, 8, 64], BF16)
    MAT = al("MAT", [64, 8, 2, 64], BF16)
    I8 = al("I8", [64, 8, 64], BF16)
    cu_halo = al("cu_halo", [128, 4, 2], BF16)
    f_halo = al("f_halo", [128, 2 * NFF, 2], BF16)
    kT = al("kT", [128, S], BF16)
    kiT = al("kiT", [64, S], BF16)
    vaug = al("vaug", [128, NT, 2, 66], BF16)
    wi = al("wi", [128, NT, 8], F32)
    rw_small = al("rw_small_sb", [128, 1536], BF16)
    Hst = al("Hst", [128, 4, 128], F32)
    zprev = al("zprev", [128, 14], BF16)
    abase = (nc.sbuf_base + 63) // 64 * 64
    ARENA = nc.sbuf_bytes_remaining - 192
    arena = al("arena", [128, (ARENA + 64) // 2], BF16)
    acur = [0]

    def aa(name, shape, dtype):
        nbytes = int(np.prod(shape[1:])) * (4 if dtype in (F32, I32) else 2)
        nbytes = (nbytes + 31) // 32 * 32
        off = abase + acur[0]
        acur[0] += nbytes
        assert acur[0] <= ARENA, (name, acur[0], ARENA)
        return nc.alloc_sbuf_tensor_at(name, list(shape), dtype, offset=off)
    acur[0] = 0
    scT = aa("scT", [128, 12, 514], BF16)
    cuT = aa("cuT", [128, 4, 514], BF16)
    ybT = aa("ybT", [128, 4, 512], BF16)
    mixT = aa("mixT", [128, KC, 512], BF16)
    gsig = [aa("gsig%d" % i, [128, 512], F32) for i in range(2)]
    macc = aa("macc", [128, 512], F32)
    mtmp = aa("mtmp", [128, 512], F32)
    acur[0] = 0
    actT = aa("actT", [128, NFF, 512], BF16)
    fg = [aa("fg%d" % i, [128, 514], BF16) for i in range(2)]
    fu = [aa("fu%d" % i, [128, 514], BF16) for i in range(2)]
    fgc = [aa("fgc%d" % i, [128, 512], F32) for i in range(1)] * 2
    fuc = [aa("fuc%d" % i, [128, 512], F32) for i in range(1)] * 2
    fsl = fgc
    acur[0] = 0
    outb = [aa("outb%d" % i, [128, D], F32) for i in range(2)]
    acur[0] = 0
    posi = aa("posi", [128, 512], I32)
    posf = aa("posf", [128, 512], F32)
    rtA = aa("rtA", [128, 512], F32)
    rtB = aa("rtB", [128, 512], F32)
    rtC = aa("rtC", [128, 512], F32)
    rtD = aa("rtD", [128, 512], F32)
    Cq = aa("Cq", [128, 512], BF16)
    Sq = aa("Sq", [128, 512], BF16)
    Ci = aa("Ci", [128, 512], BF16)
    Si = aa("Si", [128, 512], BF16)
    qT = aa("qT", [128, 4, 512], BF16)
    qiT = aa("qiT", [64, 4, 512], BF16)
    isc = aa("isc", [128, S], F32)
    rl = [aa("rl%d" % i, [128, 512], F32) for i in range(2)]
    maskb = aa("maskb", [128, S], BF16)
    maskT = aa("maskT", [128, NT, 128], BF16)
    pt = [aa("pt%d" % i, [128, 512], BF16) for i in range(2)]
    rdn = aa("rdn", [128, 512], F32)
    rdnb = aa("rdnb", [128, 512], BF16)
    accsb = aa("accsb", [64, 512], F32)
    bsm = aa("bsm", [128, 8], F32)
    acur[0] = 0
    zT = aa("zT", [128, 14, 513], BF16)

    def f4(name):
        return aa(name, [128, 4, 128], F32)
    r_ = f4("rw_r"); k_ = f4("rw_k"); v_ = f4("rw_v"); sg = f4("rw_sg"); Ls = f4("rw_Ls")
    g_ = f4("rw_g"); kkn = f4("rw_kkn"); kf_ = f4("rw_kf")
    t1 = f4("rw_t1"); t2 = f4("rw_t2"); E_ = f4("rw_E"); YT = sg
    a_ = E_
    lraw = t2
    lwla = aa("rw_lwla", [128, 128], BF16)
    lgb = aa("rw_lgb", [128, 128], BF16)
    tb = aa("rw_tb", [128, 4, 128], BF16)
    AR = aa("rw_AR", [128, 4, 2, 128], BF16)
    BT = aa("rw_BT", [128, 4, 128], BF16)
    KT_ = aa("rw_KT", [128, 4, 128], BF16)
    BH = aa("rw_BH", [128, 4, 128], BF16)
    KH = aa("rw_KH", [128, 4, 128], BF16)
    VT = aa("rw_VT", [128, 4, 128], BF16)
    wc = aa("rw_wc", [128, 4, 2], F32)
    Vpad = aa("rw_Vpad", [64, 8, 128], BF16)
    BHpad = aa("rw_BHpad", [64, 8, 128], BF16)
    KHpad = aa("rw_KHpad", [64, 8, 128], BF16)
    ARm = [aa("rw_ARm%d" % i, [128, 4, 2, 128], BF16) for i in range(2)]
    Upad = aa("rw_Upad", [64, 8, 128], BF16)
    ATb = aa("rw_ATb", [64, 8, 2, 64], BF16)
    ATk = aa("rw_ATk", [64, 8, 2, 64], BF16)
    Pm = [aa("rw_P%d" % i, [64, 8, 64], BF16) for i in range(2)]
    Qm = [aa("rw_Q%d" % i, [64, 8, 64], BF16) for i in range(2)]
    STm = [aa("rw_S%d" % i, [64, 8, 64], BF16) for i in range(2)]
    R0 = aa("rw_R0", [64, 8, 64], BF16)
    Hbf = aa("rw_Hbf", [128, 4, 128], BF16)
    print('arena bytes', ARENA, 'rwkv uses', acur[0], flush=True)

    ps = [nc.alloc_psum_tensor("ps%d" % i, [128, 512], F32) for i in range(6)]
    pst = [nc.alloc_psum_tensor("pst%d" % i, [128, 1024], BF16) for i in range(2)]

    rr = {'ps': 0, 'pst': 0, 'xnb': 0}

    ps_reserved = set()

    def next_ps():
        while True:
            i = rr['ps']
            rr['ps'] = (i + 1) % 6
            if i not in ps_reserved:
                return i

    def load_slab(parts):
        i = slab_rr[0]
        slab_rr[0] = (i + 1) % NSLAB
        key = 'slab%d' % i
        views = []
        off = 0
        for (src, p, kc, n) in parts:
            v = slabs[i][0:p, off:off + kc * n].rearrange("p (k n) -> p k n", k=kc)
            sc.dma('pool', v, src.rearrange("(k p) n -> p k n", p=p), r=(), w=(key,))
            views.append(v)
            off += kc * n
        assert off <= 4096
        return views, key

    def mm(psi, pairs, rkeys, out=None):
        o = ps[psi][:] if out is None else out

        def fn(pe):
            ins = None
            n = len(pairs)
            for j, (l_, r_) in enumerate(pairs):
                ins = pe.matmul(o, l_, r_, start=(j == 0), stop=(j == n - 1))
            return ins
        sc.op('pe', fn, r=rkeys, w=('ps%d' % psi,))

    sc.dma('sp', consts[:], consts_d, w=('consts',))
    sc.dma('pool', ident[:], consts_d[:, 0:128], w=('ident',))
    sc.dma('pool', bdb[:], consts_d[:, 519:647], w=('bdb',))
    sc.op('pool', lambda e: e.memset(onesb[:], 1.0), w=('onesb',))
    for hh in range(8):
        sc.op('dve', lambda e: e.tensor_copy(MSL8[:, hh, :], consts[0:64, 455:519]), r=('consts',), w=('MSL8',))
        sc.op('dve', lambda e: e.tensor_copy(MAT[:, hh, 0, :], consts[0:64, 327:391]), r=('consts',), w=('MAT',))
        sc.op('dve', lambda e: e.tensor_copy(MAT[:, hh, 1, :], consts[0:64, 391:455]), r=('consts',), w=('MAT',))
        sc.op('dve', lambda e: e.tensor_copy(I8[:, hh, :], consts[0:64, 775:839]), r=('consts',), w=('I8',))

    def rmsnorm_to_xnT(blk, gtile, gkey, slot):
        for tt in range(4):
            gt = blk * 4 + tt
            col = slot * NT + gt
            sc.op('act', lambda e: e.activation(junk[:], h[:, gt, :], AF.Square,
                                                accum_out=ss[:, col:col + 1]),
                  r=(('h', gt),), w=('junk', ('ss', col)))
            sc.op('act', lambda e: e.activation(rstd[:, col:col + 1], ss[:, col:col + 1], AF.Sqrt,
                                                bias=consts[:, 261:262], scale=1.0 / D),
                  r=(('ss', col), 'consts'), w=(('rstd', col),))
            sc.op('dve', lambda e: e.reciprocal(rstd[:, col:col + 1], rstd[:, col:col + 1]),
                  r=(('rstd', col),), w=(('rstd', col),))
            xi = 0
            sc.op('dve', lambda e: e.scalar_tensor_tensor(xnb[xi][:], h[:, gt, :],
                                                          rstd[:, col:col + 1], gtile[:],
                                                          ALU.mult, ALU.mult),
                  r=(('h', gt), ('rstd', col), gkey), w=('xnb%d' % xi,))
            pi = rr['pst']
            rr['pst'] = 1 - pi

            def tr(pe):
                ins = None
                for kc in range(KC):
                    ins = pe.transpose(pst[pi][:, kc * 128:(kc + 1) * 128],
                                       xnb[xi][:, kc * 128:(kc + 1) * 128], ident[:])
                return ins
            sc.op('pe', tr, r=('xnb%d' % xi, 'ident'), w=('pst%d' % pi,))
            sc.op('act', lambda e: e.copy(xnT[:, :, tt * 128:(tt + 1) * 128],
                                          pst[pi][:].rearrange("p (k t) -> p k t", k=KC)),
                  r=('pst%d' % pi,), w=('xnT',))


    def rope_tables(sq, blk):
        t0 = blk * 512
        sc.dma('sp', posi[:], pos_d[sq:sq + 1, t0:t0 + 512].broadcast_to([128, 512]), w=('posi',))
        sc.op('dve', lambda e: e.tensor_copy(posf[:], posi[:]), r=('posi',), w=('posf',))
        TWO_PI = 2 * math.pi

        def sin_of(dst, dkey, shift):
            sc.op('dve', lambda e: e.tensor_scalar(rtB[:], rtA[:], shift, 1.0 / TWO_PI, ALU.add, ALU.mult),
                  r=('rtA',), w=('rtB',))
            sc.op('dve', lambda e: e.tensor_copy(posi[:], rtB[:]), r=('rtB',), w=('posi',))
            sc.op('dve', lambda e: e.tensor_copy(rtB[:], posi[:]), r=('posi',), w=('rtB',))
            sc.op('dve', lambda e: e.scalar_tensor_tensor(rtB[:], rtB[:], -TWO_PI, rtA[:], ALU.mult, ALU.add),
                  r=('rtB', 'rtA'), w=('rtB',))
            if shift != 0.0:
                sc.op('dve', lambda e: e.tensor_scalar(rtB[:], rtB[:], shift, None, ALU.add),
                      r=('rtB',), w=('rtB',))
            sc.op('dve', lambda e: e.tensor_scalar(rtC[:], rtB[:], math.pi, -TWO_PI, ALU.is_gt, ALU.mult),
                  r=('rtB',), w=('rtC',))
            sc.op('dve', lambda e: e.tensor_tensor(rtB[:], rtB[:], rtC[:], ALU.add), r=('rtB', 'rtC'), w=('rtB',))
            sc.op('dve', lambda e: e.tensor_scalar(rtC[:], rtB[:], -math.pi, TWO_PI, ALU.is_lt, ALU.mult),
                  r=('rtB',), w=('rtC',))
            sc.op('dve', lambda e: e.tensor_tensor(rtB[:], rtB[:], rtC[:], ALU.add), r=('rtB', 'rtC'), w=('rtB',))
            sc.op('dve', lambda e: e.tensor_scalar(rtB[:], rtB[:], -3.1415925, 3.1415925, ALU.max, ALU.min),
                  r=('rtB',), w=('rtB',))
            sc.op('act', lambda e: e.activation(dst, rtB[:], AF.Sin), r=('rtB',), w=(dkey,))
        for (fc, sg, Ct, St, nm) in ((128, 129, Cq, Sq, 'q'), (130, 131, Ci, Si, 'i')):
            sc.op('dve', lambda e: e.tensor_scalar(rtA[:], posf[:], consts[:, fc:fc + 1], None, ALU.mult),
                  r=('posf', 'consts'), w=('rtA',))
            sin_of(rtD[:], 'rtD', 0.0)
            sc.op('dve', lambda e: e.tensor_scalar(St[:], rtD[:], consts[:, sg:sg + 1], None, ALU.mult),
                  r=('rtD', 'consts'), w=('S' + nm,))
            sin_of(Ct[:], 'C' + nm, 0.5 * math.pi)

    def attention_block(sq, l, blk):
        t0 = blk * 512
        if flags.get('att_stage', 9) >= 0:
            rope_tables(sq, blk)
        if flags.get('att_stage', 9) in (0, -1):
            sc.op('pool', lambda e: e.memset(yaT[:], 0.0), w=('yaT',))
            return
        nroped = [0]

        def roped(wm, ws, wk1, wk2, c0, M, Ct, St, tn, dst, dkey):
            nroped[0] += 1
            if nroped[0] > flags.get('att_sub', 99):
                return
            p1 = next_ps()
            mm(p1, [(wm[:, kc, c0:c0 + M], xnT[:, kc, :]) for kc in range(KC)], rkeys=(wk1, 'xnT'),
               out=ps[p1][0:M, :])
            p2 = next_ps()
            mm(p2, [(ws[:, kc, c0:c0 + M], xnT[:, kc, :]) for kc in range(KC)], rkeys=(wk2, 'xnT'),
               out=ps[p2][0:M, :])
            sc.op('dve', lambda e: e.tensor_tensor(rtA[0:M, :], ps[p1][0:M, :], Ct[0:M, :], ALU.mult),
                  r=('ps%d' % p1, 'C' + tn), w=('rtA',))
            sc.op('dve', lambda e: e.tensor_tensor(rtB[0:M, :], ps[p2][0:M, :], St[0:M, :], ALU.mult),
                  r=('ps%d' % p2, 'S' + tn), w=('rtB',))
            sc.op('pool', lambda e: e.tensor_tensor(dst, rtA[0:M, :], rtB[0:M, :], ALU.add),
                  r=('rtA', 'rtB'), w=(dkey,))
        (wm,), k1 = load_slab([(watt_d[l][:, 0:512], 128, KC, 512)])
        (ws,), k2 = load_slab([(watt_d[l][:, ATT_MAIN:ATT_MAIN + 512], 128, KC, 512)])
        for m in range(4):
            roped(wm, ws, k1, k2, m * 128, 128, Cq, Sq, 'q', qT[:, m, :], 'qT')
        (wm,), k1 = load_slab([(watt_d[l][:, 512:960], 128, KC, 448)])
        (ws,), k2 = load_slab([(watt_d[l][:, ATT_MAIN + 512:ATT_MAIN + 960], 128, KC, 448)])
        roped(wm, ws, k1, k2, 0, 128, Cq, Sq, 'q', kT[:, t0:t0 + 512], 'kT')
        for c in range(4):
            roped(wm, ws, k1, k2, 128 + c * 64, 64, Ci, Si, 'i', qiT[:, c, :], 'qiT')
        roped(wm, ws, k1, k2, 384, 64, Ci, Si, 'i', kiT[:, t0:t0 + 512], 'kiT')
        (wv,), k3 = load_slab([(watt_d[l][:, 2 * ATT_MAIN:2 * ATT_MAIN + 136], 128, KC, 136)])
        for tt in range(4 if flags.get('att_sub', 99) >= 20 else 0):
            gt = blk * 4 + tt
            p1 = next_ps()
            mm(p1, [(xnT[:, kc, tt * 128:(tt + 1) * 128], wv[:, kc, :]) for kc in range(KC)],
               rkeys=(k3, 'xnT'), out=ps[p1][:, 0:136])
            if flags.get('vw_var', 9) >= 2:
                sc.op('act', lambda e: e.copy(vaug[:, gt, :, 0:64],
                                              ps[p1][:, 0:128].rearrange("p (n d) -> p n d", n=2)),
                      r=('ps%d' % p1,), w=('vaug',))
            if flags.get('vw_var', 9) >= 3:
                sc.op('act', lambda e: e.copy(wi[:, gt, :], ps[p1][:, 128:136]),
                      r=('ps%d' % p1,), w=('wi',))
        if flags.get('att_stage', 9) < 2:
            sc.op('pool', lambda e: e.memset(yaT[:], 0.0), w=('yaT',))
            return
        MX, LO, RNG, MID, CNT, GEH = range(6)
        col = lambda j: bsm[:, j:j + 1]
        for tt in range(4):
            gt = blk * 4 + tt
            nk = (gt + 1) * 128
            qs = slice(tt * 128, (tt + 1) * 128)
            for k0 in range(0, nk, 512):
                n = min(512, nk - k0)
                for hh in range(8):
                    c, bp = hh // 2, 32 * (hh % 2)
                    p1 = next_ps()
                    mm(p1, [(qiT[bp:bp + 32, c, qs], kiT[bp:bp + 32, k0:k0 + n])], rkeys=('qiT', 'kiT'),
                       out=ps[p1][:, 0:n])
                    ri = hh % 2
                    sc.op('act', lambda e: e.activation(rl[ri][:, 0:n], ps[p1][:, 0:n], AF.Relu),
                          r=('ps%d' % p1,), w=('rl%d' % ri,))
                    if hh == 0:
                        sc.op('dve', lambda e: e.tensor_scalar(isc[:, k0:k0 + n], rl[ri][:, 0:n],
                                                               wi[:, gt, 0:1], None, ALU.mult),
                              r=('rl%d' % ri, 'wi'), w=('isc',))
                    else:
                        sc.op('dve', lambda e: e.scalar_tensor_tensor(isc[:, k0:k0 + n], rl[ri][:, 0:n],
                                                                      wi[:, gt, hh:hh + 1], isc[:, k0:k0 + n],
                                                                      ALU.mult, ALU.add),
                              r=('rl%d' % ri, 'wi', 'isc'), w=('isc',))
            if nk > KSEL:
                sc.op('dve', lambda e: e.tensor_reduce(col(MX), isc[:, 0:nk], AX.X, ALU.max,
                                                       apply_absolute_value=True),
                      r=('isc',), w=('bs_mx',))
            sc.op('dve', lambda e: e.tensor_tensor(isc[:, gt * 128:(gt + 1) * 128],
                                                   isc[:, gt * 128:(gt + 1) * 128],
                                                   consts[:, 133:261], ALU.add),
                  r=('isc', 'consts'), w=('isc',))
            if nk <= KSEL:
                sc.op('dve', lambda e: e.memset(col(LO), -1.0e29), w=('bs_lo',))
            else:
                sc.op('dve', lambda e: e.tensor_scalar(col(LO), col(MX), -1.0, -1.0, ALU.mult, ALU.add),
                      r=('bs_mx',), w=('bs_lo',))
                sc.op('dve', lambda e: e.tensor_scalar(col(RNG), col(MX), 2.0, 2.0, ALU.mult, ALU.add),
                      r=('bs_mx',), w=('bs_rng',))
                for it in range(BIS_ITERS):
                    ck = 0.5 ** (it + 1)
                    sc.op('dve', lambda e: e.scalar_tensor_tensor(col(MID), col(RNG), ck, col(LO),
                                                                  ALU.mult, ALU.add),
                          r=('bs_rng', 'bs_lo'), w=('bs_mid',))
                    sc.op('dve', lambda e: e.tensor_scalar(maskb[:, 0:nk], isc[:, 0:nk], col(MID), None,
                                                           ALU.is_ge, ALU.add, accum_out=col(CNT)),
                          r=('isc', 'bs_mid'), w=('maskb', 'bs_cnt'))
                    sc.op('dve', lambda e: e.tensor_scalar(col(GEH), col(CNT), KSEL - 0.5, ck,
                                                           ALU.is_ge, ALU.mult),
                          r=('bs_cnt',), w=('bs_geh',))
                    sc.op('dve', lambda e: e.scalar_tensor_tensor(col(LO), col(GEH), col(RNG), col(LO),
                                                                  ALU.mult, ALU.add),
                          r=('bs_geh', 'bs_rng', 'bs_lo'), w=('bs_lo',))
            sc.op('dve', lambda e: e.tensor_scalar(maskb[:, 0:nk], isc[:, 0:nk], col(LO), None, ALU.is_ge),
                  r=('isc', 'bs_lo'), w=('maskb',))
            for i0 in range(0, gt + 1, 8):
                nb = min(8, gt + 1 - i0)
                pi = rr['pst']
                rr['pst'] = 1 - pi

                def tr(pe, i0=i0, nb=nb, pi=pi):
                    ins = None
                    for j in range(nb):
                        ins = pe.transpose(pst[pi][:, j * 128:(j + 1) * 128],
                                           maskb[:, (i0 + j) * 128:(i0 + j + 1) * 128], ident[:])
                    return ins
                sc.op('pe', tr, r=('maskb', 'ident'), w=('pst%d' % pi,))
                sc.op('act', lambda e: e.copy(maskT[:, i0:i0 + nb, :],
                                              pst[pi][:, 0:nb * 128].rearrange("p (k t) -> p k t", k=nb)),
                      r=('pst%d' % pi,), w=('maskT',))
            if flags.get('att_stage', 9) < 3:
                sc.op('pool', lambda e: e.memset(yaT[:], 0.0), w=('yaT',))
                continue
            A3 = flags.get('a3', 9)
            for n in range(2):
                bp = 64 * n
                pacc = next_ps()
                ps_reserved.add(pacc)
                for i in range(gt + 1):
                    p1 = next_ps()
                    sc.op('pe', lambda pe: pe.matmul(ps[p1][:].rearrange("p (a b) -> p a b", a=4),
                                                     kT[bp:bp + 64, i * 128:(i + 1) * 128],
                                                     qT[bp:bp + 64, :, qs], start=True, stop=True),
                          r=('kT', 'qT'), w=('ps%d' % p1,))
                    pj = i % 2
                    sc.op('act', lambda e: e.activation(pt[pj][:], ps[p1][:], AF.Exp, scale=0.125),
                          r=('ps%d' % p1,), w=('pt%d' % pj,))
                    if A3 >= 2:
                        for a in range(4):
                            sc.op('pool', lambda e: e.tensor_tensor(
                                pt[pj][:, a * 128:(a + 1) * 128], pt[pj][:, a * 128:(a + 1) * 128],
                                maskT[:, i, :], ALU.mult),
                                r=('pt%d' % pj, 'maskT'), w=('pt%d' % pj,))
                    if A3 >= 3:
                        sc.op('pe', lambda pe: pe.matmul(ps[pacc][0:65, :], vaug[:, i, n, 0:65], pt[pj][:],
                                                         start=(i == 0), stop=(i == gt)),
                              r=('vaug', 'pt%d' % pj), w=('ps%d' % pacc,))
                if A3 >= 4:
                    sc.op('dve', lambda e: e.reciprocal(rdn[64:65, :], ps[pacc][64:65, :]),
                          r=('ps%d' % pacc,), w=('rdn',))
                    sc.op('dve', lambda e: e.tensor_copy(rdnb[64:65, :], rdn[64:65, :]),
                          r=('rdn',), w=('rdnb',))
                pb = next_ps()
                if A3 >= 5:
                    sc.op('pe', lambda pe: pe.matmul(ps[pb][0:64, :], onesb[64:65, 0:64], rdnb[64:65, :],
                                                     start=True, stop=True),
                          r=('rdnb', 'onesb'), w=('ps%d' % pb,))
                if A3 >= 6:
                    sc.op('act', lambda e: e.copy(accsb[:], ps[pacc][0:64, :]), r=('ps%d' % pacc,), w=('accsb',))
                ps_reserved.discard(pacc)
                if A3 >= 7:
                    sc.op('dve', lambda e: e.tensor_tensor(
                        yaT[:, n * 4:(n + 1) * 4, qs],
                        accsb[:].rearrange("p (a b) -> p a b", a=4),
                        ps[pb][0:64, :].rearrange("p (a b) -> p a b", a=4), ALU.mult),
                        r=('accsb', 'ps%d' % pb), w=('yaT',))
                else:
                    sc.op('pool', lambda e: e.memset(yaT[:], 0.0), w=('yaT',))


    class _Stop(Exception):
        pass

    def chk(n):
        if flags.get('rw_stage', 99) < n:
            raise _Stop()

    def rwkv_block(sq, l, blk):
        try:
            rwkv_block_(sq, l, blk)
        except _Stop:
            sc.op('pool', lambda e: e.memset(ycT[:], 0.0), w=('ycT',))

    stg_rr = [0]

    def evac2(dst_flat, psrc, pkey_, in1_flat, in1key, op, dkey):
        i = stg_rr[0]
        stg_rr[0] = 1 - i
        st_t = (t1, t2)[i]
        skey = ('t1', 't2')[i]
        stf = st_t[0:64, :, :].rearrange("p a t -> p (a t)")
        sc.op('act', lambda e: e.copy(stf, psrc), r=(pkey_,), w=(skey,))
        sc.op('dve', lambda e: e.tensor_tensor(dst_flat, stf, in1_flat, op), r=(skey, in1key), w=(dkey,))

    def rwkv_block_(sq, l, blk):
        PP = lambda c: ppl[:, c:c + 1]
        sc.op('pool', lambda e: e.tensor_copy(Hbf[:], Hst[:]), r=('Hst',), w=('Hbf',))
        for (tz, kz) in ((Vpad, 'Vpad'), (BHpad, 'BHpad'), (KHpad, 'KHpad'), (Upad, 'Upad')):
            sc.op('pool', lambda e: e.memset(tz[:], 0.0), w=(kz,))
        for s4 in range(4):
            n = 512 if s4 < 3 else 256
            (wv,), wk = load_slab([(win_d[l][:, OFF_RW + s4 * 512:OFF_RW + s4 * 512 + n], 128, KC, n)])
            for m in range(n // 128):
                c = s4 * 4 + m
                pi = next_ps()
                mm(pi, [(wv[:, kc, m * 128:(m + 1) * 128], xnT[:, kc, :]) for kc in range(KC)], rkeys=(wk, 'xnT'))
                sc.op('act', lambda e: e.copy(zT[:, c, 1:513], ps[pi][:]), r=('ps%d' % pi,), w=('zT',))
        sc.op('pool', lambda e: e.tensor_copy(zT[:, :, 0], zprev[:, :]), r=('zprev',), w=('zT',))
        sc.op('pool', lambda e: e.tensor_copy(zprev[:, :], zT[:, :, 512]), r=('zT',), w=('zprev',))
        chk(1)
        for su in range(4):
            s0 = su * 128
            zc = lambda c: zT[:, c, s0 + 1:s0 + 129]
            zp = lambda c: zT[:, c, s0:s0 + 128]

            def shift(dst, dkey, c):
                sc.op('dve', lambda e: e.tensor_tensor(E_[:, 0, :], zp(c), zc(c), ALU.subtract),
                      r=('zT',), w=('E_',))
                sc.op('dve', lambda e: e.scalar_tensor_tensor(dst, E_[:, 0, :], PP(168 + c), zc(c),
                                                              ALU.mult, ALU.add),
                      r=('E_', 'zT', 'ppl'), w=(dkey,))
            for p in range(4):
                shift(r_[:, p, :], 'r_', p)
                shift(k_[:, p, :], 'k_', 4 + p)
                shift(v_[:, p, :], 'v_', 8 + p)
            shift(lraw[:, 0, :], 't2', 12)
            shift(lraw[:, 1, :], 't2', 13)
            sc.op('act', lambda e: e.activation(lwla[0:64, :], lraw[0:64, 0, :], AF.Tanh), r=('t2',), w=('lwla',))
            sc.op('act', lambda e: e.copy(lwla[64:128, :], lraw[64:128, 0, :]), r=('t2',), w=('lwla',))
            sc.op('act', lambda e: e.activation(lgb[:], lraw[:, 1, :], AF.Sigmoid), r=('t2',), w=('lgb',))
            pzw = next_ps()

            def f_zw(pe):
                ins = None
                for p in range(4):
                    ins = pe.matmul(ps[pzw][:, p * 128:(p + 1) * 128], rw_small[0:64, p * 128:(p + 1) * 128],
                                    lwla[0:64, :], start=True, stop=True)
                return ins
            sc.op('pe', f_zw, r=('rw_small', 'lwla'), w=('ps%d' % pzw,))
            for p in range(4):
                sc.op('act', lambda e: e.activation(sg[:, p, :], ps[pzw][:, p * 128:(p + 1) * 128], AF.Sigmoid,
                                                    bias=PP(182 + p), scale=1.0),
                      r=('ps%d' % pzw, 'ppl'), w=('sg',))
            pza = next_ps()

            def f_za(pe):
                ins = None
                for p in range(4):
                    ins = pe.matmul(ps[pza][:, p * 128:(p + 1) * 128],
                                    rw_small[64:128, 512 + p * 128:512 + (p + 1) * 128],
                                    lwla[64:128, :], start=True, stop=True)
                return ins
            sc.op('pe', f_za, r=('rw_small', 'lwla'), w=('ps%d' % pza,))
            for p in range(4):
                sc.op('act', lambda e: e.activation(a_[:, p, :], ps[pza][:, p * 128:(p + 1) * 128], AF.Sigmoid,
                                                    bias=PP(186 + p), scale=1.0),
                      r=('ps%d' % pza, 'ppl'), w=('E_',))
            pg = next_ps()

            def f_g(pe):
                ins = None
                for p in range(4):
                    ins = pe.matmul(ps[pg][:, p * 128:(p + 1) * 128],
                                    rw_small[:, 1024 + p * 128:1024 + (p + 1) * 128],
                                    lgb[:, :], start=True, stop=True)
                return ins
            sc.op('pe', f_g, r=('rw_small', 'lgb'), w=('ps%d' % pg,))
            sc.op('act', lambda e: e.copy(g_[:].rearrange("p a t -> p (a t)"), ps[pg][:]),
                  r=('ps%d' % pg,), w=('g_',))
            chk(2)
            for p in range(4):
                sc.op('dve', lambda e: e.tensor_scalar(kkn[:, p, :], k_[:, p, :], PP(190 + p), None, ALU.mult),
                      r=('k_', 'ppl'), w=('kkn',))
            sc.op('dve', lambda e: e.tensor_tensor(tb[:], kkn[:], kkn[:], ALU.mult), r=('kkn',), w=('tb',))
            pn = next_ps()
            sc.op('pe', lambda pe: pe.matmul(ps[pn][:], bdb[:], tb[:].rearrange("p a t -> p (a t)"),
                                             start=True, stop=True),
                  r=('bdb', 'tb'), w=('ps%d' % pn,))
            sc.op('act', lambda e: e.activation(t2[:].rearrange("p a t -> p (a t)"), ps[pn][:], AF.Sqrt),
                  r=('ps%d' % pn,), w=('t2',))
            sc.op('dve', lambda e: e.tensor_scalar(t2[:], t2[:], 1.0e-12, None, ALU.max), r=('t2',), w=('t2',))
            sc.op('dve', lambda e: e.reciprocal(t2[:], t2[:]), r=('t2',), w=('t2',))
            sc.op('dve', lambda e: e.tensor_tensor(kkn[:], kkn[:], t2[:], ALU.mult), r=('kkn', 't2'), w=('kkn',))
            chk(3)
            for p in range(4):
                sc.op('dve', lambda e: e.tensor_scalar(t1[:, p, :], a_[:, p, :], -1.0, PP(194 + p),
                                                       ALU.add, ALU.mult),
                      r=('E_', 'ppl'), w=('t1',))
            sc.op('dve', lambda e: e.scalar_tensor_tensor(kf_[:], t1[:], 1.0, k_[:], ALU.add, ALU.mult),
                  r=('t1', 'k_'), w=('kf_',))
            sc.op('dve', lambda e: e.tensor_tensor(t1[:], kkn[:], a_[:], ALU.mult), r=('kkn', 'E_'), w=('t1',))
            for p in range(4):
                sc.op('dve', lambda e: e.tensor_tensor_scan(Ls[:, p, :], consts[:, 647:775], sg[:, p, :], 0.0,
                                                            ALU.mult, ALU.add),
                      r=('consts', 'sg'), w=('Ls',))
            sc.op('dve', lambda e: e.tensor_tensor(t2[:], Ls[:], sg[:], ALU.subtract), r=('Ls', 'sg'), w=('t2',))
            sc.op('act', lambda e: e.activation(E_[:], t2[:], AF.Exp, scale=-C0), r=('t2',), w=('E_',))
            sc.op('dve', lambda e: e.scalar_tensor_tensor(AR[:, :, 0, :], kkn[:], -1.0, E_[:], ALU.mult, ALU.mult),
                  r=('kkn', 'E_'), w=('AR',))
            sc.op('act', lambda e: e.activation(E_[:], Ls[:], AF.Exp, scale=-C0), r=('Ls',), w=('E_',))
            sc.op('dve', lambda e: e.tensor_tensor(AR[:, :, 1, :], r_[:], E_[:], ALU.mult), r=('r_', 'E_'), w=('AR',))
            for j in range(2):
                sc.op('dve', lambda e: e.tensor_scalar(ARm[j][:].rearrange("p a b t -> p (a b t)"),
                                                       AR[:].rearrange("p a b t -> p (a b t)"),
                                                       consts[:, 839 + j:840 + j], None, ALU.mult),
                      r=('AR', 'consts'), w=('ARm%d' % j,))
            sc.op('act', lambda e: e.activation(E_[:], Ls[:], AF.Exp, scale=C0), r=('Ls',), w=('E_',))
            sc.op('dve', lambda e: e.tensor_tensor(BT[:], t1[:], E_[:], ALU.mult), r=('t1', 'E_'), w=('BT',))
            sc.op('dve', lambda e: e.tensor_tensor(KT_[:], kf_[:], E_[:], ALU.mult), r=('kf_', 'E_'), w=('KT_',))
            for p in range(4):
                for c in range(2):
                    cs = slice(c * 64, (c + 1) * 64)
                    sc.op('dve', lambda e: e.tensor_scalar(t2[:, p, cs], Ls[:, p, cs],
                                                           Ls[:, p, c * 64 + 63:c * 64 + 64], None, ALU.subtract),
                          r=('Ls',), w=('t2',))
            sc.op('act', lambda e: e.activation(E_[:], t2[:], AF.Exp, scale=C0), r=('t2',), w=('E_',))
            sc.op('dve', lambda e: e.tensor_tensor(BH[:], t1[:], E_[:], ALU.mult), r=('t1', 'E_'), w=('BH',))
            sc.op('dve', lambda e: e.tensor_tensor(KH[:], kf_[:], E_[:], ALU.mult), r=('kf_', 'E_'), w=('KH',))
            sc.op('act', lambda e: e.activation(wc[:], Ls[:].rearrange("p a (c t) -> p a c t", c=2)[:, :, :, 63],
                                                AF.Exp, scale=-C0),
                  r=('Ls',), w=('wc',))
            chk(4)
            sc.op('dve', lambda e: e.tensor_tensor(t2[:], r_[:], kf_[:], ALU.mult), r=('r_', 'kf_'), w=('t2',))
            for p in range(4):
                sc.op('dve', lambda e: e.tensor_scalar(tb[:, p, :], t2[:, p, :], PP(198 + p), None, ALU.mult),
                      r=('t2', 'ppl'), w=('tb',))
            chk(4.1)
            pbn = next_ps()
            sc.op('pe', lambda pe: pe.matmul(ps[pbn][:], bdb[:], tb[:].rearrange("p a t -> p (a t)"),
                                             start=True, stop=True),
                  r=('bdb', 'tb'), w=('ps%d' % pbn,))
            chk(4.2)
            sc.op('dve', lambda e: e.tensor_tensor(E_[:].rearrange("p a t -> p (a t)"), ps[pbn][:],
                                                   v_[:].rearrange("p a t -> p (a t)"), ALU.mult),
                  r=('ps%d' % pbn, 'v_'), w=('E_',))
            chk(4.3)
            sc.op('act', lambda e: e.copy(VT[:], v_[:]), r=('v_',), w=('VT',))
            chk(4.4)
            chk(5)
            for c in range(2):
                cs = slice(c * 64, (c + 1) * 64)
                hp = lambda hh: (hh // 2, 64 * (hh % 2))
                for (src, skey, dst, dkey) in ((VT, 'VT', Vpad, 'Vpad'), (BH, 'BH', BHpad, 'BHpad'),
                                               (KH, 'KH', KHpad, 'KHpad')):
                    pi = next_ps()

                    def trf(pe, src=src, pi=pi, c=c):
                        ins = None
                        for p in range(4):
                            ins = pe.matmul(ps[pi][0:64, p * 128:(p + 1) * 128],
                                            src[:, p, c * 64:(c + 1) * 64], ident[:], start=True, stop=True)
                        return ins
                    sc.op('pe', trf, r=(skey, 'ident'), w=('ps%d' % pi,))
                    for j in range(2):
                        sc.op('dve', lambda e: e.tensor_copy(
                            dst[:, j::2, j * 64:(j + 1) * 64],
                            ps[pi][0:64, :].rearrange("p (a b) -> p a b", a=4)[:, :, j * 64:(j + 1) * 64]),
                            r=('ps%d' % pi,), w=(dkey,))
                pX = next_ps()

                def f_x(pe):
                    ins = None
                    for hh in range(0, 8, 2 if flags.get('rwx', 0) == 5 else 1):
                        p, bp = hp(hh)
                        ins = pe.matmul(ps[pX][0:64, hh * 64:(hh + 1) * 64], ARm[hh % 2][:, p, 0, cs],
                                        BT[:, p, cs], start=True, stop=True)
                    return ins
                RWX = flags.get('rwx', 0)
                if RWX != 1:
                    sc.op('pe', f_x, r=('ARm0', 'ARm1', 'BT'), w=('ps%d' % pX,))
                if RWX not in (1, 3):
                    evac2(Pm[0][:].rearrange("p h t -> p (h t)"), ps[pX][0:64, :], 'ps%d' % pX,
                          MSL8[:].rearrange("p h t -> p (h t)"), 'MSL8', ALU.mult, 'Pm0')
                for (lh, lkey, dstA, dkey) in ((BT, 'BT', ATb, 'ATb'), (KT_, 'KT_', ATk, 'ATk')):
                    for half in range(2):
                        pY = next_ps()

                        def f_y(pe, lh=lh, half=half, pY=pY):
                            ins = None
                            for q4 in range(4):
                                hh = half * 4 + q4
                                p, bp = hp(hh)
                                ins = pe.matmul(ps[pY][0:64, q4 * 128:(q4 + 1) * 128].rearrange(
                                    "p (a b) -> p a b", a=2), lh[:, p, cs], ARm[hh % 2][:, p, :, cs],
                                    start=True, stop=True)
                            return ins
                        if RWX in (2, 5):
                            continue
                        sc.op('pe', f_y, r=(lkey, 'ARm0', 'ARm1'), w=('ps%d' % pY,))
                        if RWX == 4:
                            continue
                        evac2(dstA[:, half * 4:(half + 1) * 4, :, :].rearrange("p h a t -> p (h a t)"),
                              ps[pY][0:64, :], 'ps%d' % pY,
                              MAT[:, half * 4:(half + 1) * 4, :, :].rearrange("p h a t -> p (h a t)"), 'MAT',
                              ALU.mult, dkey)
                chk(6)
                sc.op('dve', lambda e: e.tensor_tensor(STm[0][:], ATb[:, :, 0, :], I8[:], ALU.add),
                      r=('ATb', 'I8'), w=('STm0',))
                Pc = lambda hh: Pm[0][:, hh, :]
                Qc = lambda hh: ATb[:, hh, 0, :]
                pkey, qkey = 'Pm0', 'ATb'
                si = 0
                pq = 0
                for rd in range(1, 7):
                    do_sq = rd <= 5
                    do_s = rd >= 2
                    if do_sq:
                        pP = next_ps()
                        pQ = next_ps()

                        def f_p(pe, Pc=Pc, Qc=Qc, pP=pP):
                            ins = None
                            for hh in range(8):
                                ins = pe.matmul(ps[pP][0:64, hh * 64:(hh + 1) * 64], Qc(hh), Pc(hh),
                                                start=True, stop=True)
                            return ins

                        def f_q(pe, Pc=Pc, Qc=Qc, pQ=pQ):
                            ins = None
                            for hh in range(8):
                                ins = pe.matmul(ps[pQ][0:64, hh * 64:(hh + 1) * 64], Pc(hh), Qc(hh),
                                                start=True, stop=True)
                            return ins
                        sc.op('pe', f_p, r=(pkey, qkey), w=('ps%d' % pP,))
                        sc.op('pe', f_q, r=(pkey, qkey), w=('ps%d' % pQ,))
                    if do_s:
                        pS = next_ps()

                        def f_s(pe, Pc=Pc, si=si, pS=pS):
                            ins = None
                            for hh in range(8):
                                ins = pe.matmul(ps[pS][0:64, hh * 64:(hh + 1) * 64], Pc(hh), STm[si][:, hh, :],
                                                start=True, stop=True)
                            return ins
                        sc.op('pe', f_s, r=(pkey, 'STm%d' % si), w=('ps%d' % pS,))
                        evac2(STm[1 - si][:].rearrange("p h t -> p (h t)"), ps[pS][0:64, :], 'ps%d' % pS,
                              STm[si][:].rearrange("p h t -> p (h t)"), 'STm%d' % si, ALU.add,
                              'STm%d' % (1 - si))
                        si = 1 - si
                    if do_sq:
                        nx = 1 - pq if rd > 1 else 1
                        sc.op('dve', lambda e: e.tensor_copy(Pm[nx][:].rearrange("p h t -> p (h t)"), ps[pP][0:64, :]),
                              r=('ps%d' % pP,), w=('Pm%d' % nx,))
                        sc.op('dve', lambda e: e.tensor_copy(Qm[nx][:].rearrange("p h t -> p (h t)"), ps[pQ][0:64, :]),
                              r=('ps%d' % pQ,), w=('Qm%d' % nx,))
                        Pc = lambda hh, nx=nx: Pm[nx][:, hh, :]
                        Qc = lambda hh, nx=nx: Qm[nx][:, hh, :]
                        pkey, qkey = 'Pm%d' % nx, 'Qm%d' % nx
                        pq = nx
                chk(7)
                pR = next_ps()

                def f_r(pe):
                    ins = None
                    for hh in range(8):
                        p, bp = hp(hh)
                        o = ps[pR][0:64, hh * 64:(hh + 1) * 64]
                        j = hh % 2
                        pe.matmul(o, AR[:, p, 0, cs], Hbf[:, p, j * 64:(j + 1) * 64], start=True, stop=False)
                        ins = pe.matmul(o, ATk[:, hh, 0, :], Vpad[:, hh, j * 64:(j + 1) * 64],
                                        start=False, stop=True)
                    return ins
                sc.op('pe', f_r, r=('AR', 'Hbf', 'ATk', 'Vpad'), w=('ps%d' % pR,))
                sc.op('dve', lambda e: e.tensor_copy(R0[:].rearrange("p h t -> p (h t)"), ps[pR][0:64, :]),
                      r=('ps%d' % pR,), w=('R0',))
                pU = next_ps()

                def f_u(pe):
                    ins = None
                    for hh in range(8):
                        ins = pe.matmul(ps[pU][0:64, hh * 64:(hh + 1) * 64], STm[si][:, hh, :], R0[:, hh, :],
                                        start=True, stop=True)
                    return ins
                sc.op('pe', f_u, r=('STm%d' % si, 'R0'), w=('ps%d' % pU,))
                for j in range(2):
                    sc.op('dve', lambda e: e.tensor_copy(
                        Upad[:, j::2, j * 64:(j + 1) * 64],
                        ps[pU][0:64, :].rearrange("p (a b t) -> p a b t", a=4, b=2)[:, :, j, :]),
                        r=('ps%d' % pU,), w=('Upad',))
                chk(8)
                pYo = next_ps()

                def f_yo(pe):
                    ins = None
                    for p in range(4):
                        o = ps[pYo][:, p * 64:(p + 1) * 64]
                        pe.matmul(o, Hbf[:, p, :], AR[:, p, 1, cs], start=True, stop=False)
                        for j in range(2):
                            hh = 2 * p + j
                            pe.matmul(o, Upad[:, hh, :], ATb[:, hh, 1, :], start=False, stop=False)
                            ins = pe.matmul(o, Vpad[:, hh, :], ATk[:, hh, 1, :], start=False, stop=(j == 1))
                    return ins
                sc.op('pe', f_yo, r=('Hbf', 'AR', 'Upad', 'ATb', 'Vpad', 'ATk'), w=('ps%d' % pYo,))
                sc.op('act', lambda e: e.copy(YT[:, :, cs], ps[pYo][:, 0:256].rearrange("p (a t) -> p a t", a=4)),
                      r=('ps%d' % pYo,), w=('sg',))
                pH = next_ps()

                def f_h(pe):
                    ins = None
                    for hh in range(8):
                        p, j = hh // 2, hh % 2
                        o = ps[pH][:, p * 128 + j * 64:p * 128 + (j + 1) * 64]
                        pe.matmul(o, BHpad[:, hh, :], Upad[:, hh, j * 64:(j + 1) * 64], start=True, stop=False)
                        ins = pe.matmul(o, KHpad[:, hh, :], Vpad[:, hh, j * 64:(j + 1) * 64],
                                        start=False, stop=True)
                    return ins
                sc.op('pe', f_h, r=('BHpad', 'Upad', 'KHpad', 'Vpad'), w=('ps%d' % pH,))
                for p in range(4):
                    sc.op('dve', lambda e: e.scalar_tensor_tensor(Hst[:, p, :], Hst[:, p, :], wc[:, p, c:c + 1],
                                                                  ps[pH][:, p * 128:(p + 1) * 128],
                                                                  ALU.mult, ALU.add),
                          r=('Hst', 'wc', 'ps%d' % pH), w=('Hst',))
                sc.op('pool', lambda e: e.tensor_copy(Hbf[:], Hst[:]), r=('Hst',), w=('Hbf',))
            chk(9)
            F = lambda t: t[:].rearrange("p a t -> p (a t)")
            sc.op('act', lambda e: e.copy(tb[:], YT[:]), r=('sg',), w=('tb',))
            p1 = next_ps()
            sc.op('pe', lambda pe: pe.matmul(ps[p1][:], bdb[:], F(tb), start=True, stop=True),
                  r=('bdb', 'tb'), w=('ps%d' % p1,))
            sc.op('dve', lambda e: e.scalar_tensor_tensor(F(t1), ps[p1][:], -1.0 / 64, F(YT), ALU.mult, ALU.add),
                  r=('ps%d' % p1, 'sg'), w=('t1',))
            sc.op('dve', lambda e: e.tensor_tensor(tb[:], t1[:], t1[:], ALU.mult), r=('t1',), w=('tb',))
            p2 = next_ps()
            sc.op('pe', lambda pe: pe.matmul(ps[p2][:], bdb[:], F(tb), start=True, stop=True),
                  r=('bdb', 'tb'), w=('ps%d' % p2,))
            sc.op('act', lambda e: e.activation(F(t2), ps[p2][:], AF.Sqrt, bias=consts[:, 262:263], scale=1.0 / 64),
                  r=('ps%d' % p2, 'consts'), w=('t2',))
            sc.op('dve', lambda e: e.reciprocal(t2[:], t2[:]), r=('t2',), w=('t2',))
            sc.op('dve', lambda e: e.tensor_tensor(t1[:], t1[:], t2[:], ALU.mult), r=('t1', 't2'), w=('t1',))
            for p in range(4):
                sc.op('dve', lambda e: e.tensor_scalar(t1[:, p, :], t1[:, p, :], PP(202 + p), PP(206 + p),
                                                       ALU.mult, ALU.add),
                      r=('t1', 'ppl'), w=('t1',))
            sc.op('dve', lambda e: e.tensor_tensor(t1[:], t1[:], E_[:], ALU.add), r=('t1', 'E_'), w=('t1',))
            sc.op('dve', lambda e: e.tensor_tensor(ycT[:, :, s0:s0 + 128], t1[:], g_[:], ALU.mult),
                  r=('t1', 'g_'), w=('ycT',))

    for sq in range(NSEQ):
        for gt in range(NT):
            sc.dma('sp', h[:, gt, :], x_d[sq, gt * 128:(gt + 1) * 128, :], w=(('h', gt),))
        sc.op('pool', lambda e: e.memset(vaug[:], 1.0), w=('vaug',))
        for l in range(L):
            sc.dma('pool', gmix[:], norms_d[2 * l:2 * l + 1, :].broadcast_to([128, D]), w=('gmix',))
            sc.dma('pool', gffn[:], norms_d[2 * l + 1:2 * l + 2, :].broadcast_to([128, D]), w=('gffn',))
            sc.dma('sp', ppl[:], ppl_d[l], w=('ppl',))
            sc.dma('pool', rw_small[:], rww_d[l], w=('rw_small',))
            sc.op('pool', lambda e: e.memset(Hst[:], 0.0), w=('Hst',))
            sc.op('pool', lambda e: e.memset(zprev[:], 0.0), w=('zprev',))
            sc.op('pool', lambda e: e.memset(cu_halo[:], 0.0), w=('cu_halo',))
            sc.op('pool', lambda e: e.memset(f_halo[:], 0.0), w=('f_halo',))
            for blk in range(NB):
                t0 = blk * 512
                rmsnorm_to_xnT(blk, gmix, 'gmix', 0)
                if do_attn:
                    sc.barrier()
                    attention_block(sq, l, blk)
                if do_rwkv:
                    sc.barrier()
                    rwkv_block(sq, l, blk)
                sc.barrier()
                for grp in range(3):
                    (wv,), wk = load_slab([(win_d[l][:, OFF_SC + grp * 512:OFF_SC + (grp + 1) * 512],
                                            128, KC, 512)])
                    for m in range(4):
                        pi = next_ps()
                        mm(pi, [(wv[:, kc, m * 128:(m + 1) * 128], xnT[:, kc, :]) for kc in range(KC)],
                           rkeys=(wk, 'xnT'))
                        ch = grp * 4 + m
                        sc.op('act', lambda e: e.copy(scT[:, ch, 2:514], ps[pi][:]),
                              r=('ps%d' % pi,), w=(('scT', ch),))
                for m in range(4):
                    sc.op('pool', lambda e: e.tensor_copy(cuT[:, m, 0:2], cu_halo[:, m, :]),
                          r=('cu_halo',), w=(('cuT', m),))
                    sc.op('dve', lambda e: e.tensor_tensor(cuT[:, m, 2:514], scT[:, 8 + m, 2:514],
                                                           scT[:, m, 2:514], ALU.mult),
                          r=(('scT', 8 + m), ('scT', m)), w=(('cuT', m),))
                    sc.op('pool', lambda e: e.tensor_copy(cu_halo[:, m, :], cuT[:, m, 512:514]),
                          r=(('cuT', m),), w=('cu_halo',))
                    cw = lambda j: ppl[:, 24 + m * 3 + j:24 + m * 3 + j + 1]
                    sc.op('dve', lambda e: e.tensor_scalar(macc[:], cuT[:, m, 0:512], cw(0), None, ALU.mult),
                          r=(('cuT', m), 'ppl'), w=('macc',))
                    sc.op('dve', lambda e: e.scalar_tensor_tensor(macc[:], cuT[:, m, 1:513], cw(1), macc[:],
                                                                  ALU.mult, ALU.add),
                          r=(('cuT', m), 'ppl', 'macc'), w=('macc',))
                    sc.op('dve', lambda e: e.scalar_tensor_tensor(macc[:], cuT[:, m, 2:514], cw(2), macc[:],
                                                                  ALU.mult, ALU.add),
                          r=(('cuT', m), 'ppl', 'macc'), w=('macc',))
                    sc.op('dve', lambda e: e.tensor_tensor(ybT[:, m, :], macc[:], scT[:, 4 + m, 2:514], ALU.mult),
                          r=('macc', ('scT', 4 + m)), w=('ybT',))
                if not do_attn:
                    sc.op('pool', lambda e: e.memset(yaT[:], 0.0), w=('yaT',))
                if not do_rwkv:
                    sc.op('pool', lambda e: e.memset(ycT[:], 0.0), w=('ycT',))
                for m in range(KC):
                    gparts = [(win_d[l][:, OFF_GATE + g * D + m * 128:OFF_GATE + g * D + (m + 1) * 128],
                               128, KC, 128) for g in range(3)]
                    gv, gk = load_slab(gparts)
                    bparts = [(wbr_d[l, 0][:, m * 128:(m + 1) * 128], 64, 8, 128),
                              (wbr_d[l, 1][:, m * 128:(m + 1) * 128], 128, 4, 128),
                              (wbr_d[l, 2][:, m * 128:(m + 1) * 128], 128, 4, 128)]
                    bv, bk = load_slab(bparts)
                    for g in range(3):
                        pg = next_ps()
                        mm(pg, [(gv[g][:, kc, :], xnT[:, kc, :]) for kc in range(KC)], rkeys=(gk, 'xnT'))
                        gi = g % 2
                        sc.op('act', lambda e: e.activation(gsig[gi][:], ps[pg][:], AF.Sigmoid,
                                                            bias=ppl[:, g * 8 + m:g * 8 + m + 1], scale=1.0),
                              r=('ps%d' % pg, 'ppl'), w=('gsig%d' % gi,))
                        pu = next_ps()
                        if g == 0:
                            prs = [(bv[0][:, hh, :], yaT[:, hh, :]) for hh in range(8)]
                            rk = (bk, 'yaT')
                        elif g == 1:
                            prs = [(bv[1][:, c4, :], ybT[:, c4, :]) for c4 in range(4)]
                            rk = (bk, 'ybT')
                        else:
                            prs = [(bv[2][:, c4, :], ycT[:, c4, :]) for c4 in range(4)]
                            rk = (bk, 'ycT')
                        mm(pu, prs, rkeys=rk)
                        if g == 0:
                            sc.op('dve', lambda e: e.tensor_tensor(macc[:], gsig[gi][:], ps[pu][:], ALU.mult),
                                  r=('gsig%d' % gi, 'ps%d' % pu), w=('macc',))
                        else:
                            sc.op('dve', lambda e: e.tensor_tensor(mtmp[:], gsig[gi][:], ps[pu][:], ALU.mult),
                                  r=('gsig%d' % gi, 'ps%d' % pu), w=('mtmp',))
                            dst = macc[:] if g == 1 else mixT[:, m, :]
                            dk = 'macc' if g == 1 else 'mixT'
                            sc.op('pool', lambda e: e.tensor_tensor(dst, macc[:], mtmp[:], ALU.add),
                                  r=('macc', 'mtmp'), w=(dk,))
                for half in range(2):
                    (wv,), wk = load_slab([(wout_d[l][:, half * 512:(half + 1) * 512], 128, KC, 512)])
                    for tt in range(4):
                        gt = blk * 4 + tt
                        pi = next_ps()
                        mm(pi, [(mixT[:, kc, tt * 128:(tt + 1) * 128], wv[:, kc, :]) for kc in range(KC)],
                           rkeys=(wk, 'mixT'))
                        sc.op('dve', lambda e: e.tensor_tensor(h[:, gt, half * 512:(half + 1) * 512],
                                                               h[:, gt, half * 512:(half + 1) * 512],
                                                               ps[pi][:], ALU.add),
                              r=(('h', gt), 'ps%d' % pi), w=(('h', gt),))
                sc.barrier()
                if flags.get('skip_ffn', False):
                    continue
                rmsnorm_to_xnT(blk, gffn, 'gffn', 1)
                for f in range(NFF):
                    wparts = [(fup_d[l][:, f * 128:(f + 1) * 128], 128, KC, 128),
                              (fup_d[l][:, DFF + f * 128:DFF + (f + 1) * 128], 128, KC, 128)]
                    wv, wk = load_slab(wparts)
                    bi = f % 2
                    for which in range(2):
                        pi = next_ps()
                        mm(pi, [(wv[which][:, kc, :], xnT[:, kc, :]) for kc in range(KC)], rkeys=(wk, 'xnT'))
                        buf = (fg if which == 0 else fu)[bi]
                        bkey = ('fg%d' if which == 0 else 'fu%d') % bi
                        cbuf = (fgc if which == 0 else fuc)[bi]
                        ckey = 'fgc0' if which == 0 else 'fuc0'
                        hc = which * NFF + f
                        sc.op('act', lambda e: e.copy(buf[:, 2:514], ps[pi][:]), r=('ps%d' % pi,), w=(bkey,))
                        sc.op('pool', lambda e: e.tensor_copy(buf[:, 0:2], f_halo[:, hc, :]),
                              r=(('f_halo', hc),), w=(bkey,))
                        sc.op('pool', lambda e: e.tensor_copy(f_halo[:, hc, :], buf[:, 512:514]),
                              r=(bkey,), w=(('f_halo', hc),))
                        eng = 'dve'
                        cw = lambda j: ppl[:, 36 + hc * 3 + j:36 + hc * 3 + j + 1]
                        sc.op(eng, lambda e: e.tensor_scalar(cbuf[:], buf[:, 0:512], cw(0), None, ALU.mult),
                              r=(bkey, 'ppl'), w=(ckey,))
                        sc.op(eng, lambda e: e.scalar_tensor_tensor(cbuf[:], buf[:, 1:513], cw(1), cbuf[:],
                                                                    ALU.mult, ALU.add),
                              r=(bkey, 'ppl', ckey), w=(ckey,))
                        sc.op(eng, lambda e: e.scalar_tensor_tensor(cbuf[:], buf[:, 2:514], cw(2), cbuf[:],
                                                                    ALU.mult, ALU.add),
                              r=(bkey, 'ppl', ckey), w=(ckey,))
                    sc.op('act', lambda e: e.activation(fsl[bi][:], fgc[bi][:], AF.Silu),
                          r=('fgc0',), w=('fgc0',))
                    sc.op('dve', lambda e: e.tensor_tensor(actT[:, f, :], fsl[bi][:], fuc[bi][:], ALU.mult),
                          r=('fgc0', 'fuc0'), w=(('actT', f),))
                for half in range(2):
                    pis = [next_ps() for _ in range(4)]
                    groups = [(0, 8), (8, 16), (16, 22)]
                    for gi_, (f0, f1) in enumerate(groups):
                        (wv,), wk = load_slab([(fdn_d[l][f0 * 128:f1 * 128, half * 512:(half + 1) * 512],
                                                128, f1 - f0, 512)])
                        for tt in range(4):
                            def fn(pe, tt=tt, wv=wv, f0=f0, f1=f1):
                                ins = None
                                for f in range(f0, f1):
                                    ins = pe.matmul(ps[pis[tt]][:], actT[:, f, tt * 128:(tt + 1) * 128],
                                                    wv[:, f - f0, :], start=(f == 0), stop=(f == NFF - 1))
                                return ins
                            rk = (wk,) + tuple(('actT', f) for f in range(f0, f1))
                            if gi_ == 0:
                                sc.op('pe', fn, r=rk, w=('ps%d' % pis[tt],))
                            else:
                                sc.op('pe', fn, r=rk + ('ps%d' % pis[tt],), w=('ps%d' % pis[tt],))
                    for tt in range(4):
                        gt = blk * 4 + tt
                        sc.op('dve', lambda e: e.tensor_tensor(h[:, gt, half * 512:(half + 1) * 512],
                                                               h[:, gt, half * 512:(half + 1) * 512],
                                                               ps[pis[tt]][:], ALU.add),
                              r=(('h', gt), 'ps%d' % pis[tt]), w=(('h', gt),))
        sc.barrier()
        sc.dma('pool', gmix[:], norms_d[2 * L:2 * L + 1, :].broadcast_to([128, D]), w=('gmix',))
        for gt in range(NT):
            col = gt
            sc.op('act', lambda e: e.activation(junk[:], h[:, gt, :], AF.Square,
                                                accum_out=ss[:, col:col + 1]),
                  r=(('h', gt),), w=('junk', ('ss', col)))
            sc.op('act', lambda e: e.activation(rstd[:, col:col + 1], ss[:, col:col + 1], AF.Sqrt,
                                                bias=consts[:, 261:262], scale=1.0 / D),
                  r=(('ss', col), 'consts'), w=(('rstd', col),))
            sc.op('dve', lambda e: e.reciprocal(rstd[:, col:col + 1], rstd[:, col:col + 1]),
                  r=(('rstd', col),), w=(('rstd', col),))
            oi = gt % 2
            sc.op('dve', lambda e: e.scalar_tensor_tensor(outb[oi][:], h[:, gt, :],
                                                          rstd[:, col:col + 1], gmix[:],
                                                          ALU.mult, ALU.mult),
                  r=(('h', gt), ('rstd', col), 'gmix'), w=('outb%d' % oi,))
            sc.dma('sp', y_d[sq, gt * 128:(gt + 1) * 128, :], outb[oi][:], r=('outb%d' % oi,), w=(('y', sq, gt),))
    sc.finish([('y', sq, gt) for sq in range(NSEQ) for gt in range(NT)])
    print('instructions emitted:', sc.nins, sc.cnt, flush=True)
    return nc


def _swap_idx(dh):
    rot = dh // 4
    half = rot // 2
    idx = np.arange(dh)
    idx[:half] = np.arange(half) + half
    idx[half:rot] = np.arange(half)
    return idx


def _att_cols():
    main = []
    swap = []
    s64 = _swap_idx(64)
    s32 = _swap_idx(32)
    for i in range(4):
        for hh in (i, 4 + i):
            main += [OFF_Q + hh * 64 + d for d in range(64)]
            swap += [OFF_Q + hh * 64 + int(s64[d]) for d in range(64)]
    for hh in range(2):
        main += [OFF_K + hh * 64 + d for d in range(64)]
        swap += [OFF_K + hh * 64 + int(s64[d]) for d in range(64)]
    for hh in range(8):
        main += [OFF_QI + hh * 32 + d for d in range(32)]
        swap += [OFF_QI + hh * 32 + int(s32[d]) for d in range(32)]
    for rep in range(2):
        main += [OFF_KI + d for d in range(32)]
        swap += [OFF_KI + int(s32[d]) for d in range(32)]
    vw = [OFF_V + d for d in range(128)] + [OFF_WI + d for d in range(8)]
    assert len(main) == ATT_MAIN and len(swap) == ATT_MAIN
    return np.array(main + swap + vw)


def _consts():
    c = np.zeros((128, 1024), np.float32)
    c[:, 0:128] = np.eye(128, dtype=np.float32)
    p = np.arange(128)
    theta = 500000.0
    d = p % 64
    c[:, 128] = np.where(d < 16, theta ** (-(d % 8) * 2.0 / 16), 0.0)
    c[:, 129] = np.where(d < 8, -1.0, np.where(d < 16, 1.0, 0.0))
    d = p % 32
    c[:, 130] = np.where(d < 8, theta ** (-(d % 4) * 2.0 / 8), 0.0)
    c[:, 131] = np.where(d < 4, -1.0, np.where(d < 8, 1.0, 0.0))
    c[:, 132] = -math.pi
    c[:, 261] = RMS_EPS
    c[:, 263:327] = 1.0
    c[0:64, 327:391] = np.triu(np.ones((64, 64), np.float32), 1)
    c[0:64, 391:455] = np.triu(np.ones((64, 64), np.float32), 0)
    c[0:64, 455:519] = np.tril(np.ones((64, 64), np.float32), -1)
    c[0:64, 519:583] = 1.0
    c[64:128, 583:647] = 1.0
    c[:, 647:775] = 1.0
    c[:, 647] = 0.0
    c[:, 711] = 0.0
    c[0:64, 775:839] = np.eye(64, dtype=np.float32)
    c[0:64, 839] = 1.0
    c[64:128, 840] = 1.0
    c[:, 262] = 64e-5
    t = np.arange(128)[:, None]
    s_ = np.arange(128)[None, :]
    c[:, 133:261] = np.where(s_ <= t, 0.0, NEG)
    return c


def _prep(inputs, L, ncores):
    f = lambda k: np.asarray(inputs[k], dtype=np.float32)
    w_in = f('w_in')[:L]
    w_att = np.ascontiguousarray(w_in[:, :, _att_cols()])
    norms = np.concatenate([np.stack([f('norm_mix')[l], f('norm_ffn')[l]]) for l in range(L)]
                           + [f('norm_final')[None]], axis=0)
    pp = np.zeros((L, 128, 256), np.float32)
    bg = f('b_gate')[:L].reshape(L, 3, 8, 128)
    pp[:, :, 0:24] = bg.transpose(0, 3, 1, 2).reshape(L, 128, 24)
    scv = f('sc_conv')[:L].reshape(L, 3, 4, 128)
    pp[:, :, 24:36] = scv.transpose(0, 3, 2, 1).reshape(L, 128, 12)
    fc = f('ffn_conv')[:L].reshape(L, 3, 2, NFF, 128)
    pp[:, :, 36:168] = fc.transpose(0, 4, 2, 3, 1).reshape(L, 128, 132)
    mu = f('rw_mu')[:L]
    pp[:, :, 168:180] = mu[:, 0:1536].reshape(L, 12, 128).transpose(0, 2, 1)
    pp[:, :, 180] = mu[:, 1536:1664]
    pp[:, :, 181] = mu[:, 1664:1792]
    def pair4(v):
        return v.reshape(L, 4, 128).transpose(0, 2, 1)
    pp[:, :, 182:186] = pair4(f('rw_w0')[:L])
    pp[:, :, 186:190] = pair4(f('rw_a0')[:L])
    pp[:, :, 190:194] = pair4(f('rw_k_k')[:L])
    pp[:, :, 194:198] = pair4(f('rw_k_a')[:L])
    pp[:, :, 198:202] = pair4(f('rw_r_k')[:L].reshape(L, 512))
    pp[:, :, 202:206] = pair4(f('rw_ln_w')[:L])
    pp[:, :, 206:210] = pair4(f('rw_ln_b')[:L])
    rws = np.zeros((L, 128, 1536), np.float32)
    rws[:, 0:64, 0:512] = f('rw_w_up')[:L]
    rws[:, 64:128, 512:1024] = f('rw_a_up')[:L]
    rws[:, :, 1024:1536] = f('rw_g_up')[:L]
    shared = {
        'w_att': w_att, 'w_in': np.ascontiguousarray(w_in),
        'w_branch': np.ascontiguousarray(f('w_branch')[:L]),
        'w_out': np.ascontiguousarray(f('w_out')[:L]),
        'ffn_up': np.ascontiguousarray(f('ffn_up')[:L]),
        'ffn_down': np.ascontiguousarray(f('ffn_down')[:L]),
        'norms': np.ascontiguousarray(norms), 'pp_layer': pp, 'consts': _consts(), 'rw_small': rws,
    }
    x = f('x')
    pos = np.asarray(inputs['positions']).astype(np.int32)
    B = x.shape[0]
    per = B // ncores
    maps = []
    for c in range(ncores):
        m = dict(shared)
        m['x'] = np.ascontiguousarray(x[c * per:(c + 1) * per])
        m['positions'] = np.ascontiguousarray(pos[c * per:(c + 1) * per])
        maps.append(m)
    return maps, per


def run(inputs, L=4, ncores=8, flags=None, trace=False):
    flags = flags or {}
    maps, per = _prep(inputs, L, ncores)
    S = maps[0]['x'].shape[1]
    nc = build_program(S, L, per, flags)
    res = run_bass_kernel_spmd(nc, maps, core_ids=list(range(ncores)), trace=trace)
    out = np.concatenate([r['y'] for r in res.results], axis=0)
    return out, res


def kernel(**inputs):
    out, _ = run(inputs, L=4, ncores=8)
    return out.astype(np.float32)
```

```python
import math
import numpy as np
import concourse.bass as bass
import concourse.mybir as mybir
from concourse.bass_utils import run_bass_kernel_spmd

F32 = mybir.dt.float32
BF16 = mybir.dt.bfloat16
I32 = mybir.dt.int32
AF = mybir.ActivationFunctionType
ALU = mybir.AluOpType
AX = mybir.AxisListType

D = 1024
KC = 8
NIN = 7464
DFF = 2816
NFF = 22
OFF_Q, OFF_K, OFF_V, OFF_QI, OFF_KI, OFF_WI = 0, 512, 640, 768, 1024, 1056
OFF_SC, OFF_RW, OFF_GATE = 1064, 2600, 4392
ATT_MAIN = 960
ATT_COLS = 2 * ATT_MAIN + 136
RMS_EPS = 1e-6
C0 = math.exp(-0.5)
NEG = -1.0e30
BIS_ITERS = 12
STRICT = False


class Sched:
    def __init__(self, nc, n_dma_sems=32):
        if STRICT:
            n_dma_sems = 8
        self.nc = nc
        self.nc = nc
        self.E = {'pe': nc.tensor, 'act': nc.scalar, 'dve': nc.vector,
                  'pool': nc.gpsimd, 'sp': nc.sync}
        self.sem = {k: nc.alloc_semaphore('sem_' + k) for k in ('pe', 'act', 'dve', 'pool')}
        self.cnt = {k: 0 for k in self.sem}
        self.dsem = [nc.alloc_semaphore('dsem%d' % i) for i in range(n_dma_sems)]
        self.dval = [0] * n_dma_sems
        self.nrot = n_dma_sems
        self.drr = 0
        self.known = {k: {} for k in self.E}
        self.lastw = {}
        self.readers = {}
        self.nins = 0
        self.strict = STRICT
        self.strict_dma = STRICT

    def _semh(self, sk):
        return self.sem[sk] if isinstance(sk, str) else self.dsem[sk]

    def _wait(self, e, sk, val):
        if self.known[e].get(sk, 0) >= val:
            return
        self.E[e].wait_ge(self._semh(sk), val)
        self.known[e][sk] = val
        self.nins += 1

    def _deps(self, e, r, w):
        need = {}

        def add(sk, v):
            if need.get(sk, 0) < v:
                need[sk] = v
        for k in r:
            if k in self.lastw:
                add(*self.lastw[k])
        for k in w:
            if k in self.lastw:
                sk, v = self.lastw[k]
                if sk != e or self.strict:
                    add(sk, v)
            for sk, v in self.readers.get(k, {}).items():
                if sk != e or self.strict:
                    add(sk, v)
        for sk, v in need.items():
            self._wait(e, sk, v)

    def _commit(self, ev, r, w):
        for k in w:
            self.lastw[k] = ev
            self.readers[k] = {}
        for k in r:
            d = self.readers.setdefault(k, {})
            if d.get(ev[0], 0) < ev[1]:
                d[ev[0]] = ev[1]

    def op(self, e, fn, r=(), w=()):
        self._deps(e, r, w)
        ins = fn(self.E[e])
        self.cnt[e] += 1
        ins.then_inc(self.sem[e], 1)
        self.nins += 1
        self._commit((e, self.cnt[e]), r, w)

    def dma(self, q, out, in_, r=(), w=()):
        self._deps(q, r, w)
        if self.strict_dma and q == 'pool':
            self.dsem.append(self.nc.alloc_semaphore('dsx%d' % len(self.dsem)))
            self.dval.append(0)
            i = len(self.dsem) - 1
            ins = self.E[q].dma_start(out=out, in_=in_)
            self.dval[i] += 16
            ins.then_inc(self.dsem[i], 16)
            self._commit((i, self.dval[i]), r, w)
            return
        i = self.drr
        self.drr = (self.drr + 1) % self.nrot
        if self.dval[i] > 0:
            self._wait(q, i, self.dval[i])
        ins = self.E[q].dma_start(out=out, in_=in_)
        self.dval[i] += 16
        ins.then_inc(self.dsem[i], 16)
        self.nins += 1
        self._commit((i, self.dval[i]), r, w)

    def barrier(self):
        for e in ('pe', 'act', 'dve', 'pool', 'sp'):
            for f in ('pe', 'act', 'dve', 'pool'):
                if f != e and self.cnt[f] > 0:
                    self._wait(e, f, self.cnt[f])

    def finish(self, keys):
        for k in keys:
            if k in self.lastw:
                sk, v = self.lastw[k]
                self._wait('sp', sk, v)
        for i in range(len(self.dsem)):
            if self.dval[i] > 0:
                self._wait('sp', i, self.dval[i])


def build_program(S, L, NSEQ, flags):
    do_attn = flags.get('attn', True)
    do_rwkv = flags.get('rwkv', True)
    NT = S // 128
    NB = S // 512
    KSEL = min(256, S // 4)
    nc = bass.Bass("TRN2", target_bir_lowering=False)
    dt = nc.dram_tensor

    def din(name, shape, dtype=F32):
        return dt(name, list(shape), dtype, kind="ExternalInput").ap()
    x_d = din("x", [NSEQ, S, D])
    pos_d = din("positions", [NSEQ, S], I32)
    watt_d = din("w_att", [L, D, ATT_COLS])
    win_d = din("w_in", [L, D, NIN])
    wbr_d = din("w_branch", [L, 3, 512, D])
    wout_d = din("w_out", [L, D, D])
    fup_d = din("ffn_up", [L, D, 2 * DFF])
    fdn_d = din("ffn_down", [L, DFF, D])
    norms_d = din("norms", [2 * L + 1, D])
    ppl_d = din("pp_layer", [L, 128, 256])
    consts_d = din("consts", [128, 1024])
    rww_d = din("rw_small", [L, 128, 1536])
    y_d = dt("y", [NSEQ, S, D], F32, kind="ExternalOutput").ap()

    sc = Sched(nc)
    if flags.get('strict_deps', False):
        sc.strict = True
    al = nc.alloc_sbuf_tensor

    h = al("h_sb", [128, NT, D], F32)
    xnT = al("xnT", [128, KC, 512], BF16)
    NSLAB = 2
    slabs = [al("slab%d" % i, [128, 4096], BF16) for i in range(NSLAB)]
    slab_rr = [0]
    gmix = al("gmix", [128, D], BF16)
    gffn = al("gffn", [128, D], BF16)
    ppl = al("ppl", [128, 256], F32)
    consts = al("consts_sb", [128, 1024], F32)
    ident = al("ident", [128, 128], BF16)
    onesb = al("onesb", [128, 128], BF16)
    bdb = al("bdb", [128, 128], BF16)
    ss = al("ss", [128, 2 * NT], F32)
    rstd = al("rstd", [128, 2 * NT], F32)
    junk = al("junk", [128, D], BF16)
    xnb = [al("xnb0", [128, D], BF16)] * 2
    yaT = al("yaT", [64, 8, 512], BF16)
    ycT = al("ycT", [128, 4, 512], BF16)
    MSL8 = al("MSL8", [64, 8, 64], BF16)
    MAT = al("MAT", [64, 8, 2, 64], BF16)
    I8 = al("I8", [64, 8, 64], BF16)
    cu_halo = al("cu_halo", [128, 4, 2], BF16)
    f_halo = al("f_halo", [128, 2 * NFF, 2], BF16)
    kT = al("kT", [128, S], BF16)
    kiT = al("kiT", [64, S], BF16)
    vaug = al("vaug", [128, NT, 2, 66], BF16)
    wi = al("wi", [128, NT, 8], F32)
    rw_small = al("rw_small_sb", [128, 1536], BF16)
    Hst = al("Hst", [128, 4, 128], F32)
    zprev = al("zprev", [128, 14], BF16)
    abase = (nc.sbuf_base + 63) // 64 * 64
    ARENA = nc.sbuf_bytes_remaining - 192
    arena = al("arena", [128, (ARENA + 64) // 2], BF16)
    acur = [0]

    def aa(name, shape, dtype):
        nbytes = int(np.prod(shape[1:])) * (4 if dtype in (F32, I32) else 2)
        nbytes = (nbytes + 31) // 32 * 32
        off = abase + acur[0]
        acur[0] += nbytes
        assert acur[0] <= ARENA, (name, acur[0], ARENA)
        return nc.alloc_sbuf_tensor_at(name, list(shape), dtype, offset=off)
    acur[0] = 0
    scT = aa("scT", [128, 12, 514], BF16)
    cuT = aa("cuT", [128, 4, 514], BF16)
    ybT = aa("ybT", [128, 4, 512], BF16)
    mixT = aa("mixT", [128, KC, 512], BF16)
    gsig = [aa("gsig%d" % i, [128, 512], F32) for i in range(2)]
    macc = aa("macc", [128, 512], F32)
    mtmp = aa("mtmp", [128, 512], F32)
    acur[0] = 0
    actT = aa("actT", [128, NFF, 512], BF16)
    fg = [aa("fg%d" % i, [128, 514], BF16) for i in range(2)]
    fu = [aa("fu%d" % i, [128, 514], BF16) for i in range(2)]
    fgc = [aa("fgc%d" % i, [128, 512], F32) for i in range(1)] * 2
    fuc = [aa("fuc%d" % i, [128, 512], F32) for i in range(1)] * 2
    fsl = fgc
    acur[0] = 0
    outb = [aa("outb%d" % i, [128, D], F32) for i in range(2)]
    acur[0] = 0
    posi = aa("posi", [128, 512], I32)
    posf = aa("posf", [128, 512], F32)
    rtA = aa("rtA", [128, 512], F32)
    rtB = aa("rtB", [128, 512], F32)
    rtC = aa("rtC", [128, 512], F32)
    rtD = aa("rtD", [128, 512], F32)
    Cq = aa("Cq", [128, 512], BF16)
    Sq = aa("Sq", [128, 512], BF16)
    Ci = aa("Ci", [128, 512], BF16)
    Si = aa("Si", [128, 512], BF16)
    qT = aa("qT", [128, 4, 512], BF16)
    qiT = aa("qiT", [64, 4, 512], BF16)
    isc = aa("isc", [128, S], F32)
    rl = [aa("rl%d" % i, [128, 512], F32) for i in range(2)]
    maskb = aa("maskb", [128, S], BF16)
    maskT = aa("maskT", [128, NT, 128], BF16)
    pt = [aa("pt%d" % i, [128, 512], BF16) for i in range(2)]
    rdn = aa("rdn", [128, 512], F32)
    rdnb = aa("rdnb", [128, 512], BF16)
    accsb = aa("accsb", [64, 512], F32)
    bsm = aa("bsm", [128, 8], F32)
    acur[0] = 0
    zT = aa("zT", [128, 14, 513], BF16)

    def f4(name):
        return aa(name, [128, 4, 128], F32)
    r_ = f4("rw_r"); k_ = f4("rw_k"); v_ = f4("rw_v"); sg = f4("rw_sg"); Ls = f4("rw_Ls")
    g_ = f4("rw_g"); kkn = f4("rw_kkn"); kf_ = f4("rw_kf")
    t1 = f4("rw_t1"); t2 = f4("rw_t2"); E_ = f4("rw_E"); YT = sg
    a_ = E_
    lraw = t2
    lwla = aa("rw_lwla", [128, 128], BF16)
    lgb = aa("rw_lgb", [128, 128], BF16)
    tb = aa("rw_tb", [128, 4, 128], BF16)
    AR = aa("rw_AR", [128, 4, 2, 128], BF16)
    BT = aa("rw_BT", [128, 4, 128], BF16)
    KT_ = aa("rw_KT", [128, 4, 128], BF16)
    BH = aa("rw_BH", [128, 4, 128], BF16)
    KH = aa("rw_KH", [128, 4, 128], BF16)
    VT = aa("rw_VT", [128, 4, 128], BF16)
    wc = aa("rw_wc", [128, 4, 2], F32)
    Vpad = aa("rw_Vpad", [64, 8, 128], BF16)
    BHpad = aa("rw_BHpad", [64, 8, 128], BF16)
    KHpad = aa("rw_KHpad", [64, 8, 128], BF16)
    ARm = [aa("rw_ARm%d" % i, [128, 4, 2, 128], BF16) for i in range(2)]
    Upad = aa("rw_Upad", [64, 8, 128], BF16)
    ATb = aa("rw_ATb", [64, 8, 2, 64], BF16)
    ATk = aa("rw_ATk", [64, 8, 2, 64], BF16)
    Pm = [aa("rw_P%d" % i, [64, 8, 64], BF16) for i in range(2)]
    Qm = [aa("rw_Q%d" % i, [64, 8, 64], BF16) for i in range(2)]
    STm = [aa("rw_S%d" % i, [64, 8, 64], BF16) for i in range(2)]
    R0 = aa("rw_R0", [64, 8, 64], BF16)
    Hbf = aa("rw_Hbf", [128, 4, 128], BF16)
    print('arena bytes', ARENA, 'rwkv uses', acur[0], flush=True)

    ps = [nc.alloc_psum_tensor("ps%d" % i, [128, 512], F32) for i in range(6)]
    pst = [nc.alloc_psum_tensor("pst%d" % i, [128, 1024], BF16) for i in range(2)]

    rr = {'ps': 0, 'pst': 0, 'xnb': 0}

    ps_reserved = set()

    def next_ps():
        while True:
            i = rr['ps']
            rr['ps'] = (i + 1) % 6
            if i not in ps_reserved:
                return i

    def load_slab(parts):
        i = slab_rr[0]
        slab_rr[0] = (i + 1) % NSLAB
        key = 'slab%d' % i
        views = []
        off = 0
        for (src, p, kc, n) in parts:
            v = slabs[i][0:p, off:off + kc * n].rearrange("p (k n) -> p k n", k=kc)
            sc.dma('pool', v, src.rearrange("(k p) n -> p k n", p=p), r=(), w=(key,))
            views.append(v)
            off += kc * n
        assert off <= 4096
        return views, key

    def mm(psi, pairs, rkeys, out=None):
        o = ps[psi][:] if out is None else out

        def fn(pe):
            ins = None
            n = len(pairs)
            for j, (l_, r_) in enumerate(pairs):
                ins = pe.matmul(o, l_, r_, start=(j == 0), stop=(j == n - 1))
            return ins
        sc.op('pe', fn, r=rkeys, w=('ps%d' % psi,))

    sc.dma('sp', consts[:], consts_d, w=('consts',))
    sc.dma('pool', ident[:], consts_d[:, 0:128], w=('ident',))
    sc.dma('pool', bdb[:], consts_d[:, 519:647], w=('bdb',))
    sc.op('pool', lambda e: e.memset(onesb[:], 1.0), w=('onesb',))
    for hh in range(8):
        sc.op('dve', lambda e: e.tensor_copy(MSL8[:, hh, :], consts[0:64, 455:519]), r=('consts',), w=('MSL8',))
        sc.op('dve', lambda e: e.tensor_copy(MAT[:, hh, 0, :], consts[0:64, 327:391]), r=('consts',), w=('MAT',))
        sc.op('dve', lambda e: e.tensor_copy(MAT[:, hh, 1, :], consts[0:64, 391:455]), r=('consts',), w=('MAT',))
        sc.op('dve', lambda e: e.tensor_copy(I8[:, hh, :], consts[0:64, 775:839]), r=('consts',), w=('I8',))

    def rmsnorm_to_xnT(blk, gtile, gkey, slot):
        for tt in range(4):
            gt = blk * 4 + tt
            col = slot * NT + gt
            sc.op('act', lambda e: e.activation(junk[:], h[:, gt, :], AF.Square,
                                                accum_out=ss[:, col:col + 1]),
                  r=(('h', gt),), w=('junk', ('ss', col)))
            sc.op('act', lambda e: e.activation(rstd[:, col:col + 1], ss[:, col:col + 1], AF.Sqrt,
                                                bias=consts[:, 261:262], scale=1.0 / D),
                  r=(('ss', col), 'consts'), w=(('rstd', col),))
            sc.op('dve', lambda e: e.reciprocal(rstd[:, col:col + 1], rstd[:, col:col + 1]),
                  r=(('rstd', col),), w=(('rstd', col),))
            xi = 0
            sc.op('dve', lambda e: e.scalar_tensor_tensor(xnb[xi][:], h[:, gt, :],
                                                          rstd[:, col:col + 1], gtile[:],
                                                          ALU.mult, ALU.mult),
                  r=(('h', gt), ('rstd', col), gkey), w=('xnb%d' % xi,))
            pi = rr['pst']
            rr['pst'] = 1 - pi

            def tr(pe):
                ins = None
                for kc in range(KC):
                    ins = pe.transpose(pst[pi][:, kc * 128:(kc + 1) * 128],
                                       xnb[xi][:, kc * 128:(kc + 1) * 128], ident[:])
                return ins
            sc.op('pe', tr, r=('xnb%d' % xi, 'ident'), w=('pst%d' % pi,))
            sc.op('act', lambda e: e.copy(xnT[:, :, tt * 128:(tt + 1) * 128],
                                          pst[pi][:].rearrange("p (k t) -> p k t", k=KC)),
                  r=('pst%d' % pi,), w=('xnT',))


    def rope_tables(sq, blk):
        t0 = blk * 512
        sc.dma('sp', posi[:], pos_d[sq:sq + 1, t0:t0 + 512].broadcast_to([128, 512]), w=('posi',))
        sc.op('dve', lambda e: e.tensor_copy(posf[:], posi[:]), r=('posi',), w=('posf',))
        TWO_PI = 2 * math.pi

        def sin_of(dst, dkey, shift):
            sc.op('dve', lambda e: e.tensor_scalar(rtB[:], rtA[:], shift, 1.0 / TWO_PI, ALU.add, ALU.mult),
                  r=('rtA',), w=('rtB',))
            sc.op('dve', lambda e: e.tensor_copy(posi[:], rtB[:]), r=('rtB',), w=('posi',))
            sc.op('dve', lambda e: e.tensor_copy(rtB[:], posi[:]), r=('posi',), w=('rtB',))
            sc.op('dve', lambda e: e.scalar_tensor_tensor(rtB[:], rtB[:], -TWO_PI, rtA[:], ALU.mult, ALU.add),
                  r=('rtB', 'rtA'), w=('rtB',))
            if shift != 0.0:
                sc.op('dve', lambda e: e.tensor_scalar(rtB[:], rtB[:], shift, None, ALU.add),
                      r=('rtB',), w=('rtB',))
            sc.op('dve', lambda e: e.tensor_scalar(rtC[:], rtB[:], math.pi, -TWO_PI, ALU.is_gt, ALU.mult),
                  r=('rtB',), w=('rtC',))
            sc.op('dve', lambda e: e.tensor_tensor(rtB[:], rtB[:], rtC[:], ALU.add), r=('rtB', 'rtC'), w=('rtB',))
            sc.op('dve', lambda e: e.tensor_scalar(rtC[:], rtB[:], -math.pi, TWO_PI, ALU.is_lt, ALU.mult),
                  r=('rtB',), w=('rtC',))
            sc.op('dve', lambda e: e.tensor_tensor(rtB[:], rtB[:], rtC[:], ALU.add), r=('rtB', 'rtC'), w=('rtB',))
            sc.op('dve', lambda e: e.tensor_scalar(rtB[:], rtB[:], -3.1415925, 3.1415925, ALU.max, ALU.min),
                  r=('rtB',), w=('rtB',))
            sc.op('act', lambda e: e.activation(dst, rtB[:], AF.Sin), r=('rtB',), w=(dkey,))
        for (fc, sg, Ct, St, nm) in ((128, 129, Cq, Sq, 'q'), (130, 131, Ci, Si, 'i')):
            sc.op('dve', lambda e: e.tensor_scalar(rtA[:], posf[:], consts[:, fc:fc + 1], None, ALU.mult),
                  r=('posf', 'consts'), w=('rtA',))
            sin_of(rtD[:], 'rtD', 0.0)
            sc.op('dve', lambda e: e.tensor_scalar(St[:], rtD[:], consts[:, sg:sg + 1], None, ALU.mult),
                  r=('rtD', 'consts'), w=('S' + nm,))
            sin_of(Ct[:], 'C' + nm, 0.5 * math.pi)

    def attention_block(sq, l, blk):
        t0 = blk * 512
        if flags.get('att_stage', 9) >= 0:
            rope_tables(sq, blk)
        if flags.get('att_stage', 9) in (0, -1):
            sc.op('pool', lambda e: e.memset(yaT[:], 0.0), w=('yaT',))
            return
        nroped = [0]

        def roped(wm, ws, wk1, wk2, c0, M, Ct, St, tn, dst, dkey):
            nroped[0] += 1
            if nroped[0] > flags.get('att_sub', 99):
                return
            p1 = next_ps()
            mm(p1, [(wm[:, kc, c0:c0 + M], xnT[:, kc, :]) for kc in range(KC)], rkeys=(wk1, 'xnT'),
               out=ps[p1][0:M, :])
            p2 = next_ps()
            mm(p2, [(ws[:, kc, c0:c0 + M], xnT[:, kc, :]) for kc in range(KC)], rkeys=(wk2, 'xnT'),
               out=ps[p2][0:M, :])
            sc.op('dve', lambda e: e.tensor_tensor(rtA[0:M, :], ps[p1][0:M, :], Ct[0:M, :], ALU.mult),
                  r=('ps%d' % p1, 'C' + tn), w=('rtA',))
            sc.op('dve', lambda e: e.tensor_tensor(rtB[0:M, :], ps[p2][0:M, :], St[0:M, :], ALU.mult),
                  r=('ps%d' % p2, 'S' + tn), w=('rtB',))
            sc.op('pool', lambda e: e.tensor_tensor(dst, rtA[0:M, :], rtB[0:M, :], ALU.add),
                  r=('rtA', 'rtB'), w=(dkey,))
        (wm,), k1 = load_slab([(watt_d[l][:, 0:512], 128, KC, 512)])
        (ws,), k2 = load_slab([(watt_d[l][:, ATT_MAIN:ATT_MAIN + 512], 128, KC, 512)])
        for m in range(4):
            roped(wm, ws, k1, k2, m * 128, 128, Cq, Sq, 'q', qT[:, m, :], 'qT')
        (wm,), k1 = load_slab([(watt_d[l][:, 512:960], 128, KC, 448)])
        (ws,), k2 = load_slab([(watt_d[l][:, ATT_MAIN + 512:ATT_MAIN + 960], 128, KC, 448)])
        roped(wm, ws, k1, k2, 0, 128, Cq, Sq, 'q', kT[:, t0:t0 + 512], 'kT')
        for c in range(4):
            roped(wm, ws, k1, k2, 128 + c * 64, 64, Ci, Si, 'i', qiT[:, c, :], 'qiT')
        roped(wm, ws, k1, k2, 384, 64, Ci, Si, 'i', kiT[:, t0:t0 + 512], 'kiT')
        (wv,), k3 = load_slab([(watt_d[l][:, 2 * ATT_MAIN:2 * ATT_MAIN + 136], 128, KC, 136)])
        for tt in range(4 if flags.get('att_sub', 99) >= 20 else 0):
            gt = blk * 4 + tt
            p1 = next_ps()
            mm(p1, [(xnT[:, kc, tt * 128:(tt + 1) * 128], wv[:, kc, :]) for kc in range(KC)],
               rkeys=(k3, 'xnT'), out=ps[p1][:, 0:136])
            if flags.get('vw_var', 9) >= 2:
                sc.op('act', lambda e: e.copy(vaug[:, gt, :, 0:64],
                                              ps[p1][:, 0:128].rearrange("p (n d) -> p n d", n=2)),
                      r=('ps%d' % p1,), w=('vaug',))
            if flags.get('vw_var', 9) >= 3:
                sc.op('act', lambda e: e.copy(wi[:, gt, :], ps[p1][:, 128:136]),
                      r=('ps%d' % p1,), w=('wi',))
        if flags.get('att_stage', 9) < 2:
            sc.op('pool', lambda e: e.memset(yaT[:], 0.0), w=('yaT',))
            return
        MX, LO, RNG, MID, CNT, GEH = range(6)
        col = lambda j: bsm[:, j:j + 1]
        for tt in range(4):
            gt = blk * 4 + tt
            nk = (gt + 1) * 128
            qs = slice(tt * 128, (tt + 1) * 128)
            for k0 in range(0, nk, 512):
                n = min(512, nk - k0)
                for hh in range(8):
                    c, bp = hh // 2, 32 * (hh % 2)
                    p1 = next_ps()
                    mm(p1, [(qiT[bp:bp + 32, c, qs], kiT[bp:bp + 32, k0:k0 + n])], rkeys=('qiT', 'kiT'),
                       out=ps[p1][:, 0:n])
                    ri = hh % 2
                    sc.op('act', lambda e: e.activation(rl[ri][:, 0:n], ps[p1][:, 0:n], AF.Relu),
                          r=('ps%d' % p1,), w=('rl%d' % ri,))
                    if hh == 0:
                        sc.op('dve', lambda e: e.tensor_scalar(isc[:, k0:k0 + n], rl[ri][:, 0:n],
                                                               wi[:, gt, 0:1], None, ALU.mult),
                              r=('rl%d' % ri, 'wi'), w=('isc',))
                    else:
                        sc.op('dve', lambda e: e.scalar_tensor_tensor(isc[:, k0:k0 + n], rl[ri][:, 0:n],
                                                                      wi[:, gt, hh:hh + 1], isc[:, k0:k0 + n],
                                                                      ALU.mult, ALU.add),
                              r=('rl%d' % ri, 'wi', 'isc'), w=('isc',))
            if nk > KSEL:
                sc.op('dve', lambda e: e.tensor_reduce(col(MX), isc[:, 0:nk], AX.X, ALU.max,
                                                       apply_absolute_value=True),
                      r=('isc',), w=('bs_mx',))
            sc.op('dve', lambda e: e.tensor_tensor(isc[:, gt * 128:(gt + 1) * 128],
                                                   isc[:, gt * 128:(gt + 1) * 128],
                                                   consts[:, 133:261], ALU.add),
                  r=('isc', 'consts'), w=('isc',))
            if nk <= KSEL:
                sc.op('dve', lambda e: e.memset(col(LO), -1.0e29), w=('bs_lo',))
            else:
                sc.op('dve', lambda e: e.tensor_scalar(col(LO), col(MX), -1.0, -1.0, ALU.mult, ALU.add),
                      r=('bs_mx',), w=('bs_lo',))
                sc.op('dve', lambda e: e.tensor_scalar(col(RNG), col(MX), 2.0, 2.0, ALU.mult, ALU.add),
                      r=('bs_mx',), w=('bs_rng',))
                for it in range(BIS_ITERS):
                    ck = 0.5 ** (it + 1)
                    sc.op('dve', lambda e: e.scalar_tensor_tensor(col(MID), col(RNG), ck, col(LO),
                                                                  ALU.mult, ALU.add),
                          r=('bs_rng', 'bs_lo'), w=('bs_mid',))
                    sc.op('dve', lambda e: e.tensor_scalar(maskb[:, 0:nk], isc[:, 0:nk], col(MID), None,
                                                           ALU.is_ge, ALU.add, accum_out=col(CNT)),
                          r=('isc', 'bs_mid'), w=('maskb', 'bs_cnt'))
                    sc.op('dve', lambda e: e.tensor_scalar(col(GEH), col(CNT), KSEL - 0.5, ck,
                                                           ALU.is_ge, ALU.mult),
                          r=('bs_cnt',), w=('bs_geh',))
                    sc.op('dve', lambda e: e.scalar_tensor_tensor(col(LO), col(GEH), col(RNG), col(LO),
                                                                  ALU.mult, ALU.add),
                          r=('bs_geh', 'bs_rng', 'bs_lo'), w=('bs_lo',))
            sc.op('dve', lambda e: e.tensor_scalar(maskb[:, 0:nk], isc[:, 0:nk], col(LO), None, ALU.is_ge),
                  r=('isc', 'bs_lo'), w=('maskb',))
            for i0 in range(0, gt + 1, 8):
                nb = min(8, gt + 1 - i0)
                pi = rr['pst']
                rr['pst'] = 1 - pi

                def tr(pe, i0=i0, nb=nb, pi=pi):
                    ins = None
                    for j in range(nb):
                        ins = pe.transpose(pst[pi][:, j * 128:(j + 1) * 128],
                                           maskb[:, (i0 + j) * 128:(i0 + j + 1) * 128], ident[:])
                    return ins
                sc.op('pe', tr, r=('maskb', 'ident'), w=('pst%d' % pi,))
                sc.op('act', lambda e: e.copy(maskT[:, i0:i0 + nb, :],
                                              pst[pi][:, 0:nb * 128].rearrange("p (k t) -> p k t", k=nb)),
                      r=('pst%d' % pi,), w=('maskT',))
            if flags.get('att_stage', 9) < 3:
                sc.op('pool', lambda e: e.memset(yaT[:], 0.0), w=('yaT',))
                continue
            A3 = flags.get('a3', 9)
            for n in range(2):
                bp = 64 * n
                pacc = next_ps()
                ps_reserved.add(pacc)
                for i in range(gt + 1):
                    p1 = next_ps()
                    sc.op('pe', lambda pe: pe.matmul(ps[p1][:].rearrange("p (a b) -> p a b", a=4),
                                                     kT[bp:bp + 64, i * 128:(i + 1) * 128],
                                                     qT[bp:bp + 64, :, qs], start=True, stop=True),
                          r=('kT', 'qT'), w=('ps%d' % p1,))
                    pj = i % 2
                    sc.op('act', lambda e: e.activation(pt[pj][:], ps[p1][:], AF.Exp, scale=0.125),
                          r=('ps%d' % p1,), w=('pt%d' % pj,))
                    if A3 >= 2:
                        for a in range(4):
                            sc.op('pool', lambda e: e.tensor_tensor(
                                pt[pj][:, a * 128:(a + 1) * 128], pt[pj][:, a * 128:(a + 1) * 128],
                                maskT[:, i, :], ALU.mult),
                                r=('pt%d' % pj, 'maskT'), w=('pt%d' % pj,))
                    if A3 >= 3:
                        sc.op('pe', lambda pe: pe.matmul(ps[pacc][0:65, :], vaug[:, i, n, 0:65], pt[pj][:],
                                                         start=(i == 0), stop=(i == gt)),
                              r=('vaug', 'pt%d' % pj), w=('ps%d' % pacc,))
                if A3 >= 4:
                    sc.op('dve', lambda e: e.reciprocal(rdn[64:65, :], ps[pacc][64:65, :]),
                          r=('ps%d' % pacc,), w=('rdn',))
                    sc.op('dve', lambda e: e.tensor_copy(rdnb[64:65, :], rdn[64:65, :]),
                          r=('rdn',), w=('rdnb',))
                pb = next_ps()
                if A3 >= 5:
                    sc.op('pe', lambda pe: pe.matmul(ps[pb][0:64, :], onesb[64:65, 0:64], rdnb[64:65, :],
                                                     start=True, stop=True),
                          r=('rdnb', 'onesb'), w=('ps%d' % pb,))
                if A3 >= 6:
                    sc.op('act', lambda e: e.copy(accsb[:], ps[pacc][0:64, :]), r=('ps%d' % pacc,), w=('accsb',))
                ps_reserved.discard(pacc)
                if A3 >= 7:
                    sc.op('dve', lambda e: e.tensor_tensor(
                        yaT[:, n * 4:(n + 1) * 4, qs],
                        accsb[:].rearrange("p (a b) -> p a b", a=4),
                        ps[pb][0:64, :].rearrange("p (a b) -> p a b", a=4), ALU.mult),
                        r=('accsb', 'ps%d' % pb), w=('yaT',))
                else:
                    sc.op('pool', lambda e: e.memset(yaT[:], 0.0), w=('yaT',))


    class _Stop(Exception):
        pass

    def chk(n):
        if flags.get('rw_stage', 99) < n:
            raise _Stop()

    def rwkv_block(sq, l, blk):
        try:
            rwkv_block_(sq, l, blk)
        except _Stop:
            sc.op('pool', lambda e: e.memset(ycT[:], 0.0), w=('ycT',))

    stg_rr = [0]

    def evac2(dst_flat, psrc, pkey_, in1_flat, in1key, op, dkey):
        i = stg_rr[0]
        stg_rr[0] = 1 - i
        st_t = (t1, t2)[i]
        skey = ('t1', 't2')[i]
        stf = st_t[0:64, :, :].rearrange("p a t -> p (a t)")
        sc.op('act', lambda e: e.copy(stf, psrc), r=(pkey_,), w=(skey,))
        sc.op('dve', lambda e: e.tensor_tensor(dst_flat, stf, in1_flat, op), r=(skey, in1key), w=(dkey,))

    def rwkv_block_(sq, l, blk):
        PP = lambda c: ppl[:, c:c + 1]
        sc.op('pool', lambda e: e.tensor_copy(Hbf[:], Hst[:]), r=('Hst',), w=('Hbf',))
        for (tz, kz) in ((Vpad, 'Vpad'), (BHpad, 'BHpad'), (KHpad, 'KHpad'), (Upad, 'Upad')):
            sc.op('pool', lambda e: e.memset(tz[:], 0.0), w=(kz,))
        for s4 in range(4):
            n = 512 if s4 < 3 else 256
            (wv,), wk = load_slab([(win_d[l][:, OFF_RW + s4 * 512:OFF_RW + s4 * 512 + n], 128, KC, n)])
            for m in range(n // 128):
                c = s4 * 4 + m
                pi = next_ps()
                mm(pi, [(wv[:, kc, m * 128:(m + 1) * 128], xnT[:, kc, :]) for kc in range(KC)], rkeys=(wk, 'xnT'))
                sc.op('act', lambda e: e.copy(zT[:, c, 1:513], ps[pi][:]), r=('ps%d' % pi,), w=('zT',))
        sc.op('pool', lambda e: e.tensor_copy(zT[:, :, 0], zprev[:, :]), r=('zprev',), w=('zT',))
        sc.op('pool', lambda e: e.tensor_copy(zprev[:, :], zT[:, :, 512]), r=('zT',), w=('zprev',))
        chk(1)
        for su in range(4):
            s0 = su * 128
            zc = lambda c: zT[:, c, s0 + 1:s0 + 129]
            zp = lambda c: zT[:, c, s0:s0 + 128]

            def shift(dst, dkey, c):
                sc.op('dve', lambda e: e.tensor_tensor(E_[:, 0, :], zp(c), zc(c), ALU.subtract),
                      r=('zT',), w=('E_',))
                sc.op('dve', lambda e: e.scalar_tensor_tensor(dst, E_[:, 0, :], PP(168 + c), zc(c),
                                                              ALU.mult, ALU.add),
                      r=('E_', 'zT', 'ppl'), w=(dkey,))
            for p in range(4):
                shift(r_[:, p, :], 'r_', p)
                shift(k_[:, p, :], 'k_', 4 + p)
                shift(v_[:, p, :], 'v_', 8 + p)
            shift(lraw[:, 0, :], 't2', 12)
            shift(lraw[:, 1, :], 't2', 13)
            sc.op('act', lambda e: e.activation(lwla[0:64, :], lraw[0:64, 0, :], AF.Tanh), r=('t2',), w=('lwla',))
            sc.op('act', lambda e: e.copy(lwla[64:128, :], lraw[64:128, 0, :]), r=('t2',), w=('lwla',))
            sc.op('act', lambda e: e.activation(lgb[:], lraw[:, 1, :], AF.Sigmoid), r=('t2',), w=('lgb',))
            pzw = next_ps()

            def f_zw(pe):
                ins = None
                for p in range(4):
                    ins = pe.matmul(ps[pzw][:, p * 128:(p + 1) * 128], rw_small[0:64, p * 128:(p + 1) * 128],
                                    lwla[0:64, :], start=True, stop=True)
                return ins
            sc.op('pe', f_zw, r=('rw_small', 'lwla'), w=('ps%d' % pzw,))
            for p in range(4):
                sc.op('act', lambda e: e.activation(sg[:, p, :], ps[pzw][:, p * 128:(p + 1) * 128], AF.Sigmoid,
                                                    bias=PP(182 + p), scale=1.0),
                      r=('ps%d' % pzw, 'ppl'), w=('sg',))
            pza = next_ps()

            def f_za(pe):
                ins = None
                for p in range(4):
                    ins = pe.matmul(ps[pza][:, p * 128:(p + 1) * 128],
                                    rw_small[64:128, 512 + p * 128:512 + (p + 1) * 128],
                                    lwla[64:128, :], start=True, stop=True)
                return ins
            sc.op('pe', f_za, r=('rw_small', 'lwla'), w=('ps%d' % pza,))
            for p in range(4):
                sc.op('act', lambda e: e.activation(a_[:, p, :], ps[pza][:, p * 128:(p + 1) * 128], AF.Sigmoid,
                                                    bias=PP(186 + p), scale=1.0),
                      r=('ps%d' % pza, 'ppl'), w=('E_',))
            pg = next_ps()

            def f_g(pe):
                ins = None
                for p in range(4):
                    ins = pe.matmul(ps[pg][:, p * 128:(p + 1) * 128],
                                    rw_small[:, 1024 + p * 128:1024 + (p + 1) * 128],
                                    lgb[:, :], start=True, stop=True)
                return ins
            sc.op('pe', f_g, r=('rw_small', 'lgb'), w=('ps%d' % pg,))
            sc.op('act', lambda e: e.copy(g_[:].rearrange("p a t -> p (a t)"), ps[pg][:]),
                  r=('ps%d' % pg,), w=('g_',))
            chk(2)
            for p in range(4):
                sc.op('dve', lambda e: e.tensor_scalar(kkn[:, p, :], k_[:, p, :], PP(190 + p), None, ALU.mult),
                      r=('k_', 'ppl'), w=('kkn',))
            sc.op('dve', lambda e: e.tensor_tensor(tb[:], kkn[:], kkn[:], ALU.mult), r=('kkn',), w=('tb',))
            pn = next_ps()
            sc.op('pe', lambda pe: pe.matmul(ps[pn][:], bdb[:], tb[:].rearrange("p a t -> p (a t)"),
                                             start=True, stop=True),
                  r=('bdb', 'tb'), w=('ps%d' % pn,))
            sc.op('act', lambda e: e.activation(t2[:].rearrange("p a t -> p (a t)"), ps[pn][:], AF.Sqrt),
                  r=('ps%d' % pn,), w=('t2',))
            sc.op('dve', lambda e: e.tensor_scalar(t2[:], t2[:], 1.0e-12, None, ALU.max), r=('t2',), w=('t2',))
            sc.op('dve', lambda e: e.reciprocal(t2[:], t2[:]), r=('t2',), w=('t2',))
            sc.op('dve', lambda e: e.tensor_tensor(kkn[:], kkn[:], t2[:], ALU.mult), r=('kkn', 't2'), w=('kkn',))
            chk(3)
            for p in range(4):
                sc.op('dve', lambda e: e.tensor_scalar(t1[:, p, :], a_[:, p, :], -1.0, PP(194 + p),
                                                       ALU.add, ALU.mult),
                      r=('E_', 'ppl'), w=('t1',))
            sc.op('dve', lambda e: e.scalar_tensor_tensor(kf_[:], t1[:], 1.0, k_[:], ALU.add, ALU.mult),
                  r=('t1', 'k_'), w=('kf_',))
            sc.op('dve', lambda e: e.tensor_tensor(t1[:], kkn[:], a_[:], ALU.mult), r=('kkn', 'E_'), w=('t1',))
            for p in range(4):
                sc.op('dve', lambda e: e.tensor_tensor_scan(Ls[:, p, :], consts[:, 647:775], sg[:, p, :], 0.0,
                                                            ALU.mult, ALU.add),
                      r=('consts', 'sg'), w=('Ls',))
            sc.op('dve', lambda e: e.tensor_tensor(t2[:], Ls[:], sg[:], ALU.subtract), r=('Ls', 'sg'), w=('t2',))
            sc.op('act', lambda e: e.activation(E_[:], t2[:], AF.Exp, scale=-C0), r=('t2',), w=('E_',))
            sc.op('dve', lambda e: e.scalar_tensor_tensor(AR[:, :, 0, :], kkn[:], -1.0, E_[:], ALU.mult, ALU.mult),
                  r=('kkn', 'E_'), w=('AR',))
            sc.op('act', lambda e: e.activation(E_[:], Ls[:], AF.Exp, scale=-C0), r=('Ls',), w=('E_',))
            sc.op('dve', lambda e: e.tensor_tensor(AR[:, :, 1, :], r_[:], E_[:], ALU.mult), r=('r_', 'E_'), w=('AR',))
            for j in range(2):
                sc.op('dve', lambda e: e.tensor_scalar(ARm[j][:].rearrange("p a b t -> p (a b t)"),
                                                       AR[:].rearrange("p a b t -> p (a b t)"),
                                                       consts[:, 839 + j:840 + j], None, ALU.mult),
                      r=('AR', 'consts'), w=('ARm%d' % j,))
            sc.op('act', lambda e: e.activation(E_[:], Ls[:], AF.Exp, scale=C0), r=('Ls',), w=('E_',))
            sc.op('dve', lambda e: e.tensor_tensor(BT[:], t1[:], E_[:], ALU.mult), r=('t1', 'E_'), w=('BT',))
            sc.op('dve', lambda e: e.tensor_tensor(KT_[:], kf_[:], E_[:], ALU.mult), r=('kf_', 'E_'), w=('KT_',))
            for p in range(4):
                for c in range(2):
                    cs = slice(c * 64, (c + 1) * 64)
                    sc.op('dve', lambda e: e.tensor_scalar(t2[:, p, cs], Ls[:, p, cs],
                                                           Ls[:, p, c * 64 + 63:c * 64 + 64], None, ALU.subtract),
                          r=('Ls',), w=('t2',))
            sc.op('act', lambda e: e.activation(E_[:], t2[:], AF.Exp, scale=C0), r=('t2',), w=('E_',))
            sc.op('dve', lambda e: e.tensor_tensor(BH[:], t1[:], E_[:], ALU.mult), r=('t1', 'E_'), w=('BH',))
            sc.op('dve', lambda e: e.tensor_tensor(KH[:], kf_[:], E_[:], ALU.mult), r=('kf_', 'E_'), w=('KH',))
            sc.op('act', lambda e: e.activation(wc[:], Ls[:].rearrange("p a (c t) -> p a c t", c=2)[:, :, :, 63],
                                                AF.Exp, scale=-C0),
                  r=('Ls',), w=('wc',))
            chk(4)
            sc.op('dve', lambda e: e.tensor_tensor(t2[:], r_[:], kf_[:], ALU.mult), r=('r_', 'kf_'), w=('t2',))
            for p in range(4):
                sc.op('dve', lambda e: e.tensor_scalar(tb[:, p, :], t2[:, p, :], PP(198 + p), None, ALU.mult),
                      r=('t2', 'ppl'), w=('tb',))
            chk(4.1)
            pbn = next_ps()
            sc.op('pe', lambda pe: pe.matmul(ps[pbn][:], bdb[:], tb[:].rearrange("p a t -> p (a t)"),
                                             start=True, stop=True),
                  r=('bdb', 'tb'), w=('ps%d' % pbn,))
            chk(4.2)
            sc.op('dve', lambda e: e.tensor_tensor(E_[:].rearrange("p a t -> p (a t)"), ps[pbn][:],
                                                   v_[:].rearrange("p a t -> p (a t)"), ALU.mult),
                  r=('ps%d' % pbn, 'v_'), w=('E_',))
            chk(4.3)
            sc.op('act', lambda e: e.copy(VT[:], v_[:]), r=('v_',), w=('VT',))
            chk(4.4)
            chk(5)
            for c in range(2):
                cs = slice(c * 64, (c + 1) * 64)
                hp = lambda hh: (hh // 2, 64 * (hh % 2))
                for (src, skey, dst, dkey) in ((VT, 'VT', Vpad, 'Vpad'), (BH, 'BH', BHpad, 'BHpad'),
                                               (KH, 'KH', KHpad, 'KHpad')):
                    pi = next_ps()

                    def trf(pe, src=src, pi=pi, c=c):
                        ins = None
                        for p in range(4):
                            ins = pe.matmul(ps[pi][0:64, p * 128:(p + 1) * 128],
                                            src[:, p, c * 64:(c + 1) * 64], ident[:], start=True, stop=True)
                        return ins
                    sc.op('pe', trf, r=(skey, 'ident'), w=('ps%d' % pi,))
                    for j in range(2):
                        sc.op('dve', lambda e: e.tensor_copy(
                            dst[:, j::2, j * 64:(j + 1) * 64],
                            ps[pi][0:64, :].rearrange("p (a b) -> p a b", a=4)[:, :, j * 64:(j + 1) * 64]),
                            r=('ps%d' % pi,), w=(dkey,))
                pX = next_ps()

                def f_x(pe):
                    ins = None
                    for hh in range(0, 8, 2 if flags.get('rwx', 0) == 5 else 1):
                        p, bp = hp(hh)
                        ins = pe.matmul(ps[pX][0:64, hh * 64:(hh + 1) * 64], ARm[hh % 2][:, p, 0, cs],
                                        BT[:, p, cs], start=True, stop=True)
                    return ins
                RWX = flags.get('rwx', 0)
                if RWX != 1:
                    sc.op('pe', f_x, r=('ARm0', 'ARm1', 'BT'), w=('ps%d' % pX,))
                if RWX not in (1, 3):
                    evac2(Pm[0][:].rearrange("p h t -> p (h t)"), ps[pX][0:64, :], 'ps%d' % pX,
                          MSL8[:].rearrange("p h t -> p (h t)"), 'MSL8', ALU.mult, 'Pm0')
                for (lh, lkey, dstA, dkey) in ((BT, 'BT', ATb, 'ATb'), (KT_, 'KT_', ATk, 'ATk')):
                    for half in range(2):
                        pY = next_ps()

                        def f_y(pe, lh=lh, half=half, pY=pY):
                            ins = None
                            for q4 in range(4):
                                hh = half * 4 + q4
                                p, bp = hp(hh)
                                ins = pe.matmul(ps[pY][0:64, q4 * 128:(q4 + 1) * 128].rearrange(
                                    "p (a b) -> p a b", a=2), lh[:, p, cs], ARm[hh % 2][:, p, :, cs],
                                    start=True, stop=True)
                            return ins
                        if RWX in (2, 5):
                            continue
                        sc.op('pe', f_y, r=(lkey, 'ARm0', 'ARm1'), w=('ps%d' % pY,))
                        if RWX == 4:
                            continue
                        evac2(dstA[:, half * 4:(half + 1) * 4, :, :].rearrange("p h a t -> p (h a t)"),
                              ps[pY][0:64, :], 'ps%d' % pY,
                              MAT[:, half * 4:(half + 1) * 4, :, :].rearrange("p h a t -> p (h a t)"), 'MAT',
                              ALU.mult, dkey)
                chk(6)
                sc.op('dve', lambda e: e.tensor_tensor(STm[0][:], ATb[:, :, 0, :], I8[:], ALU.add),
                      r=('ATb', 'I8'), w=('STm0',))
                Pc = lambda hh: Pm[0][:, hh, :]
                Qc = lambda hh: ATb[:, hh, 0, :]
                pkey, qkey = 'Pm0', 'ATb'
                si = 0
                pq = 0
                for rd in range(1, 7):
                    do_sq = rd <= 5
                    do_s = rd >= 2
                    if do_sq:
                        pP = next_ps()
                        pQ = next_ps()

                        def f_p(pe, Pc=Pc, Qc=Qc, pP=pP):
                            ins = None
                            for hh in range(8):
                                ins = pe.matmul(ps[pP][0:64, hh * 64:(hh + 1) * 64], Qc(hh), Pc(hh),
                                                start=True, stop=True)
                            return ins

                        def f_q(pe, Pc=Pc, Qc=Qc, pQ=pQ):
                            ins = None
                            for hh in range(8):
                                ins = pe.matmul(ps[pQ][0:64, hh * 64:(hh + 1) * 64], Pc(hh), Qc(hh),
                                                start=True, stop=True)
                            return ins
                        sc.op('pe', f_p, r=(pkey, qkey), w=('ps%d' % pP,))
                        sc.op('pe', f_q, r=(pkey, qkey), w=('ps%d' % pQ,))
                    if do_s:
                        pS = next_ps()

                        def f_s(pe, Pc=Pc, si=si, pS=pS):
                            ins = None
                            for hh in range(8):
                                ins = pe.matmul(ps[pS][0:64, hh * 64:(hh + 1) * 64], Pc(hh), STm[si][:, hh, :],
                                                start=True, stop=True)
                            return ins
                        sc.op('pe', f_s, r=(pkey, 'STm%d' % si), w=('ps%d' % pS,))
                        evac2(STm[1 - si][:].rearrange("p h t -> p (h t)"), ps[pS][0:64, :], 'ps%d' % pS,
                              STm[si][:].rearrange("p h t -> p (h t)"), 'STm%d' % si, ALU.add,
                              'STm%d' % (1 - si))
                        si = 1 - si
                    if do_sq:
                        nx = 1 - pq if rd > 1 else 1
                        sc.op('dve', lambda e: e.tensor_copy(Pm[nx][:].rearrange("p h t -> p (h t)"), ps[pP][0:64, :]),
                              r=('ps%d' % pP,), w=('Pm%d' % nx,))
                        sc.op('dve', lambda e: e.tensor_copy(Qm[nx][:].rearrange("p h t -> p (h t)"), ps[pQ][0:64, :]),
                              r=('ps%d' % pQ,), w=('Qm%d' % nx,))
                        Pc = lambda hh, nx=nx: Pm[nx][:, hh, :]
                        Qc = lambda hh, nx=nx: Qm[nx][:, hh, :]
                        pkey, qkey = 'Pm%d' % nx, 'Qm%d' % nx
                        pq = nx
                chk(7)
                pR = next_ps()

                def f_r(pe):
                    ins = None
                    for hh in range(8):
                        p, bp = hp(hh)
                        o = ps[pR][0:64, hh * 64:(hh + 1) * 64]
                        j = hh % 2
                        pe.matmul(o, AR[:, p, 0, cs], Hbf[:, p, j * 64:(j + 1) * 64], start=True, stop=False)
                        ins = pe.matmul(o, ATk[:, hh, 0, :], Vpad[:, hh, j * 64:(j + 1) * 64],
                                        start=False, stop=True)
                    return ins
                sc.op('pe', f_r, r=('AR', 'Hbf', 'ATk', 'Vpad'), w=('ps%d' % pR,))
                sc.op('dve', lambda e: e.tensor_copy(R0[:].rearrange("p h t -> p (h t)"), ps[pR][0:64, :]),
                      r=('ps%d' % pR,), w=('R0',))
                pU = next_ps()

                def f_u(pe):
                    ins = None
                    for hh in range(8):
                        ins = pe.matmul(ps[pU][0:64, hh * 64:(hh + 1) * 64], STm[si][:, hh, :], R0[:, hh, :],
                                        start=True, stop=True)
                    return ins
                sc.op('pe', f_u, r=('STm%d' % si, 'R0'), w=('ps%d' % pU,))
                for j in range(2):
                    sc.op('dve', lambda e: e.tensor_copy(
                        Upad[:, j::2, j * 64:(j + 1) * 64],
                        ps[pU][0:64, :].rearrange("p (a b t) -> p a b t", a=4, b=2)[:, :, j, :]),
                        r=('ps%d' % pU,), w=('Upad',))
                chk(8)
                pYo = next_ps()

                def f_yo(pe):
                    ins = None
                    for p in range(4):
                        o = ps[pYo][:, p * 64:(p + 1) * 64]
                        pe.matmul(o, Hbf[:, p, :], AR[:, p, 1, cs], start=True, stop=False)
                        for j in range(2):
                            hh = 2 * p + j
                            pe.matmul(o, Upad[:, hh, :], ATb[:, hh, 1, :], start=False, stop=False)
                            ins = pe.matmul(o, Vpad[:, hh, :], ATk[:, hh, 1, :], start=False, stop=(j == 1))
                    return ins
                sc.op('pe', f_yo, r=('Hbf', 'AR', 'Upad', 'ATb', 'Vpad', 'ATk'), w=('ps%d' % pYo,))
                sc.op('act', lambda e: e.copy(YT[:, :, cs], ps[pYo][:, 0:256].rearrange("p (a t) -> p a t", a=4)),
                      r=('ps%d' % pYo,), w=('sg',))
                pH = next_ps()

                def f_h(pe):
                    ins = None
                    for hh in range(8):
                        p, j = hh // 2, hh % 2
                        o = ps[pH][:, p * 128 + j * 64:p * 128 + (j + 1) * 64]
                        pe.matmul(o, BHpad[:, hh, :], Upad[:, hh, j * 64:(j + 1) * 64], start=True, stop=False)
                        ins = pe.matmul(o, KHpad[:, hh, :], Vpad[:, hh, j * 64:(j + 1) * 64],
                                        start=False, stop=True)
                    return ins
                sc.op('pe', f_h, r=('BHpad', 'Upad', 'KHpad', 'Vpad'), w=('ps%d' % pH,))
                for p in range(4):
                    sc.op('dve', lambda e: e.scalar_tensor_tensor(Hst[:, p, :], Hst[:, p, :], wc[:, p, c:c + 1],
                                                                  ps[pH][:, p * 128:(p + 1) * 128],
                                                                  ALU.mult, ALU.add),
                          r=('Hst', 'wc', 'ps%d' % pH), w=('Hst',))
                sc.op('pool', lambda e: e.tensor_copy(Hbf[:], Hst[:]), r=('Hst',), w=('Hbf',))
            chk(9)
            F = lambda t: t[:].rearrange("p a t -> p (a t)")
            sc.op('act', lambda e: e.copy(tb[:], YT[:]), r=('sg',), w=('tb',))
            p1 = next_ps()
            sc.op('pe', lambda pe: pe.matmul(ps[p1][:], bdb[:], F(tb), start=True, stop=True),
                  r=('bdb', 'tb'), w=('ps%d' % p1,))
            sc.op('dve', lambda e: e.scalar_tensor_tensor(F(t1), ps[p1][:], -1.0 / 64, F(YT), ALU.mult, ALU.add),
                  r=('ps%d' % p1, 'sg'), w=('t1',))
            sc.op('dve', lambda e: e.tensor_tensor(tb[:], t1[:], t1[:], ALU.mult), r=('t1',), w=('tb',))
            p2 = next_ps()
            sc.op('pe', lambda pe: pe.matmul(ps[p2][:], bdb[:], F(tb), start=True, stop=True),
                  r=('bdb', 'tb'), w=('ps%d' % p2,))
            sc.op('act', lambda e: e.activation(F(t2), ps[p2][:], AF.Sqrt, bias=consts[:, 262:263], scale=1.0 / 64),
                  r=('ps%d' % p2, 'consts'), w=('t2',))
            sc.op('dve', lambda e: e.reciprocal(t2[:], t2[:]), r=('t2',), w=('t2',))
            sc.op('dve', lambda e: e.tensor_tensor(t1[:], t1[:], t2[:], ALU.mult), r=('t1', 't2'), w=('t1',))
            for p in range(4):
                sc.op('dve', lambda e: e.tensor_scalar(t1[:, p, :], t1[:, p, :], PP(202 + p), PP(206 + p),
                                                       ALU.mult, ALU.add),
                      r=('t1', 'ppl'), w=('t1',))
            sc.op('dve', lambda e: e.tensor_tensor(t1[:], t1[:], E_[:], ALU.add), r=('t1', 'E_'), w=('t1',))
            sc.op('dve', lambda e: e.tensor_tensor(ycT[:, :, s0:s0 + 128], t1[:], g_[:], ALU.mult),
                  r=('t1', 'g_'), w=('ycT',))

    for sq in range(NSEQ):
        for gt in range(NT):
            sc.dma('sp', h[:, gt, :], x_d[sq, gt * 128:(gt + 1) * 128, :], w=(('h', gt),))
        sc.op('pool', lambda e: e.memset(vaug[:], 1.0), w=('vaug',))
        for l in range(L):
            sc.dma('pool', gmix[:], norms_d[2 * l:2 * l + 1, :].broadcast_to([128, D]), w=('gmix',))
            sc.dma('pool', gffn[:], norms_d[2 * l + 1:2 * l + 2, :].broadcast_to([128, D]), w=('gffn',))
            sc.dma('sp', ppl[:], ppl_d[l], w=('ppl',))
            sc.dma('pool', rw_small[:], rww_d[l], w=('rw_small',))
            sc.op('pool', lambda e: e.memset(Hst[:], 0.0), w=('Hst',))
            sc.op('pool', lambda e: e.memset(zprev[:], 0.0), w=('zprev',))
            sc.op('pool', lambda e: e.memset(cu_halo[:], 0.0), w=('cu_halo',))
            sc.op('pool', lambda e: e.memset(f_halo[:], 0.0), w=('f_halo',))
            for blk in range(NB):
                t0 = blk * 512
                rmsnorm_to_xnT(blk, gmix, 'gmix', 0)
                if do_attn:
                    sc.barrier()
                    attention_block(sq, l, blk)
                if do_rwkv:
                    sc.barrier()
                    rwkv_block(sq, l, blk)
                sc.barrier()
                for grp in range(3):
                    (wv,), wk = load_slab([(win_d[l][:, OFF_SC + grp * 512:OFF_SC + (grp + 1) * 512],
                                            128, KC, 512)])
                    for m in range(4):
                        pi = next_ps()
                        mm(pi, [(wv[:, kc, m * 128:(m + 1) * 128], xnT[:, kc, :]) for kc in range(KC)],
                           rkeys=(wk, 'xnT'))
                        ch = grp * 4 + m
                        sc.op('act', lambda e: e.copy(scT[:, ch, 2:514], ps[pi][:]),
                              r=('ps%d' % pi,), w=(('scT', ch),))
                for m in range(4):
                    sc.op('pool', lambda e: e.tensor_copy(cuT[:, m, 0:2], cu_halo[:, m, :]),
                          r=('cu_halo',), w=(('cuT', m),))
                    sc.op('dve', lambda e: e.tensor_tensor(cuT[:, m, 2:514], scT[:, 8 + m, 2:514],
                                                           scT[:, m, 2:514], ALU.mult),
                          r=(('scT', 8 + m), ('scT', m)), w=(('cuT', m),))
                    sc.op('pool', lambda e: e.tensor_copy(cu_halo[:, m, :], cuT[:, m, 512:514]),
                          r=(('cuT', m),), w=('cu_halo',))
                    cw = lambda j: ppl[:, 24 + m * 3 + j:24 + m * 3 + j + 1]
                    sc.op('dve', lambda e: e.tensor_scalar(macc[:], cuT[:, m, 0:512], cw(0), None, ALU.mult),
                          r=(('cuT', m), 'ppl'), w=('macc',))
                    sc.op('dve', lambda e: e.scalar_tensor_tensor(macc[:], cuT[:, m, 1:513], cw(1), macc[:],
                                                                  ALU.mult, ALU.add),
                          r=(('cuT', m), 'ppl', 'macc'), w=('macc',))
                    sc.op('dve', lambda e: e.scalar_tensor_tensor(macc[:], cuT[:, m, 2:514], cw(2), macc[:],
                                                                  ALU.mult, ALU.add),
                          r=(('cuT', m), 'ppl', 'macc'), w=('macc',))
                    sc.op('dve', lambda e: e.tensor_tensor(ybT[:, m, :], macc[:], scT[:, 4 + m, 2:514], ALU.mult),
                          r=('macc', ('scT', 4 + m)), w=('ybT',))
                if not do_attn:
                    sc.op('pool', lambda e: e.memset(yaT[:], 0.0), w=('yaT',))
                if not do_rwkv:
                    sc.op('pool', lambda e: e.memset(ycT[:], 0.0), w=('ycT',))
                for m in range(KC):
                    gparts = [(win_d[l][:, OFF_GATE + g * D + m * 128:OFF_GATE + g * D + (m + 1) * 128],
                               128, KC, 128) for g in range(3)]
                    gv, gk = load_slab(gparts)
                    bparts = [(wbr_d[l, 0][:, m * 128:(m + 1) * 128], 64, 8, 128),
                              (wbr_d[l, 1][:, m * 128:(m + 1) * 128], 128, 4, 128),
                              (wbr_d[l, 2][:, m * 128:(m + 1) * 128], 128, 4, 128)]
                    bv, bk = load_slab(bparts)
                    for g in range(3):
                        pg = next_ps()
                        mm(pg, [(gv[g][:, kc, :], xnT[:, kc, :]) for kc in range(KC)], rkeys=(gk, 'xnT'))
                        gi = g % 2
                        sc.op('act', lambda e: e.activation(gsig[gi][:], ps[pg][:], AF.Sigmoid,
                                                            bias=ppl[:, g * 8 + m:g * 8 + m + 1], scale=1.0),
                              r=('ps%d' % pg, 'ppl'), w=('gsig%d' % gi,))
                        pu = next_ps()
                        if g == 0:
                            prs = [(bv[0][:, hh, :], yaT[:, hh, :]) for hh in range(8)]
                            rk = (bk, 'yaT')
                        elif g == 1:
                            prs = [(bv[1][:, c4, :], ybT[:, c4, :]) for c4 in range(4)]
                            rk = (bk, 'ybT')
                        else:
                            prs = [(bv[2][:, c4, :], ycT[:, c4, :]) for c4 in range(4)]
                            rk = (bk, 'ycT')
                        mm(pu, prs, rkeys=rk)
                        if g == 0:
                            sc.op('dve', lambda e: e.tensor_tensor(macc[:], gsig[gi][:], ps[pu][:], ALU.mult),
                                  r=('gsig%d' % gi, 'ps%d' % pu), w=('macc',))
                        else:
                            sc.op('dve', lambda e: e.tensor_tensor(mtmp[:], gsig[gi][:], ps[pu][:], ALU.mult),
                                  r=('gsig%d' % gi, 'ps%d' % pu), w=('mtmp',))
                            dst = macc[:] if g == 1 else mixT[:, m, :]
                            dk = 'macc' if g == 1 else 'mixT'
                            sc.op('pool', lambda e: e.tensor_tensor(dst, macc[:], mtmp[:], ALU.add),
                                  r=('macc', 'mtmp'), w=(dk,))
                for half in range(2):
                    (wv,), wk = load_slab([(wout_d[l][:, half * 512:(half + 1) * 512], 128, KC, 512)])
                    for tt in range(4):
                        gt = blk * 4 + tt
                        pi = next_ps()
                        mm(pi, [(mixT[:, kc, tt * 128:(tt + 1) * 128], wv[:, kc, :]) for kc in range(KC)],
                           rkeys=(wk, 'mixT'))
                        sc.op('dve', lambda e: e.tensor_tensor(h[:, gt, half * 512:(half + 1) * 512],
                                                               h[:, gt, half * 512:(half + 1) * 512],
                                                               ps[pi][:], ALU.add),
                              r=(('h', gt), 'ps%d' % pi), w=(('h', gt),))
                sc.barrier()
                if flags.get('skip_ffn', False):
                    continue
                rmsnorm_to_xnT(blk, gffn, 'gffn', 1)
                for f in range(NFF):
                    wparts = [(fup_d[l][:, f * 128:(f + 1) * 128], 128, KC, 128),
                              (fup_d[l][:, DFF + f * 128:DFF + (f + 1) * 128], 128, KC, 128)]
                    wv, wk = load_slab(wparts)
                    bi = f % 2
                    for which in range(2):
                        pi = next_ps()
                        mm(pi, [(wv[which][:, kc, :], xnT[:, kc, :]) for kc in range(KC)], rkeys=(wk, 'xnT'))
                        buf = (fg if which == 0 else fu)[bi]
                        bkey = ('fg%d' if which == 0 else 'fu%d') % bi
                        cbuf = (fgc if which == 0 else fuc)[bi]
                        ckey = 'fgc0' if which == 0 else 'fuc0'
                        hc = which * NFF + f
                        sc.op('act', lambda e: e.copy(buf[:, 2:514], ps[pi][:]), r=('ps%d' % pi,), w=(bkey,))
                        sc.op('pool', lambda e: e.tensor_copy(buf[:, 0:2], f_halo[:, hc, :]),
                              r=(('f_halo', hc),), w=(bkey,))
                        sc.op('pool', lambda e: e.tensor_copy(f_halo[:, hc, :], buf[:, 512:514]),
                              r=(bkey,), w=(('f_halo', hc),))
                        eng = 'dve'
                        cw = lambda j: ppl[:, 36 + hc * 3 + j:36 + hc * 3 + j + 1]
                        sc.op(eng, lambda e: e.tensor_scalar(cbuf[:], buf[:, 0:512], cw(0), None, ALU.mult),
                              r=(bkey, 'ppl'), w=(ckey,))
                        sc.op(eng, lambda e: e.scalar_tensor_tensor(cbuf[:], buf[:, 1:513], cw(1), cbuf[:],
                                                                    ALU.mult, ALU.add),
                              r=(bkey, 'ppl', ckey), w=(ckey,))
                        sc.op(eng, lambda e: e.scalar_tensor_tensor(cbuf[:], buf[:, 2:514], cw(2), cbuf[:],
                                                                    ALU.mult, ALU.add),
                              r=(bkey, 'ppl', ckey), w=(ckey,))
                    sc.op('act', lambda e: e.activation(fsl[bi][:], fgc[bi][:], AF.Silu),
                          r=('fgc0',), w=('fgc0',))
                    sc.op('dve', lambda e: e.tensor_tensor(actT[:, f, :], fsl[bi][:], fuc[bi][:], ALU.mult),
                          r=('fgc0', 'fuc0'), w=(('actT', f),))
                for half in range(2):
                    pis = [next_ps() for _ in range(4)]
                    groups = [(0, 8), (8, 16), (16, 22)]
                    for gi_, (f0, f1) in enumerate(groups):
                        (wv,), wk = load_slab([(fdn_d[l][f0 * 128:f1 * 128, half * 512:(half + 1) * 512],
                                                128, f1 - f0, 512)])
                        for tt in range(4):
                            def fn(pe, tt=tt, wv=wv, f0=f0, f1=f1):
                                ins = None
                                for f in range(f0, f1):
                                    ins = pe.matmul(ps[pis[tt]][:], actT[:, f, tt * 128:(tt + 1) * 128],
                                                    wv[:, f - f0, :], start=(f == 0), stop=(f == NFF - 1))
                                return ins
                            rk = (wk,) + tuple(('actT', f) for f in range(f0, f1))
                            if gi_ == 0:
                                sc.op('pe', fn, r=rk, w=('ps%d' % pis[tt],))
                            else:
                                sc.op('pe', fn, r=rk + ('ps%d' % pis[tt],), w=('ps%d' % pis[tt],))
                    for tt in range(4):
                        gt = blk * 4 + tt
                        sc.op('dve', lambda e: e.tensor_tensor(h[:, gt, half * 512:(half + 1) * 512],
                                                               h[:, gt, half * 512:(half + 1) * 512],
                                                               ps[pis[tt]][:], ALU.add),
                              r=(('h', gt), 'ps%d' % pis[tt]), w=(('h', gt),))
        sc.barrier()
        sc.dma('pool', gmix[:], norms_d[2 * L:2 * L + 1, :].broadcast_to([128, D]), w=('gmix',))
        for gt in range(NT):
            col = gt
            sc.op('act', lambda e: e.activation(junk[:], h[:, gt, :], AF.Square,
                                                accum_out=ss[:, col:col + 1]),
                  r=(('h', gt),), w=('junk', ('ss', col)))
            sc.op('act', lambda e: e.activation(rstd[:, col:col + 1], ss[:, col:col + 1], AF.Sqrt,
                                                bias=consts[:, 261:262], scale=1.0 / D),
                  r=(('ss', col), 'consts'), w=(('rstd', col),))
            sc.op('dve', lambda e: e.reciprocal(rstd[:, col:col + 1], rstd[:, col:col + 1]),
                  r=(('rstd', col),), w=(('rstd', col),))
            oi = gt % 2
            sc.op('dve', lambda e: e.scalar_tensor_tensor(outb[oi][:], h[:, gt, :],
                                                          rstd[:, col:col + 1], gmix[:],
                                                          ALU.mult, ALU.mult),
                  r=(('h', gt), ('rstd', col), 'gmix'), w=('outb%d' % oi,))
            sc.dma('sp', y_d[sq, gt * 128:(gt + 1) * 128, :], outb[oi][:], r=('outb%d' % oi,), w=(('y', sq, gt),))
    sc.finish([('y', sq, gt) for sq in range(NSEQ) for gt in range(NT)])
    print('instructions emitted:', sc.nins, sc.cnt, flush=True)
    return nc


def _swap_idx(dh):
    rot = dh // 4
    half = rot // 2
    idx = np.arange(dh)
    idx[:half] = np.arange(half) + half
    idx[half:rot] = np.arange(half)
    return idx


def _att_cols():
    main = []
    swap = []
    s64 = _swap_idx(64)
    s32 = _swap_idx(32)
    for i in range(4):
        for hh in (i, 4 + i):
            main += [OFF_Q + hh * 64 + d for d in range(64)]
            swap += [OFF_Q + hh * 64 + int(s64[d]) for d in range(64)]
    for hh in range(2):
        main += [OFF_K + hh * 64 + d for d in range(64)]
        swap += [OFF_K + hh * 64 + int(s64[d]) for d in range(64)]
    for hh in range(8):
        main += [OFF_QI + hh * 32 + d for d in range(32)]
        swap += [OFF_QI + hh * 32 + int(s32[d]) for d in range(32)]
    for rep in range(2):
        main += [OFF_KI + d for d in range(32)]
        swap += [OFF_KI + int(s32[d]) for d in range(32)]
    vw = [OFF_V + d for d in range(128)] + [OFF_WI + d for d in range(8)]
    assert len(main) == ATT_MAIN and len(swap) == ATT_MAIN
    return np.array(main + swap + vw)


def _consts():
    c = np.zeros((128, 1024), np.float32)
    c[:, 0:128] = np.eye(128, dtype=np.float32)
    p = np.arange(128)
    theta = 500000.0
    d = p % 64
    c[:, 128] = np.where(d < 16, theta ** (-(d % 8) * 2.0 / 16), 0.0)
    c[:, 129] = np.where(d < 8, -1.0, np.where(d < 16, 1.0, 0.0))
    d = p % 32
    c[:, 130] = np.where(d < 8, theta ** (-(d % 4) * 2.0 / 8), 0.0)
    c[:, 131] = np.where(d < 4, -1.0, np.where(d < 8, 1.0, 0.0))
    c[:, 132] = -math.pi
    c[:, 261] = RMS_EPS
    c[:, 263:327] = 1.0
    c[0:64, 327:391] = np.triu(np.ones((64, 64), np.float32), 1)
    c[0:64, 391:455] = np.triu(np.ones((64, 64), np.float32), 0)
    c[0:64, 455:519] = np.tril(np.ones((64, 64), np.float32), -1)
    c[0:64, 519:583] = 1.0
    c[64:128, 583:647] = 1.0
    c[:, 647:775] = 1.0
    c[:, 647] = 0.0
    c[:, 711] = 0.0
    c[0:64, 775:839] = np.eye(64, dtype=np.float32)
    c[0:64, 839] = 1.0
    c[64:128, 840] = 1.0
    c[:, 262] = 64e-5
    t = np.arange(128)[:, None]
    s_ = np.arange(128)[None, :]
    c[:, 133:261] = np.where(s_ <= t, 0.0, NEG)
    return c


def _prep(inputs, L, ncores):
    f = lambda k: np.asarray(inputs[k], dtype=np.float32)
    w_in = f('w_in')[:L]
    w_att = np.ascontiguousarray(w_in[:, :, _att_cols()])
    norms = np.concatenate([np.stack([f('norm_mix')[l], f('norm_ffn')[l]]) for l in range(L)]
                           + [f('norm_final')[None]], axis=0)
    pp = np.zeros((L, 128, 256), np.float32)
    bg = f('b_gate')[:L].reshape(L, 3, 8, 128)
    pp[:, :, 0:24] = bg.transpose(0, 3, 1, 2).reshape(L, 128, 24)
    scv = f('sc_conv')[:L].reshape(L, 3, 4, 128)
    pp[:, :, 24:36] = scv.transpose(0, 3, 2, 1).reshape(L, 128, 12)
    fc = f('ffn_conv')[:L].reshape(L, 3, 2, NFF, 128)
    pp[:, :, 36:168] = fc.transpose(0, 4, 2, 3, 1).reshape(L, 128, 132)
    mu = f('rw_mu')[:L]
    pp[:, :, 168:180] = mu[:, 0:1536].reshape(L, 12, 128).transpose(0, 2, 1)
    pp[:, :, 180] = mu[:, 1536:1664]
    pp[:, :, 181] = mu[:, 1664:1792]
    def pair4(v):
        return v.reshape(L, 4, 128).transpose(0, 2, 1)
    pp[:, :, 182:186] = pair4(f('rw_w0')[:L])
    pp[:, :, 186:190] = pair4(f('rw_a0')[:L])
    pp[:, :, 190:194] = pair4(f('rw_k_k')[:L])
    pp[:, :, 194:198] = pair4(f('rw_k_a')[:L])
    pp[:, :, 198:202] = pair4(f('rw_r_k')[:L].reshape(L, 512))
    pp[:, :, 202:206] = pair4(f('rw_ln_w')[:L])
    pp[:, :, 206:210] = pair4(f('rw_ln_b')[:L])
    rws = np.zeros((L, 128, 1536), np.float32)
    rws[:, 0:64, 0:512] = f('rw_w_up')[:L]
    rws[:, 64:128, 512:1024] = f('rw_a_up')[:L]
    rws[:, :, 1024:1536] = f('rw_g_up')[:L]
    shared = {
        'w_att': w_att, 'w_in': np.ascontiguousarray(w_in),
        'w_branch': np.ascontiguousarray(f('w_branch')[:L]),
        'w_out': np.ascontiguousarray(f('w_out')[:L]),
        'ffn_up': np.ascontiguousarray(f('ffn_up')[:L]),
        'ffn_down': np.ascontiguousarray(f('ffn_down')[:L]),
        'norms': np.ascontiguousarray(norms), 'pp_layer': pp, 'consts': _consts(), 'rw_small': rws,
    }
    x = f('x')
    pos = np.asarray(inputs['positions']).astype(np.int32)
    B = x.shape[0]
    per = B // ncores
    maps = []
    for c in range(ncores):
        m = dict(shared)
        m['x'] = np.ascontiguousarray(x[c * per:(c + 1) * per])
        m['positions'] = np.ascontiguousarray(pos[c * per:(c + 1) * per])
        maps.append(m)
    return maps, per


def run(inputs, L=4, ncores=8, flags=None, trace=False):
    flags = flags or {}
    maps, per = _prep(inputs, L, ncores)
    S = maps[0]['x'].shape[1]
    nc = build_program(S, L, per, flags)
    res = run_bass_kernel_spmd(nc, maps, core_ids=list(range(ncores)), trace=trace)
    out = np.concatenate([r['y'] for r in res.results], axis=0)
    return out, res


def kernel(**inputs):
    out, _ = run(inputs, L=4, ncores=8)
    return out.astype(np.float32)
```

```python
import math
import numpy as np
import concourse.bass as bass
import concourse.mybir as mybir
from concourse.bass_utils import run_bass_kernel_spmd

F32 = mybir.dt.float32
BF16 = mybir.dt.bfloat16
I32 = mybir.dt.int32
AF = mybir.ActivationFunctionType
ALU = mybir.AluOpType
AX = mybir.AxisListType

D = 1024
KC = 8
NIN = 7464
DFF = 2816
NFF = 22
OFF_Q, OFF_K, OFF_V, OFF_QI, OFF_KI, OFF_WI = 0, 512, 640, 768, 1024, 1056
OFF_SC, OFF_RW, OFF_GATE = 1064, 2600, 4392
ATT_MAIN = 960
ATT_COLS = 2 * ATT_MAIN + 136
RMS_EPS = 1e-6
C0 = math.exp(-0.5)
NEG = -1.0e30
BIS_ITERS = 12
STRICT = False


class Sched:
    def __init__(self, nc, n_dma_sems=32):
        if STRICT:
            n_dma_sems = 8
        self.nc = nc
        self.nc = nc
        self.E = {'pe': nc.tensor, 'act': nc.scalar, 'dve': nc.vector,
                  'pool': nc.gpsimd, 'sp': nc.sync}
        self.sem = {k: nc.alloc_semaphore('sem_' + k) for k in ('pe', 'act', 'dve', 'pool')}
        self.cnt = {k: 0 for k in self.sem}
        self.dsem = [nc.alloc_semaphore('dsem%d' % i) for i in range(n_dma_sems)]
        self.dval = [0] * n_dma_sems
        self.nrot = n_dma_sems
        self.drr = 0
        self.known = {k: {} for k in self.E}
        self.lastw = {}
        self.readers = {}
        self.nins = 0
        self.strict = STRICT
        self.strict_dma = STRICT

    def _semh(self, sk):
        return self.sem[sk] if isinstance(sk, str) else self.dsem[sk]

    def _wait(self, e, sk, val):
        if self.known[e].get(sk, 0) >= val:
            return
        self.E[e].wait_ge(self._semh(sk), val)
        self.known[e][sk] = val
        self.nins += 1

    def _deps(self, e, r, w):
        need = {}

        def add(sk, v):
            if need.get(sk, 0) < v:
                need[sk] = v
        for k in r:
            if k in self.lastw:
                add(*self.lastw[k])
        for k in w:
            if k in self.lastw:
                sk, v = self.lastw[k]
                if sk != e or self.strict:
                    add(sk, v)
            for sk, v in self.readers.get(k, {}).items():
                if sk != e or self.strict:
                    add(sk, v)
        for sk, v in need.items():
            self._wait(e, sk, v)

    def _commit(self, ev, r, w):
        for k in w:
            self.lastw[k] = ev
            self.readers[k] = {}
        for k in r:
            d = self.readers.setdefault(k, {})
            if d.get(ev[0], 0) < ev[1]:
                d[ev[0]] = ev[1]

    def op(self, e, fn, r=(), w=()):
        self._deps(e, r, w)
        ins = fn(self.E[e])
        self.cnt[e] += 1
        ins.then_inc(self.sem[e], 1)
        self.nins += 1
        self._commit((e, self.cnt[e]), r, w)

    def dma(self, q, out, in_, r=(), w=()):
        self._deps(q, r, w)
        if self.strict_dma and q == 'pool':
            self.dsem.append(self.nc.alloc_semaphore('dsx%d' % len(self.dsem)))
            self.dval.append(0)
            i = len(self.dsem) - 1
            ins = self.E[q].dma_start(out=out, in_=in_)
            self.dval[i] += 16
            ins.then_inc(self.dsem[i], 16)
            self._commit((i, self.dval[i]), r, w)
            return
        i = self.drr
        self.drr = (self.drr + 1) % self.nrot
        if self.dval[i] > 0:
            self._wait(q, i, self.dval[i])
        ins = self.E[q].dma_start(out=out, in_=in_)
        self.dval[i] += 16
        ins.then_inc(self.dsem[i], 16)
        self.nins += 1
        self._commit((i, self.dval[i]), r, w)

    def barrier(self):
        for e in ('pe', 'act', 'dve', 'pool', 'sp'):
            for f in ('pe', 'act', 'dve', 'pool'):
                if f != e and self.cnt[f] > 0:
                    self._wait(e, f, self.cnt[f])

    def finish(self, keys):
        for k in keys:
            if k in self.lastw:
                sk, v = self.lastw[k]
                self._wait('sp', sk, v)
        for i in range(len(self.dsem)):
            if self.dval[i] > 0:
                self._wait('sp', i, self.dval[i])


def build_program(S, L, NSEQ, flags):
    do_attn = flags.get('attn', True)
    do_rwkv = flags.get('rwkv', True)
    NT = S // 128
    NB = S // 512
    KSEL = min(256, S // 4)
    nc = bass.Bass("TRN2", target_bir_lowering=False)
    dt = nc.dram_tensor

    def din(name, shape, dtype=F32):
        return dt(name, list(shape), dtype, kind="ExternalInput").ap()
    x_d = din("x", [NSEQ, S, D])
    pos_d = din("positions", [NSEQ, S], I32)
    watt_d = din("w_att", [L, D, ATT_COLS])
    win_d = din("w_in", [L, D, NIN])
    wbr_d = din("w_branch", [L, 3, 512, D])
    wout_d = din("w_out", [L, D, D])
    fup_d = din("ffn_up", [L, D, 2 * DFF])
    fdn_d = din("ffn_down", [L, DFF, D])
    norms_d = din("norms", [2 * L + 1, D])
    ppl_d = din("pp_layer", [L, 128, 256])
    consts_d = din("consts", [128, 1024])
    rww_d = din("rw_small", [L, 128, 1536])
    y_d = dt("y", [NSEQ, S, D], F32, kind="ExternalOutput").ap()

    sc = Sched(nc)
    if flags.get('strict_deps', False):
        sc.strict = True
    al = nc.alloc_sbuf_tensor

    h = al("h_sb", [128, NT, D], F32)
    xnT = al("xnT", [128, KC, 512], BF16)
    NSLAB = 2
    slabs = [al("slab%d" % i, [128, 4096], BF16) for i in range(NSLAB)]
    slab_rr = [0]
    gmix = al("gmix", [128, D], BF16)
    gffn = al("gffn", [128, D], BF16)
    ppl = al("ppl", [128, 256], F32)
    consts = al("consts_sb", [128, 1024], F32)
    ident = al("ident", [128, 128], BF16)
    onesb = al("onesb", [128, 128], BF16)
    bdb = al("bdb", [128, 128], BF16)
    ss = al("ss", [128, 2 * NT], F32)
    rstd = al("rstd", [128, 2 * NT], F32)
    junk = al("junk", [128, D], BF16)
    xnb = [al("xnb0", [128, D], BF16)] * 2
    yaT = al("yaT", [64, 8, 512], BF16)
    ycT = al("ycT", [128, 4, 512], BF16)
    MSL8 = al("MSL8", [64, 8, 64], BF16)
    MAT = al("MAT", [64, 8, 2, 64], BF16)
    I8 = al("I8", [64, 8, 64], BF16)
    cu_halo = al("cu_halo", [128, 4, 2], BF16)
    f_halo = al("f_halo", [128, 2 * NFF, 2], BF16)
    kT = al("kT", [128, S], BF16)
    kiT = al("kiT", [64, S], BF16)
    vaug = al("vaug", [128, NT, 2, 66], BF16)
    wi = al("wi", [128, NT, 8], F32)
    rw_small = al("rw_small_sb", [128, 1536], BF16)
    Hst = al("Hst", [128, 4, 128], F32)
    zprev = al("zprev", [128, 14], BF16)
    abase = (nc.sbuf_base + 63) // 64 * 64
    ARENA = nc.sbuf_bytes_remaining - 192
    arena = al("arena", [128, (ARENA + 64) // 2], BF16)
    acur = [0]

    def aa(name, shape, dtype):
        nbytes = int(np.prod(shape[1:])) * (4 if dtype in (F32, I32) else 2)
        nbytes = (nbytes + 31) // 32 * 32
        off = abase + acur[0]
        acur[0] += nbytes
        assert acur[0] <= ARENA, (name, acur[0], ARENA)
        return nc.alloc_sbuf_tensor_at(name, list(shape), dtype, offset=off)
    acur[0] = 0
    scT = aa("scT", [128, 12, 514], BF16)
    cuT = aa("cuT", [128, 4, 514], BF16)
    ybT = aa("ybT", [128, 4, 512], BF16)
    mixT = aa("mixT", [128, KC, 512], BF16)
    gsig = [aa("gsig%d" % i, [128, 512], F32) for i in range(2)]
    macc = aa("macc", [128, 512], F32)
    mtmp = aa("mtmp", [128, 512], F32)
    acur[0] = 0
    actT = aa("actT", [128, NFF, 512], BF16)
    fg = [aa("fg%d" % i, [128, 514], BF16) for i in range(2)]
    fu = [aa("fu%d" % i, [128, 514], BF16) for i in range(2)]
    fgc = [aa("fgc%d" % i, [128, 512], F32) for i in range(1)] * 2
    fuc = [aa("fuc%d" % i, [128, 512], F32) for i in range(1)] * 2
    fsl = fgc
    acur[0] = 0
    outb = [aa("outb%d" % i, [128, D], F32) for i in range(2)]
    acur[0] = 0
    posi = aa("posi", [128, 512], I32)
    posf = aa("posf", [128, 512], F32)
    rtA = aa("rtA", [128, 512], F32)
    rtB = aa("rtB", [128, 512], F32)
    rtC = aa("rtC", [128, 512], F32)
    rtD = aa("rtD", [128, 512], F32)
    Cq = aa("Cq", [128, 512], BF16)
    Sq = aa("Sq", [128, 512], BF16)
    Ci = aa("Ci", [128, 512], BF16)
    Si = aa("Si", [128, 512], BF16)
    qT = aa("qT", [128, 4, 512], BF16)
    qiT = aa("qiT", [64, 4, 512], BF16)
    isc = aa("isc", [128, S], F32)
    rl = [aa("rl%d" % i, [128, 512], F32) for i in range(2)]
    maskb = aa("maskb", [128, S], BF16)
    maskT = aa("maskT", [128, NT, 128], BF16)
    pt = [aa("pt%d" % i, [128, 512], BF16) for i in range(2)]
    rdn = aa("rdn", [128, 512], F32)
    rdnb = aa("rdnb", [128, 512], BF16)
    accsb = aa("accsb", [64, 512], F32)
    bsm = aa("bsm", [128, 8], F32)
    acur[0] = 0
    zT = aa("zT", [128, 14, 513], BF16)

    def f4(name):
        return aa(name, [128, 4, 128], F32)
    r_ = f4("rw_r"); k_ = f4("rw_k"); v_ = f4("rw_v"); sg = f4("rw_sg"); Ls = f4("rw_Ls")
    g_ = f4("rw_g"); kkn = f4("rw_kkn"); kf_ = f4("rw_kf")
    t1 = f4("rw_t1"); t2 = f4("rw_t2"); E_ = f4("rw_E"); YT = sg
    a_ = E_
    lraw = t2
    lwla = aa("rw_lwla", [128, 128], BF16)
    lgb = aa("rw_lgb", [128, 128], BF16)
    tb = aa("rw_tb", [128, 4, 128], BF16)
    AR = aa("rw_AR", [128, 4, 2, 128], BF16)
    BT = aa("rw_BT", [128, 4, 128], BF16)
    KT_ = aa("rw_KT", [128, 4, 128], BF16)
    BH = aa("rw_BH", [128, 4, 128], BF16)
    KH = aa("rw_KH", [128, 4, 128], BF16)
    VT = aa("rw_VT", [128, 4, 128], BF16)
    wc = aa("rw_wc", [128, 4, 2], F32)
    Vpad = aa("rw_Vpad", [64, 8, 128], BF16)
    BHpad = aa("rw_BHpad", [64, 8, 128], BF16)
    KHpad = aa("rw_KHpad", [64, 8, 128], BF16)
    ARm = [aa("rw_ARm%d" % i, [128, 4, 2, 128], BF16) for i in range(2)]
    Upad = aa("rw_Upad", [64, 8, 128], BF16)
    ATb = aa("rw_ATb", [64, 8, 2, 64], BF16)
    ATk = aa("rw_ATk", [64, 8, 2, 64], BF16)
    Pm = [aa("rw_P%d" % i, [64, 8, 64], BF16) for i in range(2)]
    Qm = [aa("rw_Q%d" % i, [64, 8, 64], BF16) for i in range(2)]
    STm = [aa("rw_S%d" % i, [64, 8, 64], BF16) for i in range(2)]
    R0 = aa("rw_R0", [64, 8, 64], BF16)
    Hbf = aa("rw_Hbf", [128, 4, 128], BF16)
    print('arena bytes', ARENA, 'rwkv uses', acur[0], flush=True)

    ps = [nc.alloc_psum_tensor("ps%d" % i, [128, 512], F32) for i in range(6)]
    pst = [nc.alloc_psum_tensor("pst%d" % i, [128, 1024], BF16) for i in range(2)]

    rr = {'ps': 0, 'pst': 0, 'xnb': 0}

    ps_reserved = set()

    def next_ps():
        while True:
            i = rr['ps']
            rr['ps'] = (i + 1) % 6
            if i not in ps_reserved:
                return i

    def load_slab(parts):
        i = slab_rr[0]
        slab_rr[0] = (i + 1) % NSLAB
        key = 'slab%d' % i
        views = []
        off = 0
        for (src, p, kc, n) in parts:
            v = slabs[i][0:p, off:off + kc * n].rearrange("p (k n) -> p k n", k=kc)
            sc.dma('pool', v, src.rearrange("(k p) n -> p k n", p=p), r=(), w=(key,))
            views.append(v)
            off += kc * n
        assert off <= 4096
        return views, key

    def mm(psi, pairs, rkeys, out=None):
        o = ps[psi][:] if out is None else out

        def fn(pe):
            ins = None
            n = len(pairs)
            for j, (l_, r_) in enumerate(pairs):
                ins = pe.matmul(o, l_, r_, start=(j == 0), stop=(j == n - 1))
            return ins
        sc.op('pe', fn, r=rkeys, w=('ps%d' % psi,))

    sc.dma('sp', consts[:], consts_d, w=('consts',))
    sc.dma('pool', ident[:], consts_d[:, 0:128], w=('ident',))
    sc.dma('pool', bdb[:], consts_d[:, 519:647], w=('bdb',))
    sc.op('pool', lambda e: e.memset(onesb[:], 1.0), w=('onesb',))
    for hh in range(8):
        sc.op('dve', lambda e: e.tensor_copy(MSL8[:, hh, :], consts[0:64, 455:519]), r=('consts',), w=('MSL8',))
        sc.op('dve', lambda e: e.tensor_copy(MAT[:, hh, 0, :], consts[0:64, 327:391]), r=('consts',), w=('MAT',))
        sc.op('dve', lambda e: e.tensor_copy(MAT[:, hh, 1, :], consts[0:64, 391:455]), r=('consts',), w=('MAT',))
        sc.op('dve', lambda e: e.tensor_copy(I8[:, hh, :], consts[0:64, 775:839]), r=('consts',), w=('I8',))

    def rmsnorm_to_xnT(blk, gtile, gkey, slot):
        for tt in range(4):
            gt = blk * 4 + tt
            col = slot * NT + gt
            sc.op('act', lambda e: e.activation(junk[:], h[:, gt, :], AF.Square,
                                                accum_out=ss[:, col:col + 1]),
                  r=(('h', gt),), w=('junk', ('ss', col)))
            sc.op('act', lambda e: e.activation(rstd[:, col:col + 1], ss[:, col:col + 1], AF.Sqrt,
                                                bias=consts[:, 261:262], scale=1.0 / D),
                  r=(('ss', col), 'consts'), w=(('rstd', col),))
            sc.op('dve', lambda e: e.reciprocal(rstd[:, col:col + 1], rstd[:, col:col + 1]),
                  r=(('rstd', col),), w=(('rstd', col),))
            xi = 0
            sc.op('dve', lambda e: e.scalar_tensor_tensor(xnb[xi][:], h[:, gt, :],
                                                          rstd[:, col:col + 1], gtile[:],
                                                          ALU.mult, ALU.mult),
                  r=(('h', gt), ('rstd', col), gkey), w=('xnb%d' % xi,))
            pi = rr['pst']
            rr['pst'] = 1 - pi

            def tr(pe):
                ins = None
                for kc in range(KC):
                    ins = pe.transpose(pst[pi][:, kc * 128:(kc + 1) * 128],
                                       xnb[xi][:, kc * 128:(kc + 1) * 128], ident[:])
                return ins
            sc.op('pe', tr, r=('xnb%d' % xi, 'ident'), w=('pst%d' % pi,))
            sc.op('act', lambda e: e.copy(xnT[:, :, tt * 128:(tt + 1) * 128],
                                          pst[pi][:].rearrange("p (k t) -> p k t", k=KC)),
                  r=('pst%d' % pi,), w=('xnT',))


    def rope_tables(sq, blk):
        t0 = blk * 512
        sc.dma('sp', posi[:], pos_d[sq:sq + 1, t0:t0 + 512].broadcast_to([128, 512]), w=('posi',))
        sc.op('dve', lambda e: e.tensor_copy(posf[:], posi[:]), r=('posi',), w=('posf',))
        TWO_PI = 2 * math.pi

        def sin_of(dst, dkey, shift):
            sc.op('dve', lambda e: e.tensor_scalar(rtB[:], rtA[:], shift, 1.0 / TWO_PI, ALU.add, ALU.mult),
                  r=('rtA',), w=('rtB',))
            sc.op('dve', lambda e: e.tensor_copy(posi[:], rtB[:]), r=('rtB',), w=('posi',))
            sc.op('dve', lambda e: e.tensor_copy(rtB[:], posi[:]), r=('posi',), w=('rtB',))
            sc.op('dve', lambda e: e.scalar_tensor_tensor(rtB[:], rtB[:], -TWO_PI, rtA[:], ALU.mult, ALU.add),
                  r=('rtB', 'rtA'), w=('rtB',))
            if shift != 0.0:
                sc.op('dve', lambda e: e.tensor_scalar(rtB[:], rtB[:], shift, None, ALU.add),
                      r=('rtB',), w=('rtB',))
            sc.op('dve', lambda e: e.tensor_scalar(rtC[:], rtB[:], math.pi, -TWO_PI, ALU.is_gt, ALU.mult),
                  r=('rtB',), w=('rtC',))
            sc.op('dve', lambda e: e.tensor_tensor(rtB[:], rtB[:], rtC[:], ALU.add), r=('rtB', 'rtC'), w=('rtB',))
            sc.op('dve', lambda e: e.tensor_scalar(rtC[:], rtB[:], -math.pi, TWO_PI, ALU.is_lt, ALU.mult),
                  r=('rtB',), w=('rtC',))
            sc.op('dve', lambda e: e.tensor_tensor(rtB[:], rtB[:], rtC[:], ALU.add), r=('rtB', 'rtC'), w=('rtB',))
            sc.op('dve', lambda e: e.tensor_scalar(rtB[:], rtB[:], -3.1415925, 3.1415925, ALU.max, ALU.min),
                  r=('rtB',), w=('rtB',))
            sc.op('act', lambda e: e.activation(dst, rtB[:], AF.Sin), r=('rtB',), w=(dkey,))
        for (fc, sg, Ct, St, nm) in ((128, 129, Cq, Sq, 'q'), (130, 131, Ci, Si, 'i')):
            sc.op('dve', lambda e: e.tensor_scalar(rtA[:], posf[:], consts[:, fc:fc + 1], None, ALU.mult),
                  r=('posf', 'consts'), w=('rtA',))
            sin_of(rtD[:], 'rtD', 0.0)
            sc.op('dve', lambda e: e.tensor_scalar(St[:], rtD[:], consts[:, sg:sg + 1], None, ALU.mult),
                  r=('rtD', 'consts'), w=('S' + nm,))
            sin_of(Ct[:], 'C' + nm, 0.5 * math.pi)

    def attention_block(sq, l, blk):
        t0 = blk * 512
        if flags.get('att_stage', 9) >= 0:
            rope_tables(sq, blk)
        if flags.get('att_stage', 9) in (0, -1):
            sc.op('pool', lambda e: e.memset(yaT[:], 0.0), w=('yaT',))
            return
        nroped = [0]

        def roped(wm, ws, wk1, wk2, c0, M, Ct, St, tn, dst, dkey):
            nroped[0] += 1
            if nroped[0] > flags.get('att_sub', 99):
                return
            p1 = next_ps()
            mm(p1, [(wm[:, kc, c0:c0 + M], xnT[:, kc, :]) for kc in range(KC)], rkeys=(wk1, 'xnT'),
               out=ps[p1][0:M, :])
            p2 = next_ps()
            mm(p2, [(ws[:, kc, c0:c0 + M], xnT[:, kc, :]) for kc in range(KC)], rkeys=(wk2, 'xnT'),
               out=ps[p2][0:M, :])
            sc.op('dve', lambda e: e.tensor_tensor(rtA[0:M, :], ps[p1][0:M, :], Ct[0:M, :], ALU.mult),
                  r=('ps%d' % p1, 'C' + tn), w=('rtA',))
            sc.op('dve', lambda e: e.tensor_tensor(rtB[0:M, :], ps[p2][0:M, :], St[0:M, :], ALU.mult),
                  r=('ps%d' % p2, 'S' + tn), w=('rtB',))
            sc.op('pool', lambda e: e.tensor_tensor(dst, rtA[0:M, :], rtB[0:M, :], ALU.add),
                  r=('rtA', 'rtB'), w=(dkey,))
        (wm,), k1 = load_slab([(watt_d[l][:, 0:512], 128, KC, 512)])
        (ws,), k2 = load_slab([(watt_d[l][:, ATT_MAIN:ATT_MAIN + 512], 128, KC, 512)])
        for m in range(4):
            roped(wm, ws, k1, k2, m * 128, 128, Cq, Sq, 'q', qT[:, m, :], 'qT')
        (wm,), k1 = load_slab([(watt_d[l][:, 512:960], 128, KC, 448)])
        (ws,), k2 = load_slab([(watt_d[l][:, ATT_MAIN + 512:ATT_MAIN + 960], 128, KC, 448)])
        roped(wm, ws, k1, k2, 0, 128, Cq, Sq, 'q', kT[:, t0:t0 + 512], 'kT')
        for c in range(4):
            roped(wm, ws, k1, k2, 128 + c * 64, 64, Ci, Si, 'i', qiT[:, c, :], 'qiT')
        roped(wm, ws, k1, k2, 384, 64, Ci, Si, 'i', kiT[:, t0:t0 + 512], 'kiT')
        (wv,), k3 = load_slab([(watt_d[l][:, 2 * ATT_MAIN:2 * ATT_MAIN + 136], 128, KC, 136)])
        for tt in range(4 if flags.get('att_sub', 99) >= 20 else 0):
            gt = blk * 4 + tt
            p1 = next_ps()
            mm(p1, [(xnT[:, kc, tt * 128:(tt + 1) * 128], wv[:, kc, :]) for kc in range(KC)],
               rkeys=(k3, 'xnT'), out=ps[p1][:, 0:136])
            if flags.get('vw_var', 9) >= 2:
                sc.op('act', lambda e: e.copy(vaug[:, gt, :, 0:64],
                                              ps[p1][:, 0:128].rearrange("p (n d) -> p n d", n=2)),
                      r=('ps%d' % p1,), w=('vaug',))
            if flags.get('vw_var', 9) >= 3:
                sc.op('act', lambda e: e.copy(wi[:, gt, :], ps[p1][:, 128:136]),
                      r=('ps%d' % p1,), w=('wi',))
        if flags.get('att_stage', 9) < 2:
            sc.op('pool', lambda e: e.memset(yaT[:], 0.0), w=('yaT',))
            return
        MX, LO, RNG, MID, CNT, GEH = range(6)
        col = lambda j: bsm[:, j:j + 1]
        for tt in range(4):
            gt = blk * 4 + tt
            nk = (gt + 1) * 128
            qs = slice(tt * 128, (tt + 1) * 128)
            for k0 in range(0, nk, 512):
                n = min(512, nk - k0)
                for hh in range(8):
                    c, bp = hh // 2, 32 * (hh % 2)
                    p1 = next_ps()
                    mm(p1, [(qiT[bp:bp + 32, c, qs], kiT[bp:bp + 32, k0:k0 + n])], rkeys=('qiT', 'kiT'),
                       out=ps[p1][:, 0:n])
                    ri = hh % 2
                    sc.op('act', lambda e: e.activation(rl[ri][:, 0:n], ps[p1][:, 0:n], AF.Relu),
                          r=('ps%d' % p1,), w=('rl%d' % ri,))
                    if hh == 0:
                        sc.op('dve', lambda e: e.tensor_scalar(isc[:, k0:k0 + n], rl[ri][:, 0:n],
                                                               wi[:, gt, 0:1], None, ALU.mult),
                              r=('rl%d' % ri, 'wi'), w=('isc',))
                    else:
                        sc.op('dve', lambda e: e.scalar_tensor_tensor(isc[:, k0:k0 + n], rl[ri][:, 0:n],
                                                                      wi[:, gt, hh:hh + 1], isc[:, k0:k0 + n],
                                                                      ALU.mult, ALU.add),
                              r=('rl%d' % ri, 'wi', 'isc'), w=('isc',))
            if nk > KSEL:
                sc.op('dve', lambda e: e.tensor_reduce(col(MX), isc[:, 0:nk], AX.X, ALU.max,
                                                       apply_absolute_value=True),
                      r=('isc',), w=('bs_mx',))
            sc.op('dve', lambda e: e.tensor_tensor(isc[:, gt * 128:(gt + 1) * 128],
                                                   isc[:, gt * 128:(gt + 1) * 128],
                                                   consts[:, 133:261], ALU.add),
                  r=('isc', 'consts'), w=('isc',))
            if nk <= KSEL:
                sc.op('dve', lambda e: e.memset(col(LO), -1.0e29), w=('bs_lo',))
            else:
                sc.op('dve', lambda e: e.tensor_scalar(col(LO), col(MX), -1.0, -1.0, ALU.mult, ALU.add),
                      r=('bs_mx',), w=('bs_lo',))
                sc.op('dve', lambda e: e.tensor_scalar(col(RNG), col(MX), 2.0, 2.0, ALU.mult, ALU.add),
                      r=('bs_mx',), w=('bs_rng',))
                for it in range(BIS_ITERS):
                    ck = 0.5 ** (it + 1)
                    sc.op('dve', lambda e: e.scalar_tensor_tensor(col(MID), col(RNG), ck, col(LO),
                                                                  ALU.mult, ALU.add),
                          r=('bs_rng', 'bs_lo'), w=('bs_mid',))
                    sc.op('dve', lambda e: e.tensor_scalar(maskb[:, 0:nk], isc[:, 0:nk], col(MID), None,
                                                           ALU.is_ge, ALU.add, accum_out=col(CNT)),
                          r=('isc', 'bs_mid'), w=('maskb', 'bs_cnt'))
                    sc.op('dve', lambda e: e.tensor_scalar(col(GEH), col(CNT), KSEL - 0.5, ck,
                                                           ALU.is_ge, ALU.mult),
                          r=('bs_cnt',), w=('bs_geh',))
                    sc.op('dve', lambda e: e.scalar_tensor_tensor(col(LO), col(GEH), col(RNG), col(LO),
                                                                  ALU.mult, ALU.add),
                          r=('bs_geh', 'bs_rng', 'bs_lo'), w=('bs_lo',))
            sc.op('dve', lambda e: e.tensor_scalar(maskb[:, 0:nk], isc[:, 0:nk], col(LO), None, ALU.is_ge),
                  r=('isc', 'bs_lo'), w=('maskb',))
            for i0 in range(0, gt + 1, 8):
                nb = min(8, gt + 1 - i0)
                pi = rr['pst']
                rr['pst'] = 1 - pi

                def tr(pe, i0=i0, nb=nb, pi=pi):
                    ins = None
                    for j in range(nb):
                        ins = pe.transpose(pst[pi][:, j * 128:(j + 1) * 128],
                                           maskb[:, (i0 + j) * 128:(i0 + j + 1) * 128], ident[:])
                    return ins
                sc.op('pe', tr, r=('maskb', 'ident'), w=('pst%d' % pi,))
                sc.op('act', lambda e: e.copy(maskT[:, i0:i0 + nb, :],
                                              pst[pi][:, 0:nb * 128].rearrange("p (k t) -> p k t", k=nb)),
                      r=('pst%d' % pi,), w=('maskT',))
            if flags.get('att_stage', 9) < 3:
                sc.op('pool', lambda e: e.memset(yaT[:], 0.0), w=('yaT',))
                continue
            A3 = flags.get('a3', 9)
            for n in range(2):
                bp = 64 * n
                pacc = next_ps()
                ps_reserved.add(pacc)
                for i in range(gt + 1):
                    p1 = next_ps()
                    sc.op('pe', lambda pe: pe.matmul(ps[p1][:].rearrange("p (a b) -> p a b", a=4),
                                                     kT[bp:bp + 64, i * 128:(i + 1) * 128],
                                                     qT[bp:bp + 64, :, qs], start=True, stop=True),
                          r=('kT', 'qT'), w=('ps%d' % p1,))
                    pj = i % 2
                    sc.op('act', lambda e: e.activation(pt[pj][:], ps[p1][:], AF.Exp, scale=0.125),
                          r=('ps%d' % p1,), w=('pt%d' % pj,))
                    if A3 >= 2:
                        for a in range(4):
                            sc.op('pool', lambda e: e.tensor_tensor(
                                pt[pj][:, a * 128:(a + 1) * 128], pt[pj][:, a * 128:(a + 1) * 128],
                                maskT[:, i, :], ALU.mult),
                                r=('pt%d' % pj, 'maskT'), w=('pt%d' % pj,))
                    if A3 >= 3:
                        sc.op('pe', lambda pe: pe.matmul(ps[pacc][0:65, :], vaug[:, i, n, 0:65], pt[pj][:],
                                                         start=(i == 0), stop=(i == gt)),
                              r=('vaug', 'pt%d' % pj), w=('ps%d' % pacc,))
                if A3 >= 4:
                    sc.op('dve', lambda e: e.reciprocal(rdn[64:65, :], ps[pacc][64:65, :]),
                          r=('ps%d' % pacc,), w=('rdn',))
                    sc.op('dve', lambda e: e.tensor_copy(rdnb[64:65, :], rdn[64:65, :]),
                          r=('rdn',), w=('rdnb',))
                pb = next_ps()
                if A3 >= 5:
                    sc.op('pe', lambda pe: pe.matmul(ps[pb][0:64, :], onesb[64:65, 0:64], rdnb[64:65, :],
                                                     start=True, stop=True),
                          r=('rdnb', 'onesb'), w=('ps%d' % pb,))
                if A3 >= 6:
                    sc.op('act', lambda e: e.copy(accsb[:], ps[pacc][0:64, :]), r=('ps%d' % pacc,), w=('accsb',))
                ps_reserved.discard(pacc)
                if A3 >= 7:
                    sc.op('dve', lambda e: e.tensor_tensor(
                        yaT[:, n * 4:(n + 1) * 4, qs],
                        accsb[:].rearrange("p (a b) -> p a b", a=4),
                        ps[pb][0:64, :].rearrange("p (a b) -> p a b", a=4), ALU.mult),
                        r=('accsb', 'ps%d' % pb), w=('yaT',))
                else:
                    sc.op('pool', lambda e: e.memset(yaT[:], 0.0), w=('yaT',))


    class _Stop(Exception):
        pass

    def chk(n):
        if flags.get('rw_stage', 99) < n:
            raise _Stop()

    def rwkv_block(sq, l, blk):
        try:
            rwkv_block_(sq, l, blk)
        except _Stop:
            sc.op('pool', lambda e: e.memset(ycT[:], 0.0), w=('ycT',))

    stg_rr = [0]

    def evac2(dst_flat, psrc, pkey_, in1_flat, in1key, op, dkey):
        sc.op('dve', lambda e: e.tensor_tensor(dst_flat, psrc, in1_flat, op), r=(pkey_, in1key), w=(dkey,))

    def rwkv_block_(sq, l, blk):
        PP = lambda c: ppl[:, c:c + 1]
        sc.op('pool', lambda e: e.tensor_copy(Hbf[:], Hst[:]), r=('Hst',), w=('Hbf',))
        for (tz, kz) in ((Vpad, 'Vpad'), (BHpad, 'BHpad'), (KHpad, 'KHpad'), (Upad, 'Upad')):
            sc.op('pool', lambda e: e.memset(tz[:], 0.0), w=(kz,))
        for s4 in range(4):
            n = 512 if s4 < 3 else 256
            (wv,), wk = load_slab([(win_d[l][:, OFF_RW + s4 * 512:OFF_RW + s4 * 512 + n], 128, KC, n)])
            for m in range(n // 128):
                c = s4 * 4 + m
                pi = next_ps()
                mm(pi, [(wv[:, kc, m * 128:(m + 1) * 128], xnT[:, kc, :]) for kc in range(KC)], rkeys=(wk, 'xnT'))
                sc.op('act', lambda e: e.copy(zT[:, c, 1:513], ps[pi][:]), r=('ps%d' % pi,), w=('zT',))
        sc.op('pool', lambda e: e.tensor_copy(zT[:, :, 0], zprev[:, :]), r=('zprev',), w=('zT',))
        sc.op('pool', lambda e: e.tensor_copy(zprev[:, :], zT[:, :, 512]), r=('zT',), w=('zprev',))
        chk(1)
        for su in range(4):
            s0 = su * 128
            zc = lambda c: zT[:, c, s0 + 1:s0 + 129]
            zp = lambda c: zT[:, c, s0:s0 + 128]

            def shift(dst, dkey, c):
                sc.op('dve', lambda e: e.tensor_tensor(E_[:, 0, :], zp(c), zc(c), ALU.subtract),
                      r=('zT',), w=('E_',))
                sc.op('dve', lambda e: e.scalar_tensor_tensor(dst, E_[:, 0, :], PP(168 + c), zc(c),
                                                              ALU.mult, ALU.add),
                      r=('E_', 'zT', 'ppl'), w=(dkey,))
            for p in range(4):
                shift(r_[:, p, :], 'r_', p)
                shift(k_[:, p, :], 'k_', 4 + p)
                shift(v_[:, p, :], 'v_', 8 + p)
            shift(lraw[:, 0, :], 't2', 12)
            shift(lraw[:, 1, :], 't2', 13)
            sc.op('act', lambda e: e.activation(lwla[0:64, :], lraw[0:64, 0, :], AF.Tanh), r=('t2',), w=('lwla',))
            sc.op('act', lambda e: e.copy(lwla[64:128, :], lraw[64:128, 0, :]), r=('t2',), w=('lwla',))
            sc.op('act', lambda e: e.activation(lgb[:], lraw[:, 1, :], AF.Sigmoid), r=('t2',), w=('lgb',))
            pzw = next_ps()

            def f_zw(pe):
                ins = None
                for p in range(4):
                    ins = pe.matmul(ps[pzw][:, p * 128:(p + 1) * 128], rw_small[0:64, p * 128:(p + 1) * 128],
                                    lwla[0:64, :], start=True, stop=True)
                return ins
            sc.op('pe', f_zw, r=('rw_small', 'lwla'), w=('ps%d' % pzw,))
            for p in range(4):
                sc.op('act', lambda e: e.activation(sg[:, p, :], ps[pzw][:, p * 128:(p + 1) * 128], AF.Sigmoid,
                                                    bias=PP(182 + p), scale=1.0),
                      r=('ps%d' % pzw, 'ppl'), w=('sg',))
            pza = next_ps()

            def f_za(pe):
                ins = None
                for p in range(4):
                    ins = pe.matmul(ps[pza][:, p * 128:(p + 1) * 128],
                                    rw_small[64:128, 512 + p * 128:512 + (p + 1) * 128],
                                    lwla[64:128, :], start=True, stop=True)
                return ins
            sc.op('pe', f_za, r=('rw_small', 'lwla'), w=('ps%d' % pza,))
            for p in range(4):
                sc.op('act', lambda e: e.activation(a_[:, p, :], ps[pza][:, p * 128:(p + 1) * 128], AF.Sigmoid,
                                                    bias=PP(186 + p), scale=1.0),
                      r=('ps%d' % pza, 'ppl'), w=('E_',))
            pg = next_ps()

            def f_g(pe):
                ins = None
                for p in range(4):
                    ins = pe.matmul(ps[pg][:, p * 128:(p + 1) * 128],
                                    rw_small[:, 1024 + p * 128:1024 + (p + 1) * 128],
                                    lgb[:, :], start=True, stop=True)
                return ins
            sc.op('pe', f_g, r=('rw_small', 'lgb'), w=('ps%d' % pg,))
            sc.op('act', lambda e: e.copy(g_[:].rearrange("p a t -> p (a t)"), ps[pg][:]),
                  r=('ps%d' % pg,), w=('g_',))
            chk(2)
            for p in range(4):
                sc.op('dve', lambda e: e.tensor_scalar(kkn[:, p, :], k_[:, p, :], PP(190 + p), None, ALU.mult),
                      r=('k_', 'ppl'), w=('kkn',))
            sc.op('dve', lambda e: e.tensor_tensor(tb[:], kkn[:], kkn[:], ALU.mult), r=('kkn',), w=('tb',))
            pn = next_ps()
            sc.op('pe', lambda pe: pe.matmul(ps[pn][:], bdb[:], tb[:].rearrange("p a t -> p (a t)"),
                                             start=True, stop=True),
                  r=('bdb', 'tb'), w=('ps%d' % pn,))
            sc.op('act', lambda e: e.activation(t2[:].rearrange("p a t -> p (a t)"), ps[pn][:], AF.Sqrt),
                  r=('ps%d' % pn,), w=('t2',))
            sc.op('dve', lambda e: e.tensor_scalar(t2[:], t2[:], 1.0e-12, None, ALU.max), r=('t2',), w=('t2',))
            sc.op('dve', lambda e: e.reciprocal(t2[:], t2[:]), r=('t2',), w=('t2',))
            sc.op('dve', lambda e: e.tensor_tensor(kkn[:], kkn[:], t2[:], ALU.mult), r=('kkn', 't2'), w=('kkn',))
            chk(3)
            for p in range(4):
                sc.op('dve', lambda e: e.tensor_scalar(t1[:, p, :], a_[:, p, :], -1.0, PP(194 + p),
                                                       ALU.add, ALU.mult),
                      r=('E_', 'ppl'), w=('t1',))
            sc.op('dve', lambda e: e.scalar_tensor_tensor(kf_[:], t1[:], 1.0, k_[:], ALU.add, ALU.mult),
                  r=('t1', 'k_'), w=('kf_',))
            sc.op('dve', lambda e: e.tensor_tensor(t1[:], kkn[:], a_[:], ALU.mult), r=('kkn', 'E_'), w=('t1',))
            for p in range(4):
                sc.op('dve', lambda e: e.tensor_tensor_scan(Ls[:, p, :], consts[:, 647:775], sg[:, p, :], 0.0,
                                                            ALU.mult, ALU.add),
                      r=('consts', 'sg'), w=('Ls',))
            sc.op('dve', lambda e: e.tensor_tensor(t2[:], Ls[:], sg[:], ALU.subtract), r=('Ls', 'sg'), w=('t2',))
            sc.op('act', lambda e: e.activation(E_[:], t2[:], AF.Exp, scale=-C0), r=('t2',), w=('E_',))
            sc.op('dve', lambda e: e.scalar_tensor_tensor(AR[:, :, 0, :], kkn[:], -1.0, E_[:], ALU.mult, ALU.mult),
                  r=('kkn', 'E_'), w=('AR',))
            sc.op('act', lambda e: e.activation(E_[:], Ls[:], AF.Exp, scale=-C0), r=('Ls',), w=('E_',))
            sc.op('dve', lambda e: e.tensor_tensor(AR[:, :, 1, :], r_[:], E_[:], ALU.mult), r=('r_', 'E_'), w=('AR',))
            for j in range(2):
                sc.op('dve', lambda e: e.tensor_scalar(ARm[j][:].rearrange("p a b t -> p (a b t)"),
                                                       AR[:].rearrange("p a b t -> p (a b t)"),
                                                       consts[:, 839 + j:840 + j], None, ALU.mult),
                      r=('AR', 'consts'), w=('ARm%d' % j,))
            sc.op('act', lambda e: e.activation(E_[:], Ls[:], AF.Exp, scale=C0), r=('Ls',), w=('E_',))
            sc.op('dve', lambda e: e.tensor_tensor(BT[:], t1[:], E_[:], ALU.mult), r=('t1', 'E_'), w=('BT',))
            sc.op('dve', lambda e: e.tensor_tensor(KT_[:], kf_[:], E_[:], ALU.mult), r=('kf_', 'E_'), w=('KT_',))
            for p in range(4):
                for c in range(2):
                    cs = slice(c * 64, (c + 1) * 64)
                    sc.op('dve', lambda e: e.tensor_scalar(t2[:, p, cs], Ls[:, p, cs],
                                                           Ls[:, p, c * 64 + 63:c * 64 + 64], None, ALU.subtract),
                          r=('Ls',), w=('t2',))
            sc.op('act', lambda e: e.activation(E_[:], t2[:], AF.Exp, scale=C0), r=('t2',), w=('E_',))
            sc.op('dve', lambda e: e.tensor_tensor(BH[:], t1[:], E_[:], ALU.mult), r=('t1', 'E_'), w=('BH',))
            sc.op('dve', lambda e: e.tensor_tensor(KH[:], kf_[:], E_[:], ALU.mult), r=('kf_', 'E_'), w=('KH',))
            sc.op('act', lambda e: e.activation(wc[:], Ls[:].rearrange("p a (c t) -> p a c t", c=2)[:, :, :, 63],
                                                AF.Exp, scale=-C0),
                  r=('Ls',), w=('wc',))
            chk(4)
            sc.op('dve', lambda e: e.tensor_tensor(t2[:], r_[:], kf_[:], ALU.mult), r=('r_', 'kf_'), w=('t2',))
            for p in range(4):
                sc.op('dve', lambda e: e.tensor_scalar(tb[:, p, :], t2[:, p, :], PP(198 + p), None, ALU.mult),
                      r=('t2', 'ppl'), w=('tb',))
            chk(4.1)
            pbn = next_ps()
            sc.op('pe', lambda pe: pe.matmul(ps[pbn][:], bdb[:], tb[:].rearrange("p a t -> p (a t)"),
                                             start=True, stop=True),
                  r=('bdb', 'tb'), w=('ps%d' % pbn,))
            chk(4.2)
            sc.op('dve', lambda e: e.tensor_tensor(E_[:].rearrange("p a t -> p (a t)"), ps[pbn][:],
                                                   v_[:].rearrange("p a t -> p (a t)"), ALU.mult),
                  r=('ps%d' % pbn, 'v_'), w=('E_',))
            chk(4.3)
            sc.op('act', lambda e: e.copy(VT[:], v_[:]), r=('v_',), w=('VT',))
            chk(4.4)
            chk(5)
            for c in range(2):
                cs = slice(c * 64, (c + 1) * 64)
                hp = lambda hh: (hh // 2, 64 * (hh % 2))
                for (src, skey, dst, dkey) in ((VT, 'VT', Vpad, 'Vpad'), (BH, 'BH', BHpad, 'BHpad'),
                                               (KH, 'KH', KHpad, 'KHpad')):
                    pi = next_ps()

                    def trf(pe, src=src, pi=pi, c=c):
                        ins = None
                        for p in range(4):
                            ins = pe.matmul(ps[pi][0:64, p * 128:(p + 1) * 128],
                                            src[:, p, c * 64:(c + 1) * 64], ident[:], start=True, stop=True)
                        return ins
                    sc.op('pe', trf, r=(skey, 'ident'), w=('ps%d' % pi,))
                    for j in range(2):
                        sc.op('dve', lambda e: e.tensor_copy(
                            dst[:, j::2, j * 64:(j + 1) * 64],
                            ps[pi][0:64, :].rearrange("p (a b) -> p a b", a=4)[:, :, j * 64:(j + 1) * 64]),
                            r=('ps%d' % pi,), w=(dkey,))
                pX = next_ps()

                def f_x(pe):
                    ins = None
                    for hh in range(0, 8, 2 if flags.get('rwx', 0) == 5 else 1):
                        p, bp = hp(hh)
                        ins = pe.matmul(ps[pX][0:64, hh * 64:(hh + 1) * 64], ARm[hh % 2][:, p, 0, cs],
                                        BT[:, p, cs], start=True, stop=True)
                    return ins
                RWX = flags.get('rwx', 0)
                if RWX != 1:
                    sc.op('pe', f_x, r=('ARm0', 'ARm1', 'BT'), w=('ps%d' % pX,))
                if RWX not in (1, 3):
                    evac2(Pm[0][:].rearrange("p h t -> p (h t)"), ps[pX][0:64, :], 'ps%d' % pX,
                          MSL8[:].rearrange("p h t -> p (h t)"), 'MSL8', ALU.mult, 'Pm0')
                for (lh, lkey, dstA, dkey) in ((BT, 'BT', ATb, 'ATb'), (KT_, 'KT_', ATk, 'ATk')):
                    for half in range(2):
                        pY = next_ps()

                        def f_y(pe, lh=lh, half=half, pY=pY):
                            ins = None
                            for q4 in range(4):
                                hh = half * 4 + q4
                                p, bp = hp(hh)
                                ins = pe.matmul(ps[pY][0:64, q4 * 128:(q4 + 1) * 128].rearrange(
                                    "p (a b) -> p a b", a=2), lh[:, p, cs], ARm[hh % 2][:, p, :, cs],
                                    start=True, stop=True)
                            return ins
                        if RWX in (2, 5):
                            continue
                        sc.op('pe', f_y, r=(lkey, 'ARm0', 'ARm1'), w=('ps%d' % pY,))
                        if RWX == 4:
                            continue
                        evac2(dstA[:, half * 4:(half + 1) * 4, :, :].rearrange("p h a t -> p (h a t)"),
                              ps[pY][0:64, :], 'ps%d' % pY,
                              MAT[:, half * 4:(half + 1) * 4, :, :].rearrange("p h a t -> p (h a t)"), 'MAT',
                              ALU.mult, dkey)
                chk(6)
                sc.op('dve', lambda e: e.tensor_tensor(STm[0][:], ATb[:, :, 0, :], I8[:], ALU.add),
                      r=('ATb', 'I8'), w=('STm0',))
                Pc = lambda hh: Pm[0][:, hh, :]
                Qc = lambda hh: ATb[:, hh, 0, :]
                pkey, qkey = 'Pm0', 'ATb'
                si = 0
                pq = 0
                for rd in range(1, 7):
                    do_sq = rd <= 5
                    do_s = rd >= 2
                    if do_sq:
                        pP = next_ps()
                        pQ = next_ps()

                        def f_p(pe, Pc=Pc, Qc=Qc, pP=pP):
                            ins = None
                            for hh in range(8):
                                ins = pe.matmul(ps[pP][0:64, hh * 64:(hh + 1) * 64], Qc(hh), Pc(hh),
                                                start=True, stop=True)
                            return ins

                        def f_q(pe, Pc=Pc, Qc=Qc, pQ=pQ):
                            ins = None
                            for hh in range(8):
                                ins = pe.matmul(ps[pQ][0:64, hh * 64:(hh + 1) * 64], Pc(hh), Qc(hh),
                                                start=True, stop=True)
                            return ins
                        sc.op('pe', f_p, r=(pkey, qkey), w=('ps%d' % pP,))
                        sc.op('pe', f_q, r=(pkey, qkey), w=('ps%d' % pQ,))
                    if do_s:
                        pS = next_ps()

                        def f_s(pe, Pc=Pc, si=si, pS=pS):
                            ins = None
                            for hh in range(8):
                                ins = pe.matmul(ps[pS][0:64, hh * 64:(hh + 1) * 64], Pc(hh), STm[si][:, hh, :],
                                                start=True, stop=True)
                            return ins
                        sc.op('pe', f_s, r=(pkey, 'STm%d' % si), w=('ps%d' % pS,))
                        evac2(STm[1 - si][:].rearrange("p h t -> p (h t)"), ps[pS][0:64, :], 'ps%d' % pS,
                              STm[si][:].rearrange("p h t -> p (h t)"), 'STm%d' % si, ALU.add,
                              'STm%d' % (1 - si))
                        si = 1 - si
                    if do_sq:
                        nx = 1 - pq if rd > 1 else 1
                        sc.op('act', lambda e: e.copy(Pm[nx][:].rearrange("p h t -> p (h t)"), ps[pP][0:64, :]),
                              r=('ps%d' % pP,), w=('Pm%d' % nx,))
                        sc.op('dve', lambda e: e.tensor_copy(Qm[nx][:].rearrange("p h t -> p (h t)"), ps[pQ][0:64, :]),
                              r=('ps%d' % pQ,), w=('Qm%d' % nx,))
                        Pc = lambda hh, nx=nx: Pm[nx][:, hh, :]
                        Qc = lambda hh, nx=nx: Qm[nx][:, hh, :]
                        pkey, qkey = 'Pm%d' % nx, 'Qm%d' % nx
                        pq = nx
                chk(7)
                pR = next_ps()

                def f_r(pe):
                    ins = None
                    for hh in range(8):
                        p, bp = hp(hh)
                        o = ps[pR][0:64, hh * 64:(hh + 1) * 64]
                        j = hh % 2
                        pe.matmul(o, AR[:, p, 0, cs], Hbf[:, p, j * 64:(j + 1) * 64], start=True, stop=False)
                        ins = pe.matmul(o, ATk[:, hh, 0, :], Vpad[:, hh, j * 64:(j + 1) * 64],
                                        start=False, stop=True)
                    return ins
                sc.op('pe', f_r, r=('AR', 'Hbf', 'ATk', 'Vpad'), w=('ps%d' % pR,))
                sc.op('dve', lambda e: e.tensor_copy(R0[:].rearrange("p h t -> p (h t)"), ps[pR][0:64, :]),
                      r=('ps%d' % pR,), w=('R0',))
                pU = next_ps()

                def f_u(pe):
                    ins = None
                    for hh in range(8):
                        ins = pe.matmul(ps[pU][0:64, hh * 64:(hh + 1) * 64], STm[si][:, hh, :], R0[:, hh, :],
                                        start=True, stop=True)
                    return ins
                sc.op('pe', f_u, r=('STm%d' % si, 'R0'), w=('ps%d' % pU,))
                for j in range(2):
                    sc.op('dve', lambda e: e.tensor_copy(
                        Upad[:, j::2, j * 64:(j + 1) * 64],
                        ps[pU][0:64, :].rearrange("p (a b t) -> p a b t", a=4, b=2)[:, :, j, :]),
                        r=('ps%d' % pU,), w=('Upad',))
                chk(8)
                pYo = next_ps()

                def f_yo(pe):
                    ins = None
                    for p in range(4):
                        o = ps[pYo][:, p * 64:(p + 1) * 64]
                        pe.matmul(o, Hbf[:, p, :], AR[:, p, 1, cs], start=True, stop=False)
                        for j in range(2):
                            hh = 2 * p + j
                            pe.matmul(o, Upad[:, hh, :], ATb[:, hh, 1, :], start=False, stop=False)
                            ins = pe.matmul(o, Vpad[:, hh, :], ATk[:, hh, 1, :], start=False, stop=(j == 1))
                    return ins
                sc.op('pe', f_yo, r=('Hbf', 'AR', 'Upad', 'ATb', 'Vpad', 'ATk'), w=('ps%d' % pYo,))
                sc.op('act', lambda e: e.copy(YT[:, :, cs], ps[pYo][:, 0:256].rearrange("p (a t) -> p a t", a=4)),
                      r=('ps%d' % pYo,), w=('sg',))
                pH = next_ps()

                def f_h(pe):
                    ins = None
                    for hh in range(8):
                        p, j = hh // 2, hh % 2
                        o = ps[pH][:, p * 128 + j * 64:p * 128 + (j + 1) * 64]
                        pe.matmul(o, BHpad[:, hh, :], Upad[:, hh, j * 64:(j + 1) * 64], start=True, stop=False)
                        ins = pe.matmul(o, KHpad[:, hh, :], Vpad[:, hh, j * 64:(j + 1) * 64],
                                        start=False, stop=True)
                    return ins
                sc.op('pe', f_h, r=('BHpad', 'Upad', 'KHpad', 'Vpad'), w=('ps%d' % pH,))
                for p in range(4):
                    sc.op('dve', lambda e: e.scalar_tensor_tensor(Hst[:, p, :], Hst[:, p, :], wc[:, p, c:c + 1],
                                                                  ps[pH][:, p * 128:(p + 1) * 128],
                                                                  ALU.mult, ALU.add),
                          r=('Hst', 'wc', 'ps%d' % pH), w=('Hst',))
                sc.op('pool', lambda e: e.tensor_copy(Hbf[:], Hst[:]), r=('Hst',), w=('Hbf',))
            chk(9)
            F = lambda t: t[:].rearrange("p a t -> p (a t)")
            sc.op('act', lambda e: e.copy(tb[:], YT[:]), r=('sg',), w=('tb',))
            p1 = next_ps()
            sc.op('pe', lambda pe: pe.matmul(ps[p1][:], bdb[:], F(tb), start=True, stop=True),
                  r=('bdb', 'tb'), w=('ps%d' % p1,))
            sc.op('dve', lambda e: e.scalar_tensor_tensor(F(t1), ps[p1][:], -1.0 / 64, F(YT), ALU.mult, ALU.add),
                  r=('ps%d' % p1, 'sg'), w=('t1',))
            sc.op('dve', lambda e: e.tensor_tensor(tb[:], t1[:], t1[:], ALU.mult), r=('t1',), w=('tb',))
            p2 = next_ps()
            sc.op('pe', lambda pe: pe.matmul(ps[p2][:], bdb[:], F(tb), start=True, stop=True),
                  r=('bdb', 'tb'), w=('ps%d' % p2,))
            sc.op('act', lambda e: e.activation(F(t2), ps[p2][:], AF.Sqrt, bias=consts[:, 262:263], scale=1.0 / 64),
                  r=('ps%d' % p2, 'consts'), w=('t2',))
            sc.op('dve', lambda e: e.reciprocal(t2[:], t2[:]), r=('t2',), w=('t2',))
            sc.op('dve', lambda e: e.tensor_tensor(t1[:], t1[:], t2[:], ALU.mult), r=('t1', 't2'), w=('t1',))
            for p in range(4):
                sc.op('dve', lambda e: e.tensor_scalar(t1[:, p, :], t1[:, p, :], PP(202 + p), PP(206 + p),
                                                       ALU.mult, ALU.add),
                      r=('t1', 'ppl'), w=('t1',))
            sc.op('dve', lambda e: e.tensor_tensor(t1[:], t1[:], E_[:], ALU.add), r=('t1', 'E_'), w=('t1',))
            sc.op('dve', lambda e: e.tensor_tensor(ycT[:, :, s0:s0 + 128], t1[:], g_[:], ALU.mult),
                  r=('t1', 'g_'), w=('ycT',))

    for sq in range(NSEQ):
        for gt in range(NT):
            sc.dma('sp', h[:, gt, :], x_d[sq, gt * 128:(gt + 1) * 128, :], w=(('h', gt),))
        sc.op('pool', lambda e: e.memset(vaug[:], 1.0), w=('vaug',))
        for l in range(L):
            sc.dma('pool', gmix[:], norms_d[2 * l:2 * l + 1, :].broadcast_to([128, D]), w=('gmix',))
            sc.dma('pool', gffn[:], norms_d[2 * l + 1:2 * l + 2, :].broadcast_to([128, D]), w=('gffn',))
            sc.dma('sp', ppl[:], ppl_d[l], w=('ppl',))
            sc.dma('pool', rw_small[:], rww_d[l], w=('rw_small',))
            sc.op('pool', lambda e: e.memset(Hst[:], 0.0), w=('Hst',))
            sc.op('pool', lambda e: e.memset(zprev[:], 0.0), w=('zprev',))
            sc.op('pool', lambda e: e.memset(cu_halo[:], 0.0), w=('cu_halo',))
            sc.op('pool', lambda e: e.memset(f_halo[:], 0.0), w=('f_halo',))
            for blk in range(NB):
                t0 = blk * 512
                rmsnorm_to_xnT(blk, gmix, 'gmix', 0)
                if do_attn:
                    sc.barrier()
                    attention_block(sq, l, blk)
                if do_rwkv:
                    sc.barrier()
                    rwkv_block(sq, l, blk)
                sc.barrier()
                for grp in range(3):
                    (wv,), wk = load_slab([(win_d[l][:, OFF_SC + grp * 512:OFF_SC + (grp + 1) * 512],
                                            128, KC, 512)])
                    for m in range(4):
                        pi = next_ps()
                        mm(pi, [(wv[:, kc, m * 128:(m + 1) * 128], xnT[:, kc, :]) for kc in range(KC)],
                           rkeys=(wk, 'xnT'))
                        ch = grp * 4 + m
                        sc.op('act', lambda e: e.copy(scT[:, ch, 2:514], ps[pi][:]),
                              r=('ps%d' % pi,), w=(('scT', ch),))
                for m in range(4):
                    sc.op('pool', lambda e: e.tensor_copy(cuT[:, m, 0:2], cu_halo[:, m, :]),
                          r=('cu_halo',), w=(('cuT', m),))
                    sc.op('dve', lambda e: e.tensor_tensor(cuT[:, m, 2:514], scT[:, 8 + m, 2:514],
                                                           scT[:, m, 2:514], ALU.mult),
                          r=(('scT', 8 + m), ('scT', m)), w=(('cuT', m),))
                    sc.op('pool', lambda e: e.tensor_copy(cu_halo[:, m, :], cuT[:, m, 512:514]),
                          r=(('cuT', m),), w=('cu_halo',))
                    cw = lambda j: ppl[:, 24 + m * 3 + j:24 + m * 3 + j + 1]
                    sc.op('dve', lambda e: e.tensor_scalar(macc[:], cuT[:, m, 0:512], cw(0), None, ALU.mult),
                          r=(('cuT', m), 'ppl'), w=('macc',))
                    sc.op('dve', lambda e: e.scalar_tensor_tensor(macc[:], cuT[:, m, 1:513], cw(1), macc[:],
                                                                  ALU.mult, ALU.add),
                          r=(('cuT', m), 'ppl', 'macc'), w=('macc',))
                    sc.op('dve', lambda e: e.scalar_tensor_tensor(macc[:], cuT[:, m, 2:514], cw(2), macc[:],
                                                                  ALU.mult, ALU.add),
                          r=(('cuT', m), 'ppl', 'macc'), w=('macc',))
                    sc.op('dve', lambda e: e.tensor_tensor(ybT[:, m, :], macc[:], scT[:, 4 + m, 2:514], ALU.mult),
                          r=('macc', ('scT', 4 + m)), w=('ybT',))
                if not do_attn:
                    sc.op('pool', lambda e: e.memset(yaT[:], 0.0), w=('yaT',))
                if not do_rwkv:
                    sc.op('pool', lambda e: e.memset(ycT[:], 0.0), w=('ycT',))
                for m in range(KC):
                    gparts = [(win_d[l][:, OFF_GATE + g * D + m * 128:OFF_GATE + g * D + (m + 1) * 128],
                               128, KC, 128) for g in range(3)]
                    gv, gk = load_slab(gparts)
                    bparts = [(wbr_d[l, 0][:, m * 128:(m + 1) * 128], 64, 8, 128),
                              (wbr_d[l, 1][:, m * 128:(m + 1) * 128], 128, 4, 128),
                              (wbr_d[l, 2][:, m * 128:(m + 1) * 128], 128, 4, 128)]
                    bv, bk = load_slab(bparts)
                    for g in range(3):
                        pg = next_ps()
                        mm(pg, [(gv[g][:, kc, :], xnT[:, kc, :]) for kc in range(KC)], rkeys=(gk, 'xnT'))
                        gi = g % 2
                        sc.op('act', lambda e: e.activation(gsig[gi][:], ps[pg][:], AF.Sigmoid,
                                                            bias=ppl[:, g * 8 + m:g * 8 + m + 1], scale=1.0),
                              r=('ps%d' % pg, 'ppl'), w=('gsig%d' % gi,))
                        pu = next_ps()
                        if g == 0:
                            prs = [(bv[0][:, hh, :], yaT[:, hh, :]) for hh in range(8)]
                            rk = (bk, 'yaT')
                        elif g == 1:
                            prs = [(bv[1][:, c4, :], ybT[:, c4, :]) for c4 in range(4)]
                            rk = (bk, 'ybT')
                        else:
                            prs = [(bv[2][:, c4, :], ycT[:, c4, :]) for c4 in range(4)]
                            rk = (bk, 'ycT')
                        mm(pu, prs, rkeys=rk)
                        if g == 0:
                            sc.op('dve', lambda e: e.tensor_tensor(macc[:], gsig[gi][:], ps[pu][:], ALU.mult),
                                  r=('gsig%d' % gi, 'ps%d' % pu), w=('macc',))
                        else:
                            sc.op('dve', lambda e: e.tensor_tensor(mtmp[:], gsig[gi][:], ps[pu][:], ALU.mult),
                                  r=('gsig%d' % gi, 'ps%d' % pu), w=('mtmp',))
                            dst = macc[:] if g == 1 else mixT[:, m, :]
                            dk = 'macc' if g == 1 else 'mixT'
                            sc.op('pool', lambda e: e.tensor_tensor(dst, macc[:], mtmp[:], ALU.add),
                                  r=('macc', 'mtmp'), w=(dk,))
                for half in range(2):
                    (wv,), wk = load_slab([(wout_d[l][:, half * 512:(half + 1) * 512], 128, KC, 512)])
                    for tt in range(4):
                        gt = blk * 4 + tt
                        pi = next_ps()
                        mm(pi, [(mixT[:, kc, tt * 128:(tt + 1) * 128], wv[:, kc, :]) for kc in range(KC)],
                           rkeys=(wk, 'mixT'))
                        sc.op('dve', lambda e: e.tensor_tensor(h[:, gt, half * 512:(half + 1) * 512],
                                                               h[:, gt, half * 512:(half + 1) * 512],
                                                               ps[pi][:], ALU.add),
                              r=(('h', gt), 'ps%d' % pi), w=(('h', gt),))
                sc.barrier()
                if flags.get('skip_ffn', False):
                    continue
                rmsnorm_to_xnT(blk, gffn, 'gffn', 1)
                for f in range(NFF):
                    wparts = [(fup_d[l][:, f * 128:(f + 1) * 128], 128, KC, 128),
                              (fup_d[l][:, DFF + f * 128:DFF + (f + 1) * 128], 128, KC, 128)]
                    wv, wk = load_slab(wparts)
                    bi = f % 2
                    for which in range(2):
                        pi = next_ps()
                        mm(pi, [(wv[which][:, kc, :], xnT[:, kc, :]) for kc in range(KC)], rkeys=(wk, 'xnT'))
                        buf = (fg if which == 0 else fu)[bi]
                        bkey = ('fg%d' if which == 0 else 'fu%d') % bi
                        cbuf = (fgc if which == 0 else fuc)[bi]
                        ckey = 'fgc0' if which == 0 else 'fuc0'
                        hc = which * NFF + f
                        sc.op('act', lambda e: e.copy(buf[:, 2:514], ps[pi][:]), r=('ps%d' % pi,), w=(bkey,))
                        sc.op('pool', lambda e: e.tensor_copy(buf[:, 0:2], f_halo[:, hc, :]),
                              r=(('f_halo', hc),), w=(bkey,))
                        sc.op('pool', lambda e: e.tensor_copy(f_halo[:, hc, :], buf[:, 512:514]),
                              r=(bkey,), w=(('f_halo', hc),))
                        eng = 'dve'
                        cw = lambda j: ppl[:, 36 + hc * 3 + j:36 + hc * 3 + j + 1]
                        sc.op(eng, lambda e: e.tensor_scalar(cbuf[:], buf[:, 0:512], cw(0), None, ALU.mult),
                              r=(bkey, 'ppl'), w=(ckey,))
                        sc.op(eng, lambda e: e.scalar_tensor_tensor(cbuf[:], buf[:, 1:513], cw(1), cbuf[:],
                                                                    ALU.mult, ALU.add),
                              r=(bkey, 'ppl', ckey), w=(ckey,))
                        sc.op(eng, lambda e: e.scalar_tensor_tensor(cbuf[:], buf[:, 2:514], cw(2), cbuf[:],
                                                                    ALU.mult, ALU.add),
                              r=(bkey, 'ppl', ckey), w=(ckey,))
                    sc.op('act', lambda e: e.activation(fsl[bi][:], fgc[bi][:], AF.Silu),
                          r=('fgc0',), w=('fgc0',))
                    sc.op('dve', lambda e: e.tensor_tensor(actT[:, f, :], fsl[bi][:], fuc[bi][:], ALU.mult),
                          r=('fgc0', 'fuc0'), w=(('actT', f),))
                for half in range(2):
                    pis = [next_ps() for _ in range(4)]
                    groups = [(0, 8), (8, 16), (16, 22)]
                    for gi_, (f0, f1) in enumerate(groups):
                        (wv,), wk = load_slab([(fdn_d[l][f0 * 128:f1 * 128, half * 512:(half + 1) * 512],
                                                128, f1 - f0, 512)])
                        for tt in range(4):
                            def fn(pe, tt=tt, wv=wv, f0=f0, f1=f1):
                                ins = None
                                for f in range(f0, f1):
                                    ins = pe.matmul(ps[pis[tt]][:], actT[:, f, tt * 128:(tt + 1) * 128],
                                                    wv[:, f - f0, :], start=(f == 0), stop=(f == NFF - 1))
                                return ins
                            rk = (wk,) + tuple(('actT', f) for f in range(f0, f1))
                            if gi_ == 0:
                                sc.op('pe', fn, r=rk, w=('ps%d' % pis[tt],))
                            else:
                                sc.op('pe', fn, r=rk + ('ps%d' % pis[tt],), w=('ps%d' % pis[tt],))
                    for tt in range(4):
                        gt = blk * 4 + tt
                        sc.op('dve', lambda e: e.tensor_tensor(h[:, gt, half * 512:(half + 1) * 512],
                                                               h[:, gt, half * 512:(half + 1) * 512],
                                                               ps[pis[tt]][:], ALU.add),
                              r=(('h', gt), 'ps%d' % pis[tt]), w=(('h', gt),))
        sc.barrier()
        sc.dma('pool', gmix[:], norms_d[2 * L:2 * L + 1, :].broadcast_to([128, D]), w=('gmix',))
        for gt in range(NT):
            col = gt
            sc.op('act', lambda e: e.activation(junk[:], h[:, gt, :], AF.Square,
                                                accum_out=ss[:, col:col + 1]),
                  r=(('h', gt),), w=('junk', ('ss', col)))
            sc.op('act', lambda e: e.activation(rstd[:, col:col + 1], ss[:, col:col + 1], AF.Sqrt,
                                                bias=consts[:, 261:262], scale=1.0 / D),
                  r=(('ss', col), 'consts'), w=(('rstd', col),))
            sc.op('dve', lambda e: e.reciprocal(rstd[:, col:col + 1], rstd[:, col:col + 1]),
                  r=(('rstd', col),), w=(('rstd', col),))
            oi = gt % 2
            sc.op('dve', lambda e: e.scalar_tensor_tensor(outb[oi][:], h[:, gt, :],
                                                          rstd[:, col:col + 1], gmix[:],
                                                          ALU.mult, ALU.mult),
                  r=(('h', gt), ('rstd', col), 'gmix'), w=('outb%d' % oi,))
            sc.dma('sp', y_d[sq, gt * 128:(gt + 1) * 128, :], outb[oi][:], r=('outb%d' % oi,), w=(('y', sq, gt),))
    sc.finish([('y', sq, gt) for sq in range(NSEQ) for gt in range(NT)])
    print('instructions emitted:', sc.nins, sc.cnt, flush=True)
    return nc


def _swap_idx(dh):
    rot = dh // 4
    half = rot // 2
    idx = np.arange(dh)
    idx[:half] = np.arange(half) + half
    idx[half:rot] = np.arange(half)
    return idx


def _att_cols():
    main = []
    swap = []
    s64 = _swap_idx(64)
    s32 = _swap_idx(32)
    for i in range(4):
        for hh in (i, 4 + i):
            main += [OFF_Q + hh * 64 + d for d in range(64)]
            swap += [OFF_Q + hh * 64 + int(s64[d]) for d in range(64)]
    for hh in range(2):
        main += [OFF_K + hh * 64 + d for d in range(64)]
        swap += [OFF_K + hh * 64 + int(s64[d]) for d in range(64)]
    for hh in range(8):
        main += [OFF_QI + hh * 32 + d for d in range(32)]
        swap += [OFF_QI + hh * 32 + int(s32[d]) for d in range(32)]
    for rep in range(2):
        main += [OFF_KI + d for d in range(32)]
        swap += [OFF_KI + int(s32[d]) for d in range(32)]
    vw = [OFF_V + d for d in range(128)] + [OFF_WI + d for d in range(8)]
    assert len(main) == ATT_MAIN and len(swap) == ATT_MAIN
    return np.array(main + swap + vw)


def _consts():
    c = np.zeros((128, 1024), np.float32)
    c[:, 0:128] = np.eye(128, dtype=np.float32)
    p = np.arange(128)
    theta = 500000.0
    d = p % 64
    c[:, 128] = np.where(d < 16, theta ** (-(d % 8) * 2.0 / 16), 0.0)
    c[:, 129] = np.where(d < 8, -1.0, np.where(d < 16, 1.0, 0.0))
    d = p % 32
    c[:, 130] = np.where(d < 8, theta ** (-(d % 4) * 2.0 / 8), 0.0)
    c[:, 131] = np.where(d < 4, -1.0, np.where(d < 8, 1.0, 0.0))
    c[:, 132] = -math.pi
    c[:, 261] = RMS_EPS
    c[:, 263:327] = 1.0
    c[0:64, 327:391] = np.triu(np.ones((64, 64), np.float32), 1)
    c[0:64, 391:455] = np.triu(np.ones((64, 64), np.float32), 0)
    c[0:64, 455:519] = np.tril(np.ones((64, 64), np.float32), -1)
    c[0:64, 519:583] = 1.0
    c[64:128, 583:647] = 1.0
    c[:, 647:775] = 1.0
    c[:, 647] = 0.0
    c[:, 711] = 0.0
    c[0:64, 775:839] = np.eye(64, dtype=np.float32)
    c[0:64, 839] = 1.0
    c[64:128, 840] = 1.0
    c[:, 262] = 64e-5
    t = np.arange(128)[:, None]
    s_ = np.arange(128)[None, :]
    c[:, 133:261] = np.where(s_ <= t, 0.0, NEG)
    return c


def _prep(inputs, L, ncores):
    f = lambda k: np.asarray(inputs[k], dtype=np.float32)
    w_in = f('w_in')[:L]
    w_att = np.ascontiguousarray(w_in[:, :, _att_cols()])
    norms = np.concatenate([np.stack([f('norm_mix')[l], f('norm_ffn')[l]]) for l in range(L)]
                           + [f('norm_final')[None]], axis=0)
    pp = np.zeros((L, 128, 256), np.float32)
    bg = f('b_gate')[:L].reshape(L, 3, 8, 128)
    pp[:, :, 0:24] = bg.transpose(0, 3, 1, 2).reshape(L, 128, 24)
    scv = f('sc_conv')[:L].reshape(L, 3, 4, 128)
    pp[:, :, 24:36] = scv.transpose(0, 3, 2, 1).reshape(L, 128, 12)
    fc = f('ffn_conv')[:L].reshape(L, 3, 2, NFF, 128)
    pp[:, :, 36:168] = fc.transpose(0, 4, 2, 3, 1).reshape(L, 128, 132)
    mu = f('rw_mu')[:L]
    pp[:, :, 168:180] = mu[:, 0:1536].reshape(L, 12, 128).transpose(0, 2, 1)
    pp[:, :, 180] = mu[:, 1536:1664]
    pp[:, :, 181] = mu[:, 1664:1792]
    def pair4(v):
        return v.reshape(L, 4, 128).transpose(0, 2, 1)
    pp[:, :, 182:186] = pair4(f('rw_w0')[:L])
    pp[:, :, 186:190] = pair4(f('rw_a0')[:L])
    pp[:, :, 190:194] = pair4(f('rw_k_k')[:L])
    pp[:, :, 194:198] = pair4(f('rw_k_a')[:L])
    pp[:, :, 198:202] = pair4(f('rw_r_k')[:L].reshape(L, 512))
    pp[:, :, 202:206] = pair4(f('rw_ln_w')[:L])
    pp[:, :, 206:210] = pair4(f('rw_ln_b')[:L])
    rws = np.zeros((L, 128, 1536), np.float32)
    rws[:, 0:64, 0:512] = f('rw_w_up')[:L]
    rws[:, 64:128, 512:1024] = f('rw_a_up')[:L]
    rws[:, :, 1024:1536] = f('rw_g_up')[:L]
    shared = {
        'w_att': w_att, 'w_in': np.ascontiguousarray(w_in),
        'w_branch': np.ascontiguousarray(f('w_branch')[:L]),
        'w_out': np.ascontiguousarray(f('w_out')[:L]),
        'ffn_up': np.ascontiguousarray(f('ffn_up')[:L]),
        'ffn_down': np.ascontiguousarray(f('ffn_down')[:L]),
        'norms': np.ascontiguousarray(norms), 'pp_layer': pp, 'consts': _consts(), 'rw_small': rws,
    }
    x = f('x')
    pos = np.asarray(inputs['positions']).astype(np.int32)
    B = x.shape[0]
    per = B // ncores
    maps = []
    for c in range(ncores):
        m = dict(shared)
        m['x'] = np.ascontiguousarray(x[c * per:(c + 1) * per])
        m['positions'] = np.ascontiguousarray(pos[c * per:(c + 1) * per])
        maps.append(m)
    return maps, per


def run(inputs, L=4, ncores=8, flags=None, trace=False):
    flags = flags or {}
    maps, per = _prep(inputs, L, ncores)
    S = maps[0]['x'].shape[1]
    nc = build_program(S, L, per, flags)
    res = run_bass_kernel_spmd(nc, maps, core_ids=list(range(ncores)), trace=trace)
    out = np.concatenate([r['y'] for r in res.results], axis=0)
    return out, res


def kernel(**inputs):
    out, _ = run(inputs, L=4, ncores=8)
    return out.astype(np.float32)
```
